# Optimizing a Trainium2 kernel written in Bass

```python
import math
import jax, jax.numpy as jnp
from jax import lax
import numpy as np

D_MODEL = 1024
BATCH = 8
SEQ = 4096
DEPTH = 4

GRID_W = 64
CTX_LEN = 256
N_MIXERS = 4
N_CONV = len(range(0, DEPTH, N_MIXERS))
N_DIFF = len(range(1, DEPTH, N_MIXERS))
N_CHUNK = len(range(2, DEPTH, N_MIXERS))
N_SWA = len(range(3, DEPTH, N_MIXERS))
N_DENSE = len(range(0, DEPTH, 2))
N_MOE = len(range(1, DEPTH, 2))

HEAD_DIM = 64
ROPE_HALF = HEAD_DIM // 2
ROPE_BASE = 10000.0
Q_BLOCK = 128
DIFF_HEADS = D_MODEL // (2 * HEAD_DIM)
SWA_Q_HEADS = D_MODEL // HEAD_DIM
SWA_KV_HEADS = 4
SWA_GROUP = SWA_Q_HEADS // SWA_KV_HEADS
SWA_WINDOW = 128
CHUNK = 128
CM_WIDTH = 2 * D_MODEL
CM_GROUPS = 8
FFN_DIM = 2816
N_EXPERTS = 8
TOP_K = 2
MOE_BLOCK = 256
EPS = 1e-6

kernel_name = "hybrid_interleaved_diffusion_backbone"


def rms_norm(x, g):
    xf = x.astype(jnp.float32)
    y = xf * lax.rsqrt(jnp.mean(xf * xf, axis=-1, keepdims=True) + EPS)
    return (y * g.astype(jnp.float32)).astype(x.dtype)


def adaln(cvec, w, b):
    mod = jax.nn.silu(cvec) @ w + b
    return jnp.split(mod[..., None, :], 6, axis=-1)


def modulate(xn, shift, scale):
    return xn * (1 + scale) + shift


def axial_angles(seq_len):
    rows = seq_len // GRID_W
    row = jnp.repeat(jnp.arange(rows), GRID_W).astype(jnp.float32)
    col = jnp.tile(jnp.arange(GRID_W), rows).astype(jnp.float32)
    n_freq = ROPE_HALF // 2
    inv = ROPE_BASE ** (-jnp.arange(n_freq, dtype=jnp.float32) / n_freq)
    return row[:, None] * inv, col[:, None] * inv


def rope_1d(x, ang):
    cos = jnp.cos(ang)[None, :, None, :]
    sin = jnp.sin(ang)[None, :, None, :]
    x1, x2 = jnp.split(x.astype(jnp.float32), 2, axis=-1)
    return jnp.concatenate([x1 * cos - x2 * sin, x1 * sin + x2 * cos], axis=-1).astype(x.dtype)


def rope_2d(x, angles):
    ang_row, ang_col = angles
    return jnp.concatenate([rope_1d(x[..., :ROPE_HALF], ang_row),
                            rope_1d(x[..., ROPE_HALF:], ang_col)], axis=-1)


def short_conv_seq(a, w_in, w_conv, w_out):
    bg, cg, xv = jnp.split(a @ w_in, 3, axis=-1)
    y = cg * xv
    yp = jnp.pad(y, ((0, 0), (1, 1), (0, 0)))
    conv = w_conv[0] * yp[:, :-2] + w_conv[1] * yp[:, 1:-1] + w_conv[2] * yp[:, 2:]
    return (bg * conv) @ w_out


def short_conv_mixer(a_lat, a_ctx, w_in, w_conv, w_out, ctx_out):
    y_lat = short_conv_seq(a_lat, w_in, w_conv, w_out)
    y_ctx = short_conv_seq(a_ctx, w_in, w_conv, w_out) if ctx_out else None
    return y_lat, y_ctx


def diff_core(q, k, v, lam):
    logits = jnp.einsum('bqhd,bkhd->bhqk', q, k, preferred_element_type=jnp.float32) * (HEAD_DIM ** -0.5)
    p = jax.nn.softmax(logits, axis=-1)
    b_, h2, nq, nk = p.shape
    p = p.reshape(b_, h2 // 2, 2, nq, nk)
    w = p[:, :, 0] - lam * p[:, :, 1]
    return jnp.einsum('bhqk,bkhe->bqhe', w.astype(v.dtype), v)


def diff_attn_mixer(a_lat, a_ctx, w_qkv, w_out, q_g, k_g, lam_p, sub_g, layer, angles, ctx_out):
    lam_init = 0.8 - 0.6 * math.exp(-0.3 * layer)
    lp = lam_p.astype(jnp.float32)
    lam = jnp.exp(jnp.sum(lp[0] * lp[1])) - jnp.exp(jnp.sum(lp[2] * lp[3])) + lam_init

    def proj(a):
        b_, s_, _ = a.shape
        q, k, v = jnp.split(a @ w_qkv, 3, axis=-1)
        q = rms_norm(q.reshape(b_, s_, 2 * DIFF_HEADS, HEAD_DIM), q_g)
        k = rms_norm(k.reshape(b_, s_, 2 * DIFF_HEADS, HEAD_DIM), k_g)
        return q, k, v.reshape(b_, s_, DIFF_HEADS, 2 * HEAD_DIM)

    def finish(o):
        o = rms_norm(o, sub_g) * (1 - lam_init)
        return o.reshape(o.shape[0], o.shape[1], -1) @ w_out

    q_l, k_l, v_l = proj(a_lat)
    q_l = rope_2d(q_l, angles)
    k_l = rope_2d(k_l, angles)
    q_c, k_c, v_c = proj(a_ctx)
    k_all = jnp.concatenate([k_c, k_l], axis=1)
    v_all = jnp.concatenate([v_c, v_l], axis=1)
    b_, s_ = a_lat.shape[:2]
    nb = s_ // Q_BLOCK
    q_blocks = q_l.reshape(b_, nb, Q_BLOCK, 2 * DIFF_HEADS, HEAD_DIM).swapaxes(0, 1)
    o = lax.map(lambda qb: diff_core(qb, k_all, v_all, lam), q_blocks)
    o = o.swapaxes(0, 1).reshape(b_, s_, DIFF_HEADS, 2 * HEAD_DIM)
    y_lat = finish(o)
    y_ctx = finish(diff_core(q_c, k_c, v_c, lam)) if ctx_out else None
    return y_lat, y_ctx


def chunk_mlp_seq(a, w_in, b_in, v_g, w_s, b_s, w_out):
    b_, s_, _ = a.shape
    z = jax.nn.gelu(a @ w_in + b_in)
    u, v = jnp.split(z, 2, axis=-1)
    v = rms_norm(v, v_g).reshape(b_, s_ // CHUNK, CHUNK, CM_GROUPS, CM_WIDTH // CM_GROUPS)
    sv = jnp.einsum('gpq,bnqgc->bnpgc', w_s, v) + b_s.T[:, :, None]
    return (u * sv.reshape(b_, s_, CM_WIDTH)) @ w_out


def chunk_mlp_mixer(a_lat, a_ctx, w_in, b_in, v_g, w_s, b_s, w_out, ctx_out):
    y_lat = chunk_mlp_seq(a_lat, w_in, b_in, v_g, w_s, b_s, w_out)
    y_ctx = chunk_mlp_seq(a_ctx, w_in, b_in, v_g, w_s, b_s, w_out) if ctx_out else None
    return y_lat, y_ctx


def sink_attend(q, ks, vs, masks, sink):
    logits = []
    for k, m in zip(ks, masks):
        l = jnp.einsum('bqhgd,bkhd->bhgqk', q, k, preferred_element_type=jnp.float32) * (HEAD_DIM ** -0.5)
        if m is not None:
            l = jnp.where(m, l, -jnp.inf)
        logits.append(l)
    b_, nq = q.shape[0], q.shape[1]
    sink_col = jnp.broadcast_to(sink.astype(jnp.float32)[None, :, :, None, None],
                                (b_, SWA_KV_HEADS, SWA_GROUP, nq, 1))
    p = jax.nn.softmax(jnp.concatenate(logits + [sink_col], axis=-1), axis=-1)
    outs = []
    off = 0
    for v in vs:
        n = v.shape[1]
        outs.append(jnp.einsum('bhgqk,bkhd->bqhgd', p[..., off:off + n].astype(v.dtype), v))
        off += n
    return sum(outs)


def swa_mixer(a_lat, a_ctx, w_qkv, w_out, q_g, k_g, sink, angles, ctx_out):
    qd = SWA_Q_HEADS * HEAD_DIM
    kvd = SWA_KV_HEADS * HEAD_DIM
    sink_kg = sink.reshape(SWA_KV_HEADS, SWA_GROUP)

    def proj(a, rotate):
        b_, s_, _ = a.shape
        qkv = a @ w_qkv
        q = rms_norm(qkv[..., :qd].reshape(b_, s_, SWA_Q_HEADS, HEAD_DIM), q_g)
        k = rms_norm(qkv[..., qd:qd + kvd].reshape(b_, s_, SWA_KV_HEADS, HEAD_DIM), k_g)
        v = qkv[..., qd + kvd:].reshape(b_, s_, SWA_KV_HEADS, HEAD_DIM)
        if rotate:
            q = rope_2d(q, angles)
            k = rope_2d(k, angles)
        return q.reshape(b_, s_, SWA_KV_HEADS, SWA_GROUP, HEAD_DIM), k, v

    q_l, k_l, v_l = proj(a_lat, True)
    q_c, k_c, v_c = proj(a_ctx, False)
    b_, s_ = a_lat.shape[:2]
    nb = s_ // Q_BLOCK
    band = Q_BLOCK + 2 * SWA_WINDOW
    pad = ((0, 0), (SWA_WINDOW, SWA_WINDOW), (0, 0), (0, 0))
    k_pad = jnp.pad(k_l, pad)
    v_pad = jnp.pad(v_l, pad)
    rel = jnp.arange(band)[None, :] - jnp.arange(Q_BLOCK)[:, None]
    in_band = (rel >= 0) & (rel <= 2 * SWA_WINDOW)
    q_blocks = q_l.reshape(b_, nb, Q_BLOCK, SWA_KV_HEADS, SWA_GROUP, HEAD_DIM).swapaxes(0, 1)
    starts = jnp.arange(nb) * Q_BLOCK

    def block(args):
        qb, st = args
        kb = lax.dynamic_slice_in_dim(k_pad, st, band, axis=1)
        vb = lax.dynamic_slice_in_dim(v_pad, st, band, axis=1)
        key_pos = st - SWA_WINDOW + jnp.arange(band)
        mask = in_band & ((key_pos >= 0) & (key_pos < s_))[None, :]
        return sink_attend(qb, [k_c, kb], [v_c, vb], [None, mask], sink_kg)

    o = lax.map(block, (q_blocks, starts)).swapaxes(0, 1).reshape(b_, s_, qd)
    y_lat = o @ w_out
    y_ctx = None
    if ctx_out:
        oc = sink_attend(q_c, [k_c], [v_c], [None], sink_kg)
        y_ctx = oc.reshape(oc.shape[0], oc.shape[1], qd) @ w_out
    return y_lat, y_ctx


def swiglu(a, w1, w3, w2):
    return (jax.nn.silu(a @ w1) * (a @ w3)) @ w2


def moe_ffn(a, w_r, b_r, w1, w3, w2):
    t, d = a.shape
    logits = (a @ w_r).astype(jnp.float32) + b_r.astype(jnp.float32)
    top_v, top_i = lax.top_k(logits, TOP_K)
    gates = jax.nn.softmax(top_v, axis=-1)
    n_assign = t * TOP_K
    expert = top_i.reshape(-1).astype(jnp.int32)
    token = jnp.arange(n_assign, dtype=jnp.int32) // TOP_K
    gate = gates.reshape(-1)
    order = jnp.argsort(expert)
    s_exp, s_tok, s_gate = expert[order], token[order], gate[order]
    counts = jnp.bincount(expert, length=N_EXPERTS).astype(jnp.int32)
    padded = (counts + MOE_BLOCK - 1) // MOE_BLOCK * MOE_BLOCK
    pad_end = jnp.cumsum(padded)
    pad_start = pad_end - padded
    grp_start = jnp.cumsum(counts) - counts
    dest = pad_start[s_exp] + jnp.arange(n_assign, dtype=jnp.int32) - grp_start[s_exp]
    n_blocks = n_assign // MOE_BLOCK + N_EXPERTS
    n_slots = n_blocks * MOE_BLOCK
    slot_tok = jnp.full((n_slots,), t, jnp.int32).at[dest].set(s_tok)
    slot_gate = jnp.zeros((n_slots,), jnp.float32).at[dest].set(s_gate)
    blk_exp = jnp.minimum(jnp.searchsorted(pad_end, jnp.arange(n_blocks, dtype=jnp.int32) * MOE_BLOCK, side='right'),
                          N_EXPERTS - 1)
    a_pad = jnp.concatenate([a, jnp.zeros((1, d), a.dtype)], axis=0)
    xs = a_pad[slot_tok].reshape(n_blocks, MOE_BLOCK, d)
    ys = lax.map(lambda args: swiglu(args[0], w1[args[1]], w3[args[1]], w2[args[1]]), (xs, blk_exp))
    ys = ys.reshape(n_slots, d) * slot_gate[:, None].astype(a.dtype)
    return jnp.zeros_like(a_pad).at[slot_tok].add(ys)[:t]


def setup_inputs(seed: int = 0) -> dict:
    key = jax.random.key(seed)
    ks = iter(jax.random.split(key, 48))
    f32 = jnp.float32
    D = D_MODEL

    def nrm(shape, fan_in):
        return jax.random.normal(next(ks), shape, f32) * (fan_in ** -0.5)

    def gain(shape):
        return 1.0 + 0.1 * jax.random.normal(next(ks), shape, f32)

    def small(shape, s):
        return s * jax.random.normal(next(ks), shape, f32)

    inp = {}
    inp["x"] = jax.random.normal(next(ks), (BATCH, SEQ, D), f32)
    inp["c"] = jax.random.normal(next(ks), (BATCH, D), f32)
    inp["ctx"] = jax.random.normal(next(ks), (BATCH, CTX_LEN, D), f32)
    inp["c_ctx"] = jax.random.normal(next(ks), (D,), f32)
    inp["ada_w"] = 0.5 * nrm((DEPTH, D, 6 * D), D)
    inp["ada_b"] = small((DEPTH, 6 * D), 0.02)
    inp["norm_mix_g"] = gain((DEPTH, D))
    inp["norm_ffn_g"] = gain((DEPTH, D))
    inp["sc_in_w"] = nrm((N_CONV, D, 3 * D), D)
    inp["sc_conv_w"] = nrm((N_CONV, 3, D), 3)
    inp["sc_out_w"] = nrm((N_CONV, D, D), D)
    inp["da_qkv_w"] = nrm((N_DIFF, D, 3 * D), D)
    inp["da_out_w"] = nrm((N_DIFF, D, D), D)
    inp["da_q_norm_g"] = gain((N_DIFF, HEAD_DIM))
    inp["da_k_norm_g"] = gain((N_DIFF, HEAD_DIM))
    inp["da_lambda"] = small((N_DIFF, 4, HEAD_DIM), 0.1)
    inp["da_sub_norm_g"] = gain((N_DIFF, 2 * HEAD_DIM))
    inp["cm_in_w"] = nrm((N_CHUNK, D, 2 * CM_WIDTH), D)
    inp["cm_in_b"] = small((N_CHUNK, 2 * CM_WIDTH), 0.02)
    inp["cm_v_norm_g"] = gain((N_CHUNK, CM_WIDTH))
    inp["cm_ws"] = nrm((N_CHUNK, CM_GROUPS, CHUNK, CHUNK), CHUNK)
    inp["cm_bs"] = small((N_CHUNK, CM_GROUPS, CHUNK), 0.1)
    inp["cm_out_w"] = nrm((N_CHUNK, CM_WIDTH, D), CM_WIDTH)
    inp["sw_qkv_w"] = nrm((N_SWA, D, (SWA_Q_HEADS + 2 * SWA_KV_HEADS) * HEAD_DIM), D)
    inp["sw_out_w"] = nrm((N_SWA, SWA_Q_HEADS * HEAD_DIM, D), SWA_Q_HEADS * HEAD_DIM)
    inp["sw_q_norm_g"] = gain((N_SWA, HEAD_DIM))
    inp["sw_k_norm_g"] = gain((N_SWA, HEAD_DIM))
    inp["sw_sink"] = small((N_SWA, SWA_Q_HEADS), 1.0)
    inp["ffn_w1"] = nrm((N_DENSE, D, FFN_DIM), D)
    inp["ffn_w3"] = nrm((N_DENSE, D, FFN_DIM), D)
    inp["ffn_w2"] = nrm((N_DENSE, FFN_DIM, D), FFN_DIM)
    inp["moe_router_w"] = nrm((N_MOE, D, N_EXPERTS), D)
    inp["moe_router_b"] = small((N_MOE, N_EXPERTS), 0.01)
    inp["moe_w1"] = nrm((N_MOE, N_EXPERTS, D, FFN_DIM), D)
    inp["moe_w3"] = nrm((N_MOE, N_EXPERTS, D, FFN_DIM), D)
    inp["moe_w2"] = nrm((N_MOE, N_EXPERTS, FFN_DIM, D), FFN_DIM)
    return inp


def reference(x, c, ctx, c_ctx, ada_w, ada_b, norm_mix_g, norm_ffn_g,
              sc_in_w, sc_conv_w, sc_out_w,
              da_qkv_w, da_out_w, da_q_norm_g, da_k_norm_g, da_lambda, da_sub_norm_g,
              cm_in_w, cm_in_b, cm_v_norm_g, cm_ws, cm_bs, cm_out_w,
              sw_qkv_w, sw_out_w, sw_q_norm_g, sw_k_norm_g, sw_sink,
              ffn_w1, ffn_w3, ffn_w2,
              moe_router_w, moe_router_b, moe_w1, moe_w3, moe_w2):
    angles = axial_angles(x.shape[1])
    h, hc = x, ctx
    for i in range(DEPTH):
        ctx_out = i < DEPTH - 1
        sh1, sc1, g1, sh2, sc2, g2 = adaln(c, ada_w[i], ada_b[i])
        csh1, csc1, cg1, csh2, csc2, cg2 = adaln(c_ctx, ada_w[i], ada_b[i])
        a = modulate(rms_norm(h, norm_mix_g[i]), sh1, sc1)
        ac = modulate(rms_norm(hc, norm_mix_g[i]), csh1, csc1)
        kind, j = i % N_MIXERS, i // N_MIXERS
        if kind == 0:
            y, yc = short_conv_mixer(a, ac, sc_in_w[j], sc_conv_w[j], sc_out_w[j], ctx_out)
        elif kind == 1:
            y, yc = diff_attn_mixer(a, ac, da_qkv_w[j], da_out_w[j], da_q_norm_g[j], da_k_norm_g[j],
                                    da_lambda[j], da_sub_norm_g[j], i, angles, ctx_out)
        elif kind == 2:
            y, yc = chunk_mlp_mixer(a, ac, cm_in_w[j], cm_in_b[j], cm_v_norm_g[j], cm_ws[j], cm_bs[j],
                                    cm_out_w[j], ctx_out)
        else:
            y, yc = swa_mixer(a, ac, sw_qkv_w[j], sw_out_w[j], sw_q_norm_g[j], sw_k_norm_g[j],
                              sw_sink[j], angles, ctx_out)
        h = h + g1 * y
        if ctx_out:
            hc = hc + cg1 * yc
        a = modulate(rms_norm(h, norm_ffn_g[i]), sh2, sc2)
        f = i // 2
        if i % 2 == 0:
            h = h + g2 * swiglu(a, ffn_w1[f], ffn_w3[f], ffn_w2[f])
            if ctx_out:
                ac = modulate(rms_norm(hc, norm_ffn_g[i]), csh2, csc2)
                hc = hc + cg2 * swiglu(ac, ffn_w1[f], ffn_w3[f], ffn_w2[f])
        else:
            b_, s_, d_ = a.shape
            if ctx_out:
                ac = modulate(rms_norm(hc, norm_ffn_g[i]), csh2, csc2)
                flat = jnp.concatenate([a.reshape(-1, d_), ac.reshape(-1, d_)], axis=0)
                out = moe_ffn(flat, moe_router_w[f], moe_router_b[f], moe_w1[f], moe_w3[f], moe_w2[f])
                h = h + g2 * out[:b_ * s_].reshape(b_, s_, d_)
                hc = hc + cg2 * out[b_ * s_:].reshape(hc.shape)
            else:
                out = moe_ffn(a.reshape(-1, d_), moe_router_w[f], moe_router_b[f], moe_w1[f], moe_w3[f], moe_w2[f])
                h = h + g2 * out.reshape(b_, s_, d_)
    return h
```

```python
import os, math
from contextlib import ExitStack
import numpy as np
import concourse.bass as bass
import concourse.mybir as mybir
from concourse.bass_utils import run_bass_kernel_spmd

F32 = mybir.dt.float32
BF16 = mybir.dt.bfloat16
ALU = mybir.AluOpType
AF = mybir.ActivationFunctionType
AX = mybir.AxisListType

D = 1024
SEQ = 4096
CTXL = 256
T = SEQ + CTXL
FF = 2816
NE = 8
EPS = 1e-6
ENGS = ('pe', 'act', 'dve', 'pool', 'sp')
NDSEM = 28


class Sched:
    def __init__(self):
        self.ops = []
        self.lw = {}
        self.rd = {}

    def add(self, eng, fn, r=(), w=(), dma=False, sg=None, cost=300.0, xfer=0.0):
        deps = set()
        for k in r:
            x = self.lw.get(k)
            if x is not None:
                deps.add(x)
        for k in w:
            x = self.lw.get(k)
            if x is not None:
                deps.add(x)
            deps.update(self.rd.get(k, ()))
        i = len(self.ops)
        self.ops.append([eng, fn, deps, dma, sg, False, 0, float(cost), float(xfer)])
        for k in r:
            self.rd.setdefault(k, []).append(i)
        for k in w:
            self.lw[k] = i
            self.rd[k] = []
        return i

    def schedule(self, window=48, lat=150.0):
        ops = self.ops
        n = len(ops)
        left = [len(o[2]) for o in ops]
        users = [[] for _ in range(n)]
        for i, o in enumerate(ops):
            for d in o[2]:
                users[d].append(i)
        rdy = [0.0] * n
        queues = {e: [] for e in ENGS}
        for i, o in enumerate(ops):
            queues[o[0]].append(i)
        qpos = {e: 0 for e in ENGS}
        win = {e: [] for e in ENGS}
        etime = {e: 0.0 for e in ENGS}
        for e in ENGS:
            q = queues[e]
            while len(win[e]) < window and qpos[e] < len(q):
                win[e].append(q[qpos[e]])
                qpos[e] += 1
        order = []
        best = {e: None for e in ENGS}
        dirty = set(ENGS)
        done = 0
        while done < n:
            for e in dirty:
                b = None
                et = etime[e]
                for i in win[e]:
                    if left[i] == 0:
                        t = rdy[i] if rdy[i] > et else et
                        if b is None or t < b[0]:
                            b = (t, i)
                            if t <= et:
                                break
                best[e] = b
            dirty = set()
            pick = None
            for e in ENGS:
                b = best[e]
                if b is not None and (pick is None or b[0] < pick[0] or (b[0] == pick[0] and b[1] < pick[1])):
                    pick = (b[0], b[1], e)
            assert pick is not None, "scheduler stuck"
            t, i, e = pick
            o = ops[i]
            etime[e] = t + o[7]
            fin = etime[e] + o[8]
            order.append(i)
            done += 1
            win[e].remove(i)
            q = queues[e]
            if qpos[e] < len(q):
                win[e].append(q[qpos[e]])
                qpos[e] += 1
            dirty.add(e)
            for u in users[i]:
                ue = ops[u][0]
                r_ = fin if ue == e and not o[3] else fin + lat
                if r_ > rdy[u]:
                    rdy[u] = r_
                left[u] -= 1
                if left[u] == 0:
                    dirty.add(ue)
        newidx = {old_: new_ for new_, old_ in enumerate(order)}
        nops = []
        for old_ in order:
            o = ops[old_]
            o[2] = {newidx[d] for d in o[2]}
            nops.append(o)
        self.ops = nops

    def emit(self, nc, esem, dsems, st):
        if getattr(self, 'use_sched', True) and os.environ.get("KSCHED", "1") == "1":
            self.schedule()
        ops = self.ops
        pos = {}
        cnt = {e: 0 for e in ENGS}
        for i, o in enumerate(ops):
            pos[i] = cnt[o[0]]
            cnt[o[0]] += 1
        for o in ops:
            if o[3]:
                o[5] = True
        for i, o in enumerate(ops):
            for d in o[2]:
                y = ops[d]
                if y[3]:
                    continue
                if y[0] != o[0] or o[3]:
                    y[5] = True
                elif o[0] != 'pe' and pos[i] - pos[d] <= 2:
                    y[5] = True
        ec = st['ebase']
        sgc = {}
        sgsem = {}
        sgidx = {}
        for o in ops:
            if o[3]:
                sg = o[4]
                if sg not in sgsem:
                    assert len(sgsem) < len(dsems), "too many dma sem groups"
                    sgidx[sg] = len(sgsem)
                    sgsem[sg] = dsems[len(sgsem)]
                    sgc[sg] = st['dbase'][sgidx[sg]]
                sgc[sg] = sgc[sg] + 16
                o[6] = sgc[sg]
            elif o[5]:
                ec[o[0]] += 1
                o[6] = ec[o[0]]
        for sg, i_ in sgidx.items():
            st['dbase'][i_] = sgc[sg]
        prog = {e: [] for e in ENGS}
        seen = {e: {} for e in ENGS}
        for i, o in enumerate(ops):
            waits = {}
            for d in o[2]:
                y = ops[d]
                if y[3]:
                    key = ('d', y[4])
                    sem = sgsem[y[4]]
                else:
                    if y[0] == o[0] and not o[3]:
                        if o[0] == 'pe' or pos[i] - pos[d] > 2:
                            continue
                    key = ('e', y[0])
                    sem = esem[y[0]]
                v = y[6]
                if waits.get(key, (None, 0))[1] < v:
                    waits[key] = (sem, v)
            wl = []
            for key, (sem, v) in waits.items():
                if seen[o[0]].get(key, 0) >= v:
                    continue
                seen[o[0]][key] = v
                wl.append((sem, v))
            prog[o[0]].append((wl, o))

        def run(e, name):
            for wl, o in prog[name]:
                for sem, v in wl:
                    e.wait_ge(sem, v)
                ins = o[1](e)
                if o[3]:
                    ins.then_inc(sgsem[o[4]], 16)
                elif o[5]:
                    ins.then_inc(esem[name], 1)
            if name == 'sp':
                for sg, v in sgc.items():
                    e.wait_ge(sgsem[sg], v)

        with nc.Block() as blk:
            blk.tensor(lambda e: run(e, 'pe'))
            blk.scalar(lambda e: run(e, 'act'))
            blk.vector(lambda e: run(e, 'dve'))
            blk.gpsimd(lambda e: run(e, 'pool'))
            blk.sync(lambda e: run(e, 'sp'))


def _fsz(ap):
    n = 1
    for d in ap.shape[1:]:
        n *= int(d)
    return n


def _cost(eng, ap):
    n = _fsz(ap)
    if eng == 'dve':
        return 70.0 + n / 0.75
    if eng == 'pool':
        return 100.0 + n / 0.485
    return 110.0 + n / 0.96


def mm(S, out, lhsT, rhs, start, stop, r, w):
    c = max(64.0, _fsz(out) / 2.4 + 45.0)
    if lhsT.dtype == F32:
        c *= 4.0
    S.add('pe', lambda e: e.matmul(out, lhsT, rhs, start=start, stop=stop), r, w, cost=c)


def act(S, out, in_, func, r, w, bias=None, scale=None, accum=None):
    kw = {}
    if bias is not None:
        kw['bias'] = bias
    if scale is not None:
        kw['scale'] = scale
    if accum is not None:
        kw['accum_out'] = accum
    S.add('act', lambda e: e.activation(out=out, in_=in_, func=func, **kw), r, w, cost=_cost('act', out))


def tt(S, eng, out, in0, in1, op, r, w):
    S.add(eng, lambda e: e.tensor_tensor(out=out, in0=in0, in1=in1, op=op), r, w, cost=_cost(eng, out))


def ts(S, eng, out, in0, s1, s2, op0, op1, r, w):
    if s2 is None:
        S.add(eng, lambda e: e.tensor_scalar(out=out, in0=in0, scalar1=s1, scalar2=None, op0=op0), r, w, cost=_cost(eng, out))
    else:
        S.add(eng, lambda e: e.tensor_scalar(out=out, in0=in0, scalar1=s1, scalar2=s2, op0=op0, op1=op1), r, w, cost=_cost(eng, out))


def stt(S, out, in0, scalar, in1, op0, op1, r, w):
    S.add('dve', lambda e: e.scalar_tensor_tensor(out=out, in0=in0, scalar=scalar, in1=in1, op0=op0, op1=op1), r, w, cost=_cost('dve', out))


def cp(S, eng, out, in_, r, w):
    S.add(eng, lambda e: e.tensor_copy(out=out, in_=in_), r, w, cost=_cost(eng, out))


def recip(S, out, in_, r, w):
    S.add('dve', lambda e: e.reciprocal(out=out, in_=in_), r, w, cost=_cost('dve', out))


def mset(S, eng, ap, val, r, w):
    S.add(eng, lambda e: e.memset(ap, val), r, w, cost=_cost(eng, ap))


def dma(S, q, out, in_, r, w, sg):
    nb_ = int(out.shape[0]) * _fsz(out) * 4
    S.add(q, lambda e: e.dma_start(out=out, in_=in_), r, w, dma=True, sg=sg,
          cost=(1000.0 if q == 'pool' else 60.0), xfer=2000.0 + nb_ / 150.0)


def pipeline(steps, depth=1):
    pend = []
    for A, B in steps:
        A()
        pend.append(B)
        if len(pend) > depth:
            pend.pop(0)()
    for B in pend:
        B()


def hview(ap):
    return ap.rearrange("(c p) t -> p c t", p=128)


def wview(ap):
    return ap.rearrange("(kc p) f -> p kc f", p=128)


class G:
    pass


_UID = [0]


def _un(name):
    _UID[0] += 1
    return f"{name}_{_UID[0]}"


class _NCW:
    def __init__(self, nc):
        self._nc = nc

    def sbuf_tensor(self, name, shape, dt):
        return self._nc.sbuf_tensor(_un(name), shape, dt)

    def psum_tensor(self, name, shape, dt):
        return self._nc.psum_tensor(_un(name), shape, dt)

    def __getattr__(self, k):
        return getattr(self._nc, k)


def MV(g, l, which, c, s):
    return g.modv[:, l, which * 8 + c, s:s + 1]


def phase_adaln(nc, g, io, sems):
    nc = _NCW(nc)
    S = Sched()
    with ExitStack() as es:
        wb = [es.enter_context(nc.sbuf_tensor(f"adw{i}", [128, 8, 3072], BF16)) for i in range(2)]
        cT = es.enter_context(nc.sbuf_tensor("cT", [128, 8, 2], F32))
        sT = es.enter_context(nc.sbuf_tensor("sT", [128, 8, 2], BF16))
        bT = es.enter_context(nc.sbuf_tensor("bT", [128, 4, 48], F32))
        gm = es.enter_context(nc.sbuf_tensor("gm", [128, 4, 8], F32))
        gf = es.enter_context(nc.sbuf_tensor("gf", [128, 4, 8], F32))
        ps = es.enter_context(nc.psum_tensor("ps_ada", [128, 512], F32))
        mset(S, 'pool', g.ones[:], 1.0, [], ['ones'])
        dma(S, 'sp', g.ident[:], io['ident'], [], ['ident'], 'c0')
        dma(S, 'sp', cT[:], io['cT'], [], ['cT'], 'c1')
        dma(S, 'sp', bT[:], io['ada_bT'], [], ['bT'], 'c2')
        dma(S, 'sp', gm[:], io['gmixT'], [], ['gm'], 'c3')
        dma(S, 'sp', gf[:], io['gffnT'], [], ['gf'], 'c4')
        act(S, sT[:], cT[:], AF.Silu, ['cT'], ['sT'])
        n = 0
        for l in range(4):
            for half in range(2):
                slot = n % 2
                n += 1
                for q in range(2):
                    c0 = half * 3072 + q * 1536
                    dma(S, 'pool', wb[slot][:, :, q * 1536:(q + 1) * 1536], wview(io['ada_w'][l])[:, :, c0:c0 + 1536],
                        [], [('adw', slot, q)], f"adw{slot}_{q}")
                for jj in range(24):
                    j = half * 24 + jj
                    q = (jj * 128) // 1536
                    col = (l * 48 + j) * 2
                    for kc in range(8):
                        mm(S, ps[:, col:col + 2], wb[slot][:, kc, jj * 128:(jj + 1) * 128], sT[:, kc, :],
                           kc == 0, kc == 7, [('adw', slot, q), 'sT'], ['psada'])
        for l in range(4):
            tt(S, 'dve', g.modv[:, l], ps[:, l * 96:(l + 1) * 96].rearrange("p (j s) -> p j s", s=2),
               bT[:, l, :].unsqueeze(2).to_broadcast([128, 48, 2]), ALU.add, ['psada', 'bT'], ['modv'])
            stt(S, g.modv[:, l, 8:16, :], g.modv[:, l, 8:16, :], 1.0,
                gm[:, l, :].unsqueeze(2).to_broadcast([128, 8, 2]), ALU.add, ALU.mult, ['modv', 'gm'], ['modv'])
            stt(S, g.modv[:, l, 32:40, :], g.modv[:, l, 32:40, :], 1.0,
                gf[:, l, :].unsqueeze(2).to_broadcast([128, 8, 2]), ALU.add, ALU.mult, ['modv', 'gf'], ['modv'])
        S.emit(nc, *sems)


def prenorm(S, g, src_cols, hb, hk, ab, ak, tmp, l, which, s, n, sg, a32=None, a32k=None):
    sq = tmp['sq'][:, :, :n]
    xn = tmp['xn'][:, :, :n]
    rs = tmp['rs'][:, :n]
    ps = tmp['ps'][:, :n]
    psk = tmp['psk']
    sqk = tmp.get('sqk', 'sq')
    hks = hk if isinstance(hk, list) else [hk]
    dma(S, 'sp', hb, src_cols, [], hks, sg)
    tt(S, 'pool', sq, hb, hb, ALU.mult, hks, [sqk])
    for c in range(8):
        mm(S, ps, g.ones[:], sq[:, c, :], c == 0, c == 7, [sqk, 'ones'], [psk])
    act(S, rs, ps, AF.Sqrt, [psk], ['rs'], bias=g.epsb[:, 0:1], scale=1.0 / 1024)
    recip(S, rs, rs, ['rs'], ['rs'])
    tt(S, 'dve', xn, hb, rs.unsqueeze(1).to_broadcast([128, 8, n]), ALU.mult, hks + ['rs'], ['xn'])
    for c in range(8):
        if a32 is None:
            act(S, ab[:, c, :], xn[:, c, :], AF.Identity, ['xn'], [ak],
                scale=MV(g, l, which * 3 + 1, c, s), bias=MV(g, l, which * 3, c, s))
        else:
            act(S, a32[:, c, :], xn[:, c, :], AF.Identity, ['xn'], [a32k],
                scale=MV(g, l, which * 3 + 1, c, s), bias=MV(g, l, which * 3, c, s))
            cp(S, 'pool', ab[:, c, :], a32[:, c, :], [a32k], [ak])


def phase_conv(nc, g, io, sems, src, dst, l=0):
    nc = _NCW(nc)
    S = Sched()
    W = 256
    with ExitStack() as es:
        win = es.enter_context(nc.sbuf_tensor("scwin", [128, 8, 3072], BF16))
        wout = es.enter_context(nc.sbuf_tensor("scwout", [128, 8, 1024], BF16))
        cw = es.enter_context(nc.sbuf_tensor("sccw", [128, 8, 3], F32))
        hb = [es.enter_context(nc.sbuf_tensor(f"sch{i}", [128, 8, W + 2], F32)) for i in range(2)]
        ab = [es.enter_context(nc.sbuf_tensor(f"sca{i}", [128, 8, W + 2], BF16)) for i in range(2)]
        sq = es.enter_context(nc.sbuf_tensor("scsq", [128, 8, W + 2], BF16))
        xn = es.enter_context(nc.sbuf_tensor("scxn", [128, 8, W + 2], F32))
        rs = es.enter_context(nc.sbuf_tensor("scrs", [128, W + 2], F32))
        xv = [es.enter_context(nc.sbuf_tensor(f"scxv{i}", [128, W + 2], F32)) for i in range(2)]
        yb = [es.enter_context(nc.sbuf_tensor(f"scy{i}", [128, W + 2], F32)) for i in range(2)]
        cv = [es.enter_context(nc.sbuf_tensor(f"sccv{i}", [128, W], F32)) for i in range(2)]
        ub = [es.enter_context(nc.sbuf_tensor(f"scu{i}", [128, 8, W], BF16)) for i in range(2)]
        pb = [es.enter_context(nc.psum_tensor(f"scp{i}", [128, 512], F32)) for i in range(8)]
        for q in range(2):
            dma(S, 'pool', win[:, :, q * 1536:(q + 1) * 1536], wview(io['sc_in_w'])[:, :, q * 1536:(q + 1) * 1536],
                [], [('win', q)], f"win{q}")
        dma(S, 'pool', wout[:], wview(io['sc_out_w']), [], ['wout'], "wout")
        dma(S, 'sp', cw[:], io['sc_convT'], [], ['cw'], "cw")
        tiles = [(i * W, 0, 0, SEQ) for i in range(SEQ // W)] + [(SEQ, 1, SEQ, T)]
        for ti, (t0, s, slo, shi) in enumerate(tiles):
            sl = ti % 2
            lo = max(t0 - 1, slo)
            hi = min(t0 + W + 1, shi)
            off = lo - (t0 - 1)
            n = hi - lo
            hk = ('h', sl)
            ak = ('a', sl)
            tmp = dict(sq=sq, xn=xn, rs=rs, ps=pb[7], psk=('ps', 7))
            prenorm(S, g, hview(src)[:, :, lo:hi], hb[sl][:, :, off:off + n], hk, ab[sl][:, :, off:off + n], ak,
                    tmp, l, 0, s, n, f"hld{sl}")
            uk = ('u', sl)
            for c in range(8):
                ps3 = [pb[(c % 2) * 3 + i] for i in range(3)]
                pk = [('ps', (c % 2) * 3 + i) for i in range(3)]
                for i in range(3):
                    col = i * 1024 + c * 128
                    q = col // 1536
                    for kc in range(8):
                        mm(S, ps3[i][:, off:off + n], win[:, kc, col:col + 128], ab[sl][:, kc, off:off + n],
                           kc == 0, kc == 7, [('win', q), ak], [pk[i]])
                ys = c % 2
                yk = ('y', ys)
                act(S, xv[ys][:, off:off + n], ps3[2][:, off:off + n], AF.Copy, [pk[2]], [('xv', ys)])
                tt(S, 'dve', yb[ys][:, off:off + n], ps3[1][:, off:off + n], xv[ys][:, off:off + n], ALU.mult,
                   [pk[1], ('xv', ys)], [yk])
                if off == 1:
                    mset(S, 'pool', yb[ys][:, 0:1], 0.0, [], [yk])
                if off + n < W + 2:
                    mset(S, 'pool', yb[ys][:, W + 1:W + 2], 0.0, [], [yk])
                ck = ('cv', ys)
                act(S, cv[ys][:], yb[ys][:, 0:W], AF.Identity, [yk], [ck], scale=cw[:, c, 0:1])
                stt(S, cv[ys][:], yb[ys][:, 1:W + 1], cw[:, c, 1:2], cv[ys][:], ALU.mult, ALU.add, [yk, ck, 'cw'], [ck])
                stt(S, cv[ys][:], yb[ys][:, 2:W + 2], cw[:, c, 2:3], cv[ys][:], ALU.mult, ALU.add, [yk, ck, 'cw'], [ck])
                tt(S, 'dve', ub[sl][:, c, :], ps3[0][:, 1:W + 1], cv[ys][:], ALU.mult, [pk[0], ck], [uk])
            for m in range(8):
                po = pb[6 + (m % 2)]
                pok = ('ps', 6 + (m % 2))
                for c in range(8):
                    mm(S, po[:, :W], wout[:, c, m * 128:(m + 1) * 128], ub[sl][:, c, :], c == 0, c == 7,
                       ['wout', uk], [pok])
                stt(S, hb[sl][:, m, 1:W + 1], po[:, :W], MV(g, l, 2, m, s), hb[sl][:, m, 1:W + 1], ALU.mult, ALU.add,
                    [pok, hk], [hk])
            dma(S, 'sp', hview(dst)[:, :, t0:t0 + W], hb[sl][:, :, 1:W + 1], [hk], [], f"hst{sl}")
        S.emit(nc, *sems)


def phase_ffn(nc, g, io, sems, src, dst, l, moe, with_ctx):
    nc = _NCW(nc)
    S = Sched()
    S.use_sched = False
    f = l // 2
    E = NE if moe else 1
    NG = int(os.environ.get('KNG', FF // 256))
    MAXW = 1280 if with_ctx else 1536
    NS = 4
    with ExitStack() as es:
        hS = es.enter_context(nc.sbuf_tensor("fh", [128, 8, MAXW], F32))
        aS = es.enter_context(nc.sbuf_tensor("fa", [128, 8, MAXW], BF16))
        w1r = [es.enter_context(nc.sbuf_tensor(f"fw1_{i}", [128, 8, 256], BF16)) for i in range(NS)]
        w3r = [es.enter_context(nc.sbuf_tensor(f"fw3_{i}", [128, 8, 256], BF16)) for i in range(NS)]
        w2r = [es.enter_context(nc.sbuf_tensor(f"fw2_{i}", [128, 2, 1024], BF16)) for i in range(NS)]
        sq = es.enter_context(nc.sbuf_tensor("fsq", [128, 8, 512], BF16))
        xn = es.enter_context(nc.sbuf_tensor("fxn", [128, 8, 512], F32))
        rs = es.enter_context(nc.sbuf_tensor("frs", [128, 512], F32))
        s1 = [es.enter_context(nc.sbuf_tensor(f"fs1_{i}", [128, 512], BF16)) for i in range(2)]
        h1 = [es.enter_context(nc.sbuf_tensor(f"fh1_{i}", [128, 2, 512], BF16)) for i in range(2)]
        pb = [es.enter_context(nc.psum_tensor(f"fp{i}", [128, 512], F32)) for i in range(8)]
        if moe:
            gB = es.enter_context(nc.sbuf_tensor("fgB", [128, NE, MAXW], BF16))
            p3g = [es.enter_context(nc.sbuf_tensor(f"fp3g_{i}", [128, 512], BF16)) for i in range(2)]
            yt = [es.enter_context(nc.sbuf_tensor(f"fyt_{i}", [128, 512], F32)) for i in range(2)]
            wr = es.enter_context(nc.sbuf_tensor("fwr", [128, 8, NE], F32))
            brt = es.enter_context(nc.sbuf_tensor("fbr", [128, NE], F32))
            lg = es.enter_context(nc.sbuf_tensor("flg", [128, 4, NE], F32))
            mx = es.enter_context(nc.sbuf_tensor("fmx", [128, 4, 8], F32))
            nt1 = es.enter_context(nc.sbuf_tensor("fnt1", [128, 4], F32))
            msk = es.enter_context(nc.sbuf_tensor("fmsk", [128, 4, NE], F32))
            ex = es.enter_context(nc.sbuf_tensor("fex", [128, 4, NE], F32))
            den = es.enter_context(nc.sbuf_tensor("fden", [128, 4], F32))
            gt = es.enter_context(nc.sbuf_tensor("fgt", [128, 4, NE], F32))
            gbc = es.enter_context(nc.sbuf_tensor("fgbc", [128, 4, NE, 128], F32))
            dma(S, 'sp', wr[:], io['moe_rwT'][f], [], ['wr'], "wr")
            dma(S, 'sp', brt[:], io['moe_rb'][f].partition_broadcast(128), [], ['br'], "br")
            W1 = io['moe_w1'][f]
            W3 = io['moe_w3'][f]
            W2 = io['moe_w2'][f]
        else:
            W1 = [io['ffn_w1'][f]]
            W3 = [io['ffn_w3'][f]]
            W2 = [io['ffn_w2'][f]]
        lat = [(i * 512, 512, 0) for i in range(8)]
        if with_ctx:
            supers = [[lat[2 * i], lat[2 * i + 1]] for i in range(4)]
            supers[3].append((SEQ, CTXL, 1))
        else:
            supers = [[lat[0], lat[1], lat[2]], [lat[3], lat[4], lat[5]], [lat[6], lat[7]]]
        supers = supers[:int(os.environ.get('KSUP', 4))]
        nld = 0
        it = 0
        PF = 2
        gseq = [(si, e_, gi) for si in range(len(supers)) for e_ in range(E) for gi in range(NG)]
        gpos = {k: i for i, k in enumerate(gseq)}
        issued = [0]

        def issue_upto(n):
            while issued[0] < min(n, len(gseq)):
                _, ee, gg = gseq[issued[0]]
                sl_ = issued[0] % NS
                jj0 = gg * 256
                dma(S, 'pool', w1r[sl_][:], wview(W1[ee])[:, :, jj0:jj0 + 256], [], [('w1', sl_)], f"w1_{sl_}")
                dma(S, 'pool', w3r[sl_][:], wview(W3[ee])[:, :, jj0:jj0 + 256], [], [('w3', sl_)], f"w3_{sl_}")
                dma(S, 'pool', w2r[sl_][:], W2[ee][jj0:jj0 + 256, :].rearrange("(j p) m -> p j m", p=128), [],
                    [('w2', sl_)], f"w2_{sl_}")
                issued[0] += 1

        issue_upto(PF)
        for si_, st in enumerate(supers):
            offs = []
            o_ = 0
            for (t0, W, s) in st:
                offs.append(o_)
                o_ += W
            for (t0, W, s), off in zip(st, offs):
                hk = [('h', off, m_) for m_ in range(8)]
                ak = ('a', off)
                tmp = dict(sq=sq, xn=xn, rs=rs, ps=pb[7], psk=('ps', 7))
                if not moe:
                    prenorm(S, g, hview(src)[:, :, t0:t0 + W], hS[:, :, off:off + W], hk, aS[:, :, off:off + W], ak,
                            tmp, l, 1, s, W, f"hld{off}")
                else:
                    prenorm(S, g, hview(src)[:, :, t0:t0 + W], hS[:, :, off:off + W], hk, aS[:, :, off:off + W], ak,
                            tmp, l, 1, s, W, f"hld{off}", a32=xn[:, :, :W], a32k='xn')
                    nb = W // 128
                    for b in range(nb):
                        for kc in range(8):
                            mm(S, pb[6][:, b * 8:(b + 1) * 8], xn[:, kc, b * 128:(b + 1) * 128], wr[:, kc, :],
                               kc == 0, kc == 7, ['xn', 'wr'], [('ps', 6)])
                    tt(S, 'dve', lg[:, :nb, :], pb[6][:, :nb * 8].rearrange("p (b e) -> p b e", e=8),
                       brt[:].unsqueeze(1).to_broadcast([128, nb, NE]), ALU.add, [('ps', 6), 'br'], ['lg'])
                    for b in range(nb):
                        S.add('dve', (lambda o_, i_: (lambda e: e.max(out=o_, in_=i_)))(mx[:, b, :], lg[:, b, :]),
                              ['lg'], ['mx'])
                    ts(S, 'dve', nt1[:, :nb], mx[:, :nb, 0], -1.0, None, ALU.mult, None, ['mx'], ['nt1'])
                    for b in range(nb):
                        ts(S, 'dve', msk[:, b, :], lg[:, b, :], mx[:, b, 1:2], None, ALU.is_ge, None, ['lg', 'mx'], ['msk'])
                        act(S, ex[:, b, :], lg[:, b, :], AF.Exp, ['lg', 'nt1'], ['ex'], bias=nt1[:, b:b + 1], scale=1.0)
                    tt(S, 'dve', ex[:, :nb, :], ex[:, :nb, :], msk[:, :nb, :], ALU.mult, ['ex', 'msk'], ['ex'])
                    S.add('dve', (lambda o_, i_: (lambda e: e.tensor_reduce(out=o_, in_=i_, axis=AX.X, op=ALU.add)))(
                        den[:, :nb], ex[:, :nb, :]), ['ex'], ['den'])
                    recip(S, den[:, :nb], den[:, :nb], ['den'], ['den'])
                    tt(S, 'dve', gt[:, :nb, :], ex[:, :nb, :], den[:, :nb].unsqueeze(2).to_broadcast([128, nb, NE]),
                       ALU.mult, ['ex', 'den'], ['gt'])
                    cp(S, 'pool', gbc[:, :nb], gt[:, :nb, :].unsqueeze(3).to_broadcast([128, nb, NE, 128]), ['gt'], ['gbc'])
                    for e_ in range(NE):
                        pg = pb[4 + (e_ % 2)]
                        pgk = ('ps', 4 + (e_ % 2))
                        for b in range(nb):
                            mm(S, pg[:, b * 128:(b + 1) * 128], gbc[:, b, e_, :], g.ident[:], True, True,
                               ['gbc', 'ident'], [pgk])
                        act(S, gB[:, e_, off:off + W], pg[:, :W], AF.Copy, [pgk], [('gB', off)])
            steps = []
            for e_ in range(E):
                for gi in range(NG):
                    gidx = gpos[(si_, e_, gi)]
                    slot = gidx % NS
                    j0 = gi * 256
                    first = True
                    for (t0, W, s), off in zip(st, offs):
                        hs = it % 2
                        it += 1

                        def A(e_=e_, slot=slot, j0=j0, first=first, W=W, s=s, off=off, hs=hs, gidx=gidx):
                            if first:
                                issue_upto(gidx + PF + 1)
                            ak = ('a', off)
                            h1k = ('h1', hs)
                            for jj in range(2):
                                ss = jj
                                p1 = pb[jj * 2]
                                p3 = pb[jj * 2 + 1]
                                p1k = ('ps', jj * 2)
                                p3k = ('ps', jj * 2 + 1)
                                for kc in range(8):
                                    mm(S, p1[:, :W], w1r[slot][:, kc, jj * 128:(jj + 1) * 128], aS[:, kc, off:off + W],
                                       kc == 0, kc == 7, [('w1', slot), ak], [p1k])
                                for kc in range(8):
                                    mm(S, p3[:, :W], w3r[slot][:, kc, jj * 128:(jj + 1) * 128], aS[:, kc, off:off + W],
                                       kc == 0, kc == 7, [('w3', slot), ak], [p3k])
                                act(S, s1[ss][:, :W], p1[:, :W], AF.Silu, [p1k], [('s1', ss)])
                                if moe:
                                    tt(S, 'dve', p3g[ss][:, :W], p3[:, :W], gB[:, e_, off:off + W], ALU.mult,
                                       [p3k, ('gB', off)], [('p3g', ss)])
                                    tt(S, 'pool', h1[hs][:, jj, :W], s1[ss][:, :W], p3g[ss][:, :W], ALU.mult,
                                       [('s1', ss), ('p3g', ss)], [h1k])
                                else:
                                    tt(S, 'dve', h1[hs][:, jj, :W], p3[:, :W], s1[ss][:, :W], ALU.mult,
                                       [p3k, ('s1', ss)], [h1k])

                        def B(slot=slot, W=W, s=s, off=off, hs=hs):
                            h1k = ('h1', hs)
                            for m in range(8):
                                po = pb[4 + (m % 4)]
                                pok = ('ps', 4 + (m % 4))
                                for jj in range(2):
                                    mm(S, po[:, :W], w2r[slot][:, jj, m * 128:(m + 1) * 128], h1[hs][:, jj, :W],
                                       jj == 0, jj == 1, [('w2', slot), h1k], [pok])
                                hk = ('h', off, m)
                                if m % 2 == 0 or not moe:
                                    stt(S, hS[:, m, off:off + W], po[:, :W], MV(g, l, 5, m, s), hS[:, m, off:off + W],
                                        ALU.mult, ALU.add, [pok, hk], [hk])
                                else:
                                    ys = (m // 2) % 2
                                    act(S, yt[ys][:, :W], po[:, :W], AF.Identity, [pok], [('yt', ys)], scale=MV(g, l, 5, m, s))
                                    tt(S, 'pool', hS[:, m, off:off + W], hS[:, m, off:off + W], yt[ys][:, :W], ALU.add,
                                       [('yt', ys), hk], [hk])

                        steps.append((A, B))
                        first = False
            pipeline(steps, 1)
            for (t0, W, s), off in zip(st, offs):
                dma(S, 'sp', hview(dst)[:, :, t0:t0 + W], hS[:, :, off:off + W], [('h', off, m_) for m_ in range(8)], [], f"hst{off}")
        S.emit(nc, *sems)


def phase_gmlp(nc, g, io, sems, src, dst, l=2):
    nc = _NCW(nc)
    S = Sched()
    S.use_sched = False
    with ExitStack() as es:
        win = es.enter_context(nc.sbuf_tensor("cmwin", [128, 8, 4096], BF16))
        wout = es.enter_context(nc.sbuf_tensor("cmwout", [128, 16, 1024], BF16))
        wsT = es.enter_context(nc.sbuf_tensor("cmws", [128, 8, 128], BF16))
        bU = es.enter_context(nc.sbuf_tensor("cmbu", [128, 16], F32))
        bV = es.enter_context(nc.sbuf_tensor("cmbv", [128, 2048], F32))
        vg = es.enter_context(nc.sbuf_tensor("cmvg", [128, 16], F32))
        bsT = es.enter_context(nc.sbuf_tensor("cmbs", [128, 8, 128], F32))
        hb = [es.enter_context(nc.sbuf_tensor(f"cmh{i}", [128, 8, 512], F32)) for i in range(1)]
        ab = [es.enter_context(nc.sbuf_tensor(f"cma{i}", [128, 8, 512], BF16)) for i in range(1)]
        xn = es.enter_context(nc.sbuf_tensor("cmxn", [128, 8, 512], F32))
        rs = es.enter_context(nc.sbuf_tensor("cmrs", [128, 512], F32))
        vt = [es.enter_context(nc.sbuf_tensor(f"cmvt{i}", [128, 512], F32)) for i in range(2)]
        gv = es.enter_context(nc.sbuf_tensor("cmgv", [128, 2048], F32))
        junk = es.enter_context(nc.sbuf_tensor("cmjunk", [128, 512], BF16))
        ss = es.enter_context(nc.sbuf_tensor("cmss", [128, 4], F32))
        rv = es.enter_context(nc.sbuf_tensor("cmrv", [128, 1], F32))
        vn = [es.enter_context(nc.sbuf_tensor(f"cmvn{i}", [128, 2048], BF16)) for i in range(4)]
        ub = [es.enter_context(nc.sbuf_tensor(f"cmu{i}", [128, 512], F32)) for i in range(2)]
        m1 = [es.enter_context(nc.sbuf_tensor(f"cmm{i}", [128, 512], F32)) for i in range(2)]
        pr = es.enter_context(nc.sbuf_tensor("cmpr", [128, 16, 512], BF16))
        pb = [es.enter_context(nc.psum_tensor(f"cmp{i}", [128, 512], F32)) for i in range(8)]
        for q in range(4):
            dma(S, 'pool', win[:, :, q * 1024:(q + 1) * 1024], wview(io['cm_in_w'])[:, :, q * 1024:(q + 1) * 1024],
                [], [('win', q)], f"win{q}")
        dma(S, 'pool', wout[:], io['cm_out_w'].rearrange("(c p) m -> p c m", p=128), [], ['wout'], "wout")
        dma(S, 'pool', wsT[:], io['cm_wsT'], [], ['wsT'], "wsT")
        dma(S, 'sp', bU[:], io['cm_buT'], [], ['bU'], "bU")
        dma(S, 'sp', bV[:], io['cm_bv'].partition_broadcast(128), [], ['bV'], "bV")
        dma(S, 'sp', vg[:], io['cm_vgT'], [], ['vg'], "vg")
        dma(S, 'sp', bsT[:], io['cm_bsf'].partition_broadcast(128).rearrange("p o (g q) -> p (o g) q", g=8), [], ['bsT'], "bsT")
        tiles = [(i * 512, 512, 0) for i in range(8)] + [(SEQ, CTXL, 1)]
        nv = 0
        nu = 0
        for ti, (t0, W, s) in enumerate(tiles):
            sl = 0
            hk = ('h', sl)
            ak = ('a', sl)
            tmp = dict(sq=pr[:, 0:8, :], sqk='pr', xn=xn, rs=rs, ps=pb[7], psk=('ps', 7))
            prenorm(S, g, hview(src)[:, :, t0:t0 + W], hb[sl][:, :, :W], hk, ab[sl][:, :, :W], ak, tmp, l, 0, s, W,
                    f"hld{sl}")
            nb = W // 128
            for b in range(nb):
                for q4 in range(4):
                    pv = pb[nv % 2]
                    pvk = ('ps', nv % 2)
                    vts = nv % 2
                    nv += 1
                    col = 2048 + q4 * 512
                    for kc in range(8):
                        mm(S, pv[:], ab[sl][:, kc, b * 128:(b + 1) * 128], win[:, kc, col:col + 512], kc == 0, kc == 7,
                           [ak, ('win', col // 1024)], [pvk])
                    tt(S, 'dve', vt[vts][:], pv[:], bV[:, q4 * 512:(q4 + 1) * 512], ALU.add, [pvk, 'bV'], [('vt', vts)])
                    act(S, gv[:, q4 * 512:(q4 + 1) * 512], vt[vts][:], AF.Gelu_apprx_tanh, [('vt', vts)], ['gv'])
                    act(S, junk[:], gv[:, q4 * 512:(q4 + 1) * 512], AF.Square, ['gv'], ['junk', 'ss'],
                        accum=ss[:, q4:q4 + 1])
                S.add('dve', (lambda o_, i_: (lambda e: e.tensor_reduce(out=o_, in_=i_, axis=AX.X, op=ALU.add)))(
                    rv[:], ss[:]), ['ss'], ['rv'])
                act(S, rv[:], rv[:], AF.Sqrt, ['rv'], ['rv'], bias=g.epsb[:, 0:1], scale=1.0 / 2048)
                recip(S, rv[:], rv[:], ['rv'], ['rv'])
                ts(S, 'dve', vn[b][:], gv[:], rv[:, 0:1], None, ALU.mult, None, ['gv', 'rv'], [('vn', b)])
            for cu in range(16):
                pu = pb[2 + (nu % 2)]
                puk = ('ps', 2 + (nu % 2))
                psv = pb[4 + (nu % 2)]
                psk = ('ps', 4 + (nu % 2))
                us = nu % 2
                nu += 1
                gq = cu // 2
                for kc in range(8):
                    mm(S, pu[:, :W], win[:, kc, cu * 128:(cu + 1) * 128], ab[sl][:, kc, :W], kc == 0, kc == 7,
                       [ak, ('win', (cu * 128) // 1024)], [puk])
                act(S, ub[us][:, :W], pu[:, :W], AF.Gelu_apprx_tanh, [puk, 'bU'], [('ub', us)], bias=bU[:, cu:cu + 1])
                for b in range(nb):
                    mm(S, psv[:, b * 128:(b + 1) * 128], vn[b][:, cu * 128:(cu + 1) * 128], wsT[:, gq, :], True, True,
                       [('vn', b), 'wsT'], [psk])
                stt(S, m1[us][:, :W].rearrange("p (b q) -> p b q", q=128),
                    psv[:, :W].rearrange("p (b q) -> p b q", q=128), vg[:, cu:cu + 1],
                    bsT[:, gq, :].unsqueeze(1).to_broadcast([128, nb, 128]), ALU.mult, ALU.add,
                    [psk, 'vg', 'bsT'], [('m1', us)])
                tt(S, 'pool', pr[:, cu, :W], ub[us][:, :W], m1[us][:, :W], ALU.mult, [('ub', us), ('m1', us)], ['pr'])
            for m in range(8):
                po = pb[6 + (m % 2)]
                pok = ('ps', 6 + (m % 2))
                for cu in range(16):
                    mm(S, po[:, :W], wout[:, cu, m * 128:(m + 1) * 128], pr[:, cu, :W], cu == 0, cu == 15,
                       ['wout', 'pr'], [pok])
                stt(S, hb[sl][:, m, :W], po[:, :W], MV(g, l, 2, m, s), hb[sl][:, m, :W], ALU.mult, ALU.add,
                    [pok, hk], [hk])
            dma(S, 'sp', hview(dst)[:, :, t0:t0 + W], hb[sl][:, :, :W], [hk], [], f"hst{sl}")
        S.emit(nc, *sems)


SWA_PERM = [0, 4, 1, 5, 2, 6, 3, 7, 8, 12, 9, 13, 10, 14, 11, 15]


def phase_qkv(nc, g, io, sems, src, l, kind, scr):
    nc = _NCW(nc)
    S = Sched()
    if kind == 'diff':
        wq, NQC, NKC, VF = io['da_qkv_w'], 8, 8, 1024
        gname = 'da_qkg'
    else:
        wq, NQC, NKC, VF = io['sw_qkv_wp'], 8, 2, 256
        gname = 'sw_qkg'
    NC_ = NQC + NKC
    WCOLS = NC_ * 128 + VF
    with ExitStack() as es:
        win = es.enter_context(nc.sbuf_tensor("qw", [128, 8, WCOLS], BF16))
        gq = es.enter_context(nc.sbuf_tensor("qg", [128, 2], F32))
        RT = es.enter_context(nc.sbuf_tensor("qRT", [128, 128], BF16))
        bones = es.enter_context(nc.sbuf_tensor("qbo", [128, 128], BF16))
        cs = [es.enter_context(nc.sbuf_tensor(f"qcs{i}", [128, 2, 512], F32)) for i in range(2)]
        hb = [es.enter_context(nc.sbuf_tensor(f"qh{i}", [128, 8, 512], F32)) for i in range(2)]
        ab = [es.enter_context(nc.sbuf_tensor(f"qa{i}", [128, 8, 512], BF16)) for i in range(2)]
        sq = es.enter_context(nc.sbuf_tensor("qsq", [128, 8, 512], BF16))
        xn = es.enter_context(nc.sbuf_tensor("qxn", [128, 8, 512], F32))
        rs = es.enter_context(nc.sbuf_tensor("qrs", [128, 512], F32))
        xg = [es.enter_context(nc.sbuf_tensor(f"qxg{i}", [128, 512], BF16)) for i in range(2)]
        xs = [es.enter_context(nc.sbuf_tensor(f"qxs{i}", [128, 512], BF16)) for i in range(2)]
        rd = [es.enter_context(nc.sbuf_tensor(f"qrd{i}", [128, 512], F32)) for i in range(2)]
        t1 = [es.enter_context(nc.sbuf_tensor(f"qt1{i}", [128, 512], F32)) for i in range(2)]
        t2 = [es.enter_context(nc.sbuf_tensor(f"qt2{i}", [128, 512], F32)) for i in range(2)]
        ob = [es.enter_context(nc.sbuf_tensor(f"qo{i}", [128, 512], BF16)) for i in range(3)]
        vb = [es.enter_context(nc.sbuf_tensor(f"qv{i}", [128, 512], BF16)) for i in range(2)]
        pb = [es.enter_context(nc.psum_tensor(f"qp{i}", [128, 512], F32)) for i in range(8)]
        nq = (WCOLS + 1023) // 1024
        for q in range(nq):
            c1 = min(WCOLS, (q + 1) * 1024)
            dma(S, 'pool', win[:, :, q * 1024:c1], wview(wq)[:, :, q * 1024:c1], [], [('win', q)], f"win{q}")
        dma(S, 'sp', gq[:], io[gname], [], ['gq'], "gq")
        dma(S, 'pool', RT[:], io['ropeRT'], [], ['RT'], "RT")
        dma(S, 'pool', bones[:], io['blkones'], [], ['bones'], "bones")
        if kind == 'diff':
            ts(S, 'dve', gq[:, 0:1], gq[:, 0:1], 0.125, None, ALU.mult, None, ['gq'], ['gq'])
        else:
            ts(S, 'dve', gq[:, 0:1], gq[:, 0:1], 0.125, None, ALU.mult, None, ['gq'], ['gq'])
        tiles = [(i * 512, 512, 0) for i in range(8)] + [(SEQ, CTXL, 1)]
        n_ = 0
        no = 0
        nvv = 0
        for ti, (t0, W, s) in enumerate(tiles):
            sl = ti % 2
            hk = ('h', sl)
            ak = ('a', sl)
            tmp = dict(sq=sq, xn=xn, rs=rs, ps=pb[7], psk=('ps', 7))
            prenorm(S, g, hview(src)[:, :, t0:t0 + W], hb[sl][:, :, :W], hk, ab[sl][:, :, :W], ak, tmp, l, 0, s, W,
                    f"hld{sl}")
            dma(S, 'sp', cs[sl][:, :, :W], io['ropecs'][:, :, t0:t0 + W], [], [('cs', sl)], f"cs{sl}")
            chunks = list(range(NC_))
            if kind == 'swa' and s == 1:
                chunks = list(range(NQC, NC_))
            steps = []
            for ch in chunks:
                k2 = n_ % 2
                n_ += 1
                o3 = no % 3
                no += 1

                def A(ch=ch, k2=k2, W=W, sl=sl):
                    isq = ch < NQC
                    pp = pb[k2 * 3]
                    ppk = ('ps', k2 * 3)
                    col = ch * 128
                    for kc in range(8):
                        mm(S, pp[:, :W], win[:, kc, col:col + 128], ab[sl][:, kc, :W], kc == 0, kc == 7,
                           [('a', sl), ('win', col // 1024)], [ppk])
                    gcol = gq[:, 0:1] if isq else gq[:, 1:2]
                    act(S, xg[k2][:, :W], pp[:, :W], AF.Identity, [ppk, 'gq'], [('xg', k2)], scale=gcol)
                    act(S, xs[k2][:, :W], pp[:, :W], AF.Square, [ppk], [('xs', k2)])

                def B(ch=ch, k2=k2, o3=o3, W=W, sl=sl, t0=t0):
                    isq = ch < NQC
                    pm = pb[k2 * 3 + 1]
                    pr_ = pb[k2 * 3 + 2]
                    pmk, prk = ('ps', k2 * 3 + 1), ('ps', k2 * 3 + 2)
                    mm(S, pm[:, :W], bones[:], xs[k2][:, :W], True, True, [('xs', k2), 'bones'], [pmk])
                    mm(S, pr_[:, :W], RT[:], xg[k2][:, :W], True, True, [('xg', k2), 'RT'], [prk])
                    act(S, rd[k2][:, :W], pm[:, :W], AF.Sqrt, [pmk], [('rd', k2)], bias=g.epsb[:, 0:1], scale=1.0 / 64)
                    recip(S, rd[k2][:, :W], rd[k2][:, :W], [('rd', k2)], [('rd', k2)])
                    tt(S, 'pool', t1[k2][:, :W], xg[k2][:, :W], cs[sl][:, 0, :W], ALU.mult, [('xg', k2), ('cs', sl)], [('t1', k2)])
                    tt(S, 'dve', t2[k2][:, :W], pr_[:, :W], cs[sl][:, 1, :W], ALU.mult, [prk, ('cs', sl)], [('t2', k2)])
                    tt(S, 'pool', t1[k2][:, :W], t1[k2][:, :W], t2[k2][:, :W], ALU.add, [('t1', k2), ('t2', k2)], [('t1', k2)])
                    tt(S, 'dve', ob[o3][:, :W], t1[k2][:, :W], rd[k2][:, :W], ALU.mult, [('t1', k2), ('rd', k2)], [('ob', o3)])
                    dst_ = scr['QT'] if isq else scr['KT']
                    cc = ch if isq else ch - NQC
                    dma(S, 'sp', dst_[cc * 128:(cc + 1) * 128, t0:t0 + W], ob[o3][:, :W], [('ob', o3)], [], f"qst{o3}")

                steps.append((A, B))
            pipeline(steps, 1)
            for b in range(W // 128):
                for vq in range((VF + 511) // 512):
                    vw = min(512, VF - vq * 512)
                    v2 = nvv % 2
                    nvv += 1
                    pv = pb[6]
                    col = NC_ * 128 + vq * 512
                    for kc in range(8):
                        mm(S, pv[:, :vw], ab[sl][:, kc, b * 128:(b + 1) * 128], win[:, kc, col:col + vw], kc == 0, kc == 7,
                           [ak, ('win', col // 1024), ('win', (col + vw - 1) // 1024)], [('ps', 6)])
                    act(S, vb[v2][:, :vw], pv[:, :vw], AF.Copy, [('ps', 6)], [('vb', v2)])
                    r0 = t0 + b * 128
                    dma(S, 'sp', scr['V'][r0:r0 + 128, vq * 512:vq * 512 + vw], vb[v2][:, :vw], [('vb', v2)], [], f"vst{v2}")
        S.emit(nc, *sems)


def phase_att(nc, g, io, sems, src, dst, l, kind, scr):
    nc = _NCW(nc)
    S = Sched()
    NB = T // 128
    if kind == 'diff':
        NKC, VF = 8, 1024
    else:
        NKC, VF = 2, 256
    with ExitStack() as es:
        KT = es.enter_context(nc.sbuf_tensor("aKT", [128, NKC, T], BF16))
        V = es.enter_context(nc.sbuf_tensor("aV", [128, NB, VF], BF16))
        if kind == 'diff':
            wout = es.enter_context(nc.sbuf_tensor("awo", [128, 8, 1024], BF16))
            lp = es.enter_context(nc.sbuf_tensor("alp", [128, 4, 64], F32))
            pr2 = es.enter_context(nc.sbuf_tensor("apr2", [128, 2, 64], F32))
            s2 = es.enter_context(nc.sbuf_tensor("as2", [128, 2], F32))
            nlam = es.enter_context(nc.sbuf_tensor("anl", [128, 1], F32))
            sg = es.enter_context(nc.sbuf_tensor("asg", [128, 1], F32))
            OT = es.enter_context(nc.sbuf_tensor("aOT", [128, 8, 512], BF16))
            La = [[es.enter_context(nc.sbuf_tensor(f"aLa{r}{q}", [128, 512], F32)) for q in range(2)] for r in range(2)]
            onesf = es.enter_context(nc.sbuf_tensor("aonesf", [128, 128], F32))
        else:
            wout = es.enter_context(nc.sbuf_tensor("awo", [64, 16, 1024], BF16))
            snk = es.enter_context(nc.sbuf_tensor("asnk", [128, 16], F32))
            msk = es.enter_context(nc.sbuf_tensor("amsk", [128, 6, 512], BF16))
            OT = es.enter_context(nc.sbuf_tensor("aOT", [64, 16, 512], BF16))
        NSL = 1 if kind == 'diff' else 2
        if kind == 'diff':
            QZ = es.enter_context(nc.sbuf_tensor("aQZ", [128, 8, 1024], BF16))
            qz4 = QZ[:].rearrange("p c (r w) -> p c r w", r=2)
            hv = QZ[:].bitcast(F32)
            QT = hb = None
        else:
            QT = [es.enter_context(nc.sbuf_tensor(f"aQ{i}", [128, 8, 1024], BF16)) for i in range(NSL)]
            qzs = [q_[:].rearrange("p c (r w) -> p c r w", r=2) for q_ in QT]
            hb = [es.enter_context(nc.sbuf_tensor(f"ah{i}", [128, 8, 512], F32)) for i in range(NSL)]
        P = [es.enter_context(nc.sbuf_tensor(f"aP{i}", [128, 512], BF16)) for i in range(4)]
        PM = [es.enter_context(nc.sbuf_tensor(f"aPM{i}", [128, 512], BF16)) for i in range(2)] if kind != 'diff' else None
        r0b = es.enter_context(nc.sbuf_tensor("ar0", [128, 512], F32))
        r1b = es.enter_context(nc.sbuf_tensor("ar1", [128, 512], F32))
        o0 = es.enter_context(nc.sbuf_tensor("ao0", [128, 512], F32))
        o1 = r1b
        osq = es.enter_context(nc.sbuf_tensor("aosq", [128, 512], BF16))
        pb = [es.enter_context(nc.psum_tensor(f"ap{i}", [128, 512], F32)) for i in range(8)]
        for c in range(NKC):
            dma(S, 'sp', KT[:, c, :], scr['KT'][c * 128:(c + 1) * 128, :], [], [('KT', c)], f"kt{c % 4}")
        for q in range(4):
            b0, b1 = q * 9, min(NB, (q + 1) * 9)
            dma(S, 'sp', V[:, b0:b1, :], scr['V'][b0 * 128:b1 * 128, :VF].rearrange("(b p) f -> p b f", p=128),
                [], [('V', q)], f"v{q}")
        if kind == 'diff':
            dma(S, 'pool', wout[:], wview(io['da_out_w']), [], ['wout'], "wout")
            mset(S, 'pool', onesf[:], 1.0, [], ['onesf'])
            dma(S, 'sp', lp[:], io['da_lam'].partition_broadcast(128).rearrange("p o (a d) -> p (o a) d", a=4), [], ['lp'], "lp")
            dma(S, 'sp', sg[:], io['da_subg'], [], ['sg'], "sg")
            lam_init = 0.8 - 0.6 * math.exp(-0.3 * l)
            for i in range(2):
                tt(S, 'dve', pr2[:, i, :], lp[:, 2 * i, :], lp[:, 2 * i + 1, :], ALU.mult, ['lp'], ['pr2'])
            S.add('dve', (lambda o_, i_: (lambda e: e.tensor_reduce(out=o_, in_=i_, axis=AX.X, op=ALU.add)))(
                s2[:], pr2[:]), ['pr2'], ['s2'])
            act(S, s2[:], s2[:], AF.Exp, ['s2'], ['s2'])
            tt(S, 'dve', nlam[:], s2[:, 1:2], s2[:, 0:1], ALU.subtract, ['s2'], ['nlam'])
            ts(S, 'dve', nlam[:], nlam[:], -lam_init, None, ALU.add, None, ['nlam'], ['nlam'])
            ts(S, 'dve', sg[:], sg[:], 1.0 - lam_init, None, ALU.mult, None, ['sg'], ['sg'])
            tiles = [(i * 512, 512, 0) for i in range(8)] + [(SEQ, CTXL, 1)]
        else:
            dma(S, 'pool', wout[:], io['sw_out_wp'].rearrange("(h p) m -> p h m", p=64), [], ['wout'], "wout")
            dma(S, 'sp', snk[:], io['sw_sinkp'].partition_broadcast(128), [], ['snk'], "snk")
            dma(S, 'pool', msk[:], io['swa_mask'], [], ['msk'], "msk")
            act(S, snk[:], snk[:], AF.Exp, ['snk'], ['snk'])
            for i_ in range(NSL):
                mset(S, 'pool', QT[i_][:], 0.0, [], [('Q', i_, 0), ('Q', i_, 1)])
            tiles = [(i * 512, 512, 0) for i in range(8)]
        np_ = 0
        for ti, (t0, W, s) in enumerate(tiles):
            sl = ti % NSL
            hk = ('h', sl)
            qk = ('Q', sl)
            if kind == 'diff':
                qsrc = scr['QT'].rearrange("(c p) t -> p c t", p=128)
                for r in range(2):
                    z0 = (1 - r) * 64
                    mset(S, 'pool', qz4[z0:z0 + 64, :, r, :W], 0.0, [], [('QZ', r)])
                    dma(S, 'sp', qz4[r * 64:(r + 1) * 64, :, r, :W], qsrc[r * 64:(r + 1) * 64, :, t0:t0 + W], [],
                        [('QZ', r)], f"qld{r}")
            else:
                dma(S, 'sp', hb[sl][:, :, :W], hview(src)[:, :, t0:t0 + W], [], [hk], f"hld{sl}")
                qsrc = scr['QT'].rearrange("(c p) t -> p c t", p=128)
                for r in range(2):
                    dma(S, 'sp', qzs[sl][r * 64:(r + 1) * 64, :, r, :W], qsrc[r * 64:(r + 1) * 64, :, t0:t0 + W], [],
                        [('Q', sl, r)], f"qld{sl}_{r}")
            if kind == 'diff':
                kbs = [(kb, None) for kb in (range(NB) if s == 0 else range(32, 34))]
                steps = []
                for vh in range(8):
                    for r in range(2):
                        for ki, (kb, _) in enumerate(kbs):
                            i3, i4 = np_ % 3, np_ % 4
                            np_ += 1
                            last = (ki == len(kbs) - 1)

                            def A(vh=vh, r=r, kb=kb, i3=i3, i4=i4, W=W, sl=sl):
                                mm(S, pb[i3][:, :W], KT[:, vh, kb * 128:(kb + 1) * 128],
                                   qz4[:, vh, r, :W], True, True, [('KT', vh), ('QZ', r)], [('ps', i3)])
                                act(S, P[i4][:, :W], pb[i3][:, :W], AF.Exp, [('ps', i3)], [('P', i4)])

                            def B(vh=vh, r=r, kb=kb, ki=ki, last=last, i4=i4, W=W):
                                po = pb[3 + r]
                                pok = ('ps', 3 + r)
                                mm(S, po[:, :W], V[:, kb, vh * 128:(vh + 1) * 128], P[i4][:, :W], ki == 0, last,
                                   [('V', kb // 9), ('P', i4)], [pok])
                                q_ = ki % 2
                                eng = 'dve' if q_ == 0 else 'pool'
                                lak = ('La', r, q_)
                                if ki < 2:
                                    cp(S, eng, La[r][q_][:, :W], P[i4][:, :W], [('P', i4)], [lak])
                                else:
                                    tt(S, eng, La[r][q_][:, :W], La[r][q_][:, :W], P[i4][:, :W], ALU.add, [lak, ('P', i4)], [lak])
                                if last and r == 1:
                                    for rr, rb, rk in ((0, r0b, 'r0'), (1, r1b, 'r1')):
                                        for q2 in range(2):
                                            mm(S, pb[5][:, :W], onesf[:], La[rr][q2][:, :W], q2 == 0, q2 == 1,
                                               [('La', rr, q2), 'onesf'], [('ps', 5)])
                                        recip(S, rb[:, :W], pb[5][:, :W], [('ps', 5)], [rk])
                                    tt(S, 'dve', o0[:, :W], pb[3][:, :W], r0b[:, :W], ALU.mult, [('ps', 3), 'r0'], ['o0'])
                                    tt(S, 'dve', r1b[:, :W], pb[4][:, :W], r1b[:, :W], ALU.mult, [('ps', 4), 'r1'], ['r1'])
                                    stt(S, o0[:, :W], r1b[:, :W], nlam[:, 0:1], o0[:, :W], ALU.mult, ALU.add,
                                        ['o0', 'r1', 'nlam'], ['o0'])
                                    tt(S, 'pool', osq[:, :W], o0[:, :W], o0[:, :W], ALU.mult, ['o0'], ['osq'])
                                    mm(S, pb[6][:, :W], g.ones[:], osq[:, :W], True, True, ['osq'], [('ps', 6)])
                                    act(S, r0b[:, :W], pb[6][:, :W], AF.Sqrt, [('ps', 6)], ['r0'], bias=g.epsb[:, 0:1],
                                        scale=1.0 / 128)
                                    recip(S, r0b[:, :W], r0b[:, :W], ['r0'], ['r0'])
                                    stt(S, OT[:, vh, :W], o0[:, :W], sg[:, 0:1], r0b[:, :W], ALU.mult, ALU.mult,
                                        ['o0', 'sg', 'r0'], ['OT'])

                            steps.append((A, B))
                pipeline(steps, 2)
                hks = [('QZ', 0), ('QZ', 1)]
                dma(S, 'sp', hv[:, :, :W], hview(src)[:, :, t0:t0 + W], [], hks, "hld0")
                for m in range(8):
                    po = pb[6 + (m % 2)]
                    pok = ('ps', 6 + (m % 2))
                    for vh in range(8):
                        mm(S, po[:, :W], wout[:, vh, m * 128:(m + 1) * 128], OT[:, vh, :W], vh == 0, vh == 7,
                           ['wout', 'OT'], [pok])
                    stt(S, hv[:, m, :W], po[:, :W], MV(g, l, 2, m, s), hv[:, m, :W], ALU.mult, ALU.add,
                        [pok] + hks, hks)
                dma(S, 'sp', hview(dst)[:, :, t0:t0 + W], hv[:, :, :W], hks, [], "hst0")
            else:
                j0 = t0 // 128
                kbs = [(32, None), (33, None)] + [(kb, kb - j0 + 1) for kb in range(j0 - 1, j0 + 5) if 0 <= kb < 32]
                steps = []
                for n in range(16):
                    for ki, (kb, mi) in enumerate(kbs):
                        i2, i3 = np_ % 2, np_ % 3
                        np_ += 1
                        last = (ki == len(kbs) - 1)

                        def A(n=n, kb=kb, mi=mi, i2=i2, i3=i3, W=W, sl=sl):
                            c, r = n // 2, n % 2
                            gk = SWA_PERM[n] // 4
                            mm(S, pb[i2][:, :W], KT[:, gk // 2, kb * 128:(kb + 1) * 128],
                               qzs[sl][:, c, r, :W], True, True, [('KT', gk // 2), ('Q', sl, r)], [('ps', i2)])
                            act(S, P[i3][:, :W], pb[i2][:, :W], AF.Exp, [('ps', i2)], [('P', i3)])
                            if mi is not None:
                                tt(S, 'pool', PM[i2][:, :W], P[i3][:, :W], msk[:, mi, :W], ALU.mult, [('P', i3), 'msk'],
                                   [('PM', i2)])

                        def B(n=n, kb=kb, mi=mi, ki=ki, last=last, i2=i2, i3=i3, W=W):
                            gk = SWA_PERM[n] // 4
                            po = pb[2 + 2 * (n % 2)]
                            pl = pb[3 + 2 * (n % 2)]
                            pok, plk = ('ps', 2 + 2 * (n % 2)), ('ps', 3 + 2 * (n % 2))
                            if mi is None:
                                pu, puk = P[i3], ('P', i3)
                            else:
                                pu, puk = PM[i2], ('PM', i2)
                            mm(S, po[:64, :W], V[:, kb, gk * 64:(gk + 1) * 64], pu[:, :W], ki == 0, last,
                               [('V', kb // 9), puk], [pok])
                            mm(S, pl[:64, :W], g.ones[:, :64], pu[:, :W], ki == 0, last, [puk], [plk])
                            if last:
                                ts(S, 'dve', r0b[:64, :W], pl[:64, :W], snk[:64, n:n + 1], None, ALU.add, None,
                                   [plk, 'snk'], ['r0'])
                                recip(S, r0b[:64, :W], r0b[:64, :W], ['r0'], ['r0'])
                                tt(S, 'dve', OT[:, n, :W], po[:64, :W], r0b[:64, :W], ALU.mult, [pok, 'r0'], ['OT'])

                        steps.append((A, B))
                pipeline(steps, 1)
                for m in range(8):
                    po = pb[6 + (m % 2)]
                    pok = ('ps', 6 + (m % 2))
                    for n in range(16):
                        mm(S, po[:, :W], wout[:, n, m * 128:(m + 1) * 128], OT[:, n, :W], n == 0, n == 15,
                           ['wout', 'OT'], [pok])
                    stt(S, hb[sl][:, m, :W], po[:, :W], MV(g, l, 2, m, s), hb[sl][:, m, :W], ALU.mult, ALU.add,
                        [pok, hk], [hk])
            if kind != 'diff':
                dma(S, 'sp', hview(dst)[:, :, t0:t0 + W], hb[sl][:, :, :W], [hk], [], f"hst{sl}")
        if kind == 'swa':
            pass
        S.emit(nc, *sems)


IN_SPECS = [
    ("hT0", [D, T]), ("cT", [128, 8, 2]), ("ada_w", [4, D, 6 * D]), ("ada_bT", [128, 4, 48]),
    ("gmixT", [128, 4, 8]), ("gffnT", [128, 4, 8]), ("ident", [128, 128]),
    ("sc_in_w", [D, 3 * D]), ("sc_convT", [128, 8, 3]), ("sc_out_w", [D, D]),
    ("ffn_w1", [2, D, FF]), ("ffn_w3", [2, D, FF]), ("ffn_w2", [2, FF, D]),
    ("moe_rwT", [2, 128, 8, NE]), ("moe_rb", [2, 1, NE]),
    ("moe_w1", [2, NE, D, FF]), ("moe_w3", [2, NE, D, FF]), ("moe_w2", [2, NE, FF, D]),
    ("cm_in_w", [D, 4096]), ("cm_out_w", [2048, D]), ("cm_wsT", [128, 8, 128]), ("cm_buT", [128, 16]),
    ("cm_bv", [1, 2048]), ("cm_vgT", [128, 16]), ("cm_bsf", [1, 1024]),
    ("da_qkv_w", [D, 3072]), ("da_out_w", [D, D]), ("da_qkg", [128, 2]), ("da_lam", [1, 256]), ("da_subg", [128, 1]),
    ("sw_qkv_wp", [D, 1536]), ("sw_out_wp", [D, D]), ("sw_qkg", [128, 2]), ("sw_sinkp", [1, 16]),
    ("ropecs", [128, 2, T]), ("ropeRT", [128, 128]), ("blkones", [128, 128]), ("swa_mask", [128, 6, 512]),
]


def build(nphase):
    nc = bass.Bass("TRN2", target_bir_lowering=False)
    io = {}
    for name, shape in IN_SPECS:
        io[name] = nc.dram_tensor(name, shape, F32, kind="ExternalInput").ap()
    out = nc.dram_tensor("out", [D, T], F32, kind="ExternalOutput").ap()
    hA = nc.dram_tensor("hA", [D, T], F32, kind="Internal").ap()
    hB = nc.dram_tensor("hB", [D, T], F32, kind="Internal").ap()
    g = G()
    with ExitStack() as es:
        g.modv = es.enter_context(nc.sbuf_tensor("modv", [128, 4, 48, 2], F32))
        g.ones = es.enter_context(nc.sbuf_tensor("ones", [128, 128], BF16))
        g.ident = es.enter_context(nc.sbuf_tensor("ident_sb", [128, 128], F32))
        g.epsb = es.enter_context(nc.sbuf_tensor("epsb", [128, 1], F32))
        NSET = 3
        semsets = []
        for k in range(NSET):
            esem = {e: es.enter_context(nc.semaphore(f"es{k}_{e}")) for e in ENGS}
            dsems = [es.enter_context(nc.semaphore(f"ds{k}_{i}")) for i in range(NDSEM)]
            semsets.append((esem, dsems, dict(ebase={e: 0 for e in ENGS}, dbase=[0] * NDSEM)))
        with nc.Block() as blk:
            blk.vector(lambda e: e.memset(g.epsb[:], EPS))

        scr = dict(
            QT=nc.dram_tensor("scrQT", [D, T], BF16, kind="Internal").ap(),
            KT=nc.dram_tensor("scrKT", [D, T], BF16, kind="Internal").ap(),
            V=nc.dram_tensor("scrV", [T, D], BF16, kind="Internal").ap(),
        )
        P_ = [
            ('conv', lambda sm, a, b: phase_conv(nc, g, io, sm, a, b), True),
            ('ffn0', lambda sm, a, b: phase_ffn(nc, g, io, sm, a, b, 0, False, True), True),
            ('qkv1', lambda sm, a, b: phase_qkv(nc, g, io, sm, a, 1, 'diff', scr), False),
            ('att1', lambda sm, a, b: phase_att(nc, g, io, sm, a, b, 1, 'diff', scr), True),
            ('moe1', lambda sm, a, b: phase_ffn(nc, g, io, sm, a, b, 1, True, True), True),
            ('gmlp', lambda sm, a, b: phase_gmlp(nc, g, io, sm, a, b), True),
            ('ffn2', lambda sm, a, b: phase_ffn(nc, g, io, sm, a, b, 2, False, True), True),
            ('qkv3', lambda sm, a, b: phase_qkv(nc, g, io, sm, a, 3, 'swa', scr), False),
            ('att3', lambda sm, a, b: phase_att(nc, g, io, sm, a, b, 3, 'swa', scr), True),
            ('moe3', lambda sm, a, b: phase_ffn(nc, g, io, sm, a, b, 3, True, False), True),
        ]
        sel = os.environ.get("KPH")
        if sel is not None:
            idx = [int(x) for x in sel.split(",") if x != ""]
        else:
            idx = list(range(min(nphase, len(P_))))
        pi = 0
        phase_adaln(nc, g, io, semsets[0])
        cur = io['hT0']
        bufs = [hB, hA]
        nb_ = 0
        for ix in idx:
            pi += 1
            sm = semsets[pi % NSET]
            dstb = bufs[nb_ % 2]
            P_[ix][1](sm, cur, dstb)
            if P_[ix][2]:
                cur = dstb
                nb_ += 1
        last = cur if nb_ > 0 else None
        with nc.semaphore("fin") as fin, nc.Block() as blk:
            def _fin(e):
                if last is not None:
                    e.dma_start(out=out, in_=last).then_inc(fin, 16)
                    e.wait_ge(fin, 16)
            blk.sync(_fin)
    return nc


_NC_CACHE = {}
_CONST = {}


def _consts():
    if _CONST:
        return _CONST
    t = np.arange(SEQ)
    row = (t // 64).astype(np.float32)
    colp = (t % 64).astype(np.float32)
    inv = (np.float32(10000.0) ** (-np.arange(16, dtype=np.float32) / np.float32(16))).astype(np.float32)
    ang = np.zeros((64, SEQ), np.float32)
    for d in range(64):
        pos = row if d < 32 else colp
        ang[d] = pos * inv[d % 16]
    cos = np.ones((64, T), np.float32)
    sin = np.zeros((64, T), np.float32)
    cos[:, :SEQ] = np.cos(ang)
    sin[:, :SEQ] = np.sin(ang)
    cs = np.stack([np.concatenate([cos, cos], 0), np.concatenate([sin, sin], 0)], axis=1)
    R = np.zeros((64, 64), np.float32)
    for part in range(2):
        b = part * 32
        for i in range(16):
            R[b + i, b + i + 16] = -1.0
            R[b + i + 16, b + i] = 1.0
    RT = np.zeros((128, 128), np.float32)
    RT[:64, :64] = R.T
    RT[64:, 64:] = R.T
    bo = np.zeros((128, 128), np.float32)
    bo[:64, :64] = 1.0
    bo[64:, 64:] = 1.0
    kk = np.arange(128)[:, None, None]
    mi = np.arange(6)[None, :, None]
    qq = np.arange(512)[None, None, :]
    dlt = (mi - 1) * 128 + kk - qq
    mask = (np.abs(dlt) <= 128).astype(np.float32)
    _CONST.update(ropecs=np.ascontiguousarray(cs), ropeRT=RT, blkones=bo, swa_mask=np.ascontiguousarray(mask))
    return _CONST


def host_inputs(inputs, b):
    x = inputs['x'][b]
    ctx = inputs['ctx'][b]
    m = {}
    m['hT0'] = np.ascontiguousarray(np.concatenate([x.T, ctx.T], axis=1))
    cc = np.stack([inputs['c'][b], inputs['c_ctx']], axis=1)
    m['cT'] = np.ascontiguousarray(cc.reshape(8, 128, 2).transpose(1, 0, 2))
    m['ada_w'] = inputs['ada_w']
    m['ada_bT'] = np.ascontiguousarray(inputs['ada_b'].reshape(4, 48, 128).transpose(2, 0, 1))
    m['gmixT'] = np.ascontiguousarray(inputs['norm_mix_g'].reshape(4, 8, 128).transpose(2, 0, 1))
    m['gffnT'] = np.ascontiguousarray(inputs['norm_ffn_g'].reshape(4, 8, 128).transpose(2, 0, 1))
    m['ident'] = np.eye(128, dtype=np.float32)
    m['sc_in_w'] = inputs['sc_in_w'][0]
    m['sc_convT'] = np.ascontiguousarray(inputs['sc_conv_w'][0].reshape(3, 8, 128).transpose(2, 1, 0))
    m['sc_out_w'] = inputs['sc_out_w'][0]
    m['ffn_w1'] = inputs['ffn_w1']
    m['ffn_w3'] = inputs['ffn_w3']
    m['ffn_w2'] = inputs['ffn_w2']
    m['moe_rwT'] = np.ascontiguousarray(inputs['moe_router_w'].reshape(2, 8, 128, NE).transpose(0, 2, 1, 3))
    m['moe_rb'] = np.ascontiguousarray(inputs['moe_router_b'].reshape(2, 1, NE))
    m['moe_w1'] = inputs['moe_w1']
    m['cm_in_w'] = inputs['cm_in_w'][0]
    m['da_qkv_w'] = inputs['da_qkv_w'][0]
    m['da_out_w'] = inputs['da_out_w'][0]
    m['da_qkg'] = np.stack([np.tile(inputs['da_q_norm_g'][0], 2), np.tile(inputs['da_k_norm_g'][0], 2)], axis=1)
    m['da_lam'] = inputs['da_lambda'][0].reshape(1, 256)
    m['da_subg'] = inputs['da_sub_norm_g'][0].reshape(128, 1)
    wqkv = inputs['sw_qkv_w'][0]
    perm = np.array(SWA_PERM)
    qcols = (perm[:, None] * 64 + np.arange(64)[None, :]).reshape(-1)
    m['sw_qkv_wp'] = np.concatenate([wqkv[:, qcols], wqkv[:, 1024:]], axis=1)
    m['sw_out_wp'] = inputs['sw_out_w'][0][qcols, :]
    m['sw_qkg'] = np.stack([np.tile(inputs['sw_q_norm_g'][0], 2), np.tile(inputs['sw_k_norm_g'][0], 2)], axis=1)
    m['sw_sinkp'] = inputs['sw_sink'][0][perm].reshape(1, 16)
    m.update(_consts())
    m['cm_out_w'] = inputs['cm_out_w'][0]
    m['cm_wsT'] = np.ascontiguousarray(inputs['cm_ws'][0].transpose(2, 0, 1))
    m['cm_buT'] = np.ascontiguousarray(inputs['cm_in_b'][0][:2048].reshape(16, 128).T)
    m['cm_bv'] = np.ascontiguousarray(inputs['cm_in_b'][0][2048:].reshape(1, 2048))
    m['cm_vgT'] = np.ascontiguousarray(inputs['cm_v_norm_g'][0].reshape(16, 128).T)
    m['cm_bsf'] = np.ascontiguousarray(inputs['cm_bs'][0].reshape(1, 1024))
    m['moe_w3'] = inputs['moe_w3']
    m['moe_w2'] = inputs['moe_w2']
    return {k: np.ascontiguousarray(v, dtype=np.float32) for k, v in m.items()}


def kernel(**inputs):
    nphase = int(os.environ.get("KSTOP", "99"))
    ncores = int(os.environ.get("KCORES", "8"))
    inputs = {k: np.asarray(v) for k, v in inputs.items()}
    if nphase not in _NC_CACHE:
        _NC_CACHE[nphase] = build(nphase)
    nc = _NC_CACHE[nphase]
    in_maps = [host_inputs(inputs, b) for b in range(ncores)]
    res = run_bass_kernel_spmd(nc, in_maps, core_ids=list(range(ncores)))
    outs = [r["out"] for r in res.results]
    if os.environ.get("KRAW"):
        return outs
    full = np.stack([o[:, :SEQ].T for o in outs], axis=0)
    return np.ascontiguousarray(full.astype(np.float32))
```

```python
import os, math
from contextlib import ExitStack
import numpy as np
import concourse.bass as bass
import concourse.mybir as mybir
from concourse.bass_utils import run_bass_kernel_spmd

F32 = mybir.dt.float32
BF16 = mybir.dt.bfloat16
ALU = mybir.AluOpType
AF = mybir.ActivationFunctionType
AX = mybir.AxisListType

D = 1024
SEQ = 4096
CTXL = 256
T = SEQ + CTXL
FF = 2816
NE = 8
EPS = 1e-6
ENGS = ('pe', 'act', 'dve', 'pool', 'sp')
NDSEM = 28


class Sched:
    def __init__(self):
        self.ops = []
        self.lw = {}
        self.rd = {}

    def add(self, eng, fn, r=(), w=(), dma=False, sg=None, cost=300.0, xfer=0.0):
        deps = set()
        for k in r:
            x = self.lw.get(k)
            if x is not None:
                deps.add(x)
        for k in w:
            x = self.lw.get(k)
            if x is not None:
                deps.add(x)
            deps.update(self.rd.get(k, ()))
        i = len(self.ops)
        self.ops.append([eng, fn, deps, dma, sg, False, 0, float(cost), float(xfer)])
        for k in r:
            self.rd.setdefault(k, []).append(i)
        for k in w:
            self.lw[k] = i
            self.rd[k] = []
        return i

    def schedule(self, window=48, lat=150.0):
        ops = self.ops
        n = len(ops)
        left = [len(o[2]) for o in ops]
        users = [[] for _ in range(n)]
        for i, o in enumerate(ops):
            for d in o[2]:
                users[d].append(i)
        rdy = [0.0] * n
        queues = {e: [] for e in ENGS}
        for i, o in enumerate(ops):
            queues[o[0]].append(i)
        qpos = {e: 0 for e in ENGS}
        win = {e: [] for e in ENGS}
        etime = {e: 0.0 for e in ENGS}
        for e in ENGS:
            q = queues[e]
            while len(win[e]) < window and qpos[e] < len(q):
                win[e].append(q[qpos[e]])
                qpos[e] += 1
        order = []
        best = {e: None for e in ENGS}
        dirty = set(ENGS)
        done = 0
        while done < n:
            for e in dirty:
                b = None
                et = etime[e]
                for i in win[e]:
                    if left[i] == 0:
                        t = rdy[i] if rdy[i] > et else et
                        if b is None or t < b[0]:
                            b = (t, i)
                            if t <= et:
                                break
                best[e] = b
            dirty = set()
            pick = None
            for e in ENGS:
                b = best[e]
                if b is not None and (pick is None or b[0] < pick[0] or (b[0] == pick[0] and b[1] < pick[1])):
                    pick = (b[0], b[1], e)
            assert pick is not None, "scheduler stuck"
            t, i, e = pick
            o = ops[i]
            etime[e] = t + o[7]
            fin = etime[e] + o[8]
            order.append(i)
            done += 1
            win[e].remove(i)
            q = queues[e]
            if qpos[e] < len(q):
                win[e].append(q[qpos[e]])
                qpos[e] += 1
            dirty.add(e)
            for u in users[i]:
                ue = ops[u][0]
                r_ = fin if ue == e and not o[3] else fin + lat
                if r_ > rdy[u]:
                    rdy[u] = r_
                left[u] -= 1
                if left[u] == 0:
                    dirty.add(ue)
        newidx = {old_: new_ for new_, old_ in enumerate(order)}
        nops = []
        for old_ in order:
            o = ops[old_]
            o[2] = {newidx[d] for d in o[2]}
            nops.append(o)
        self.ops = nops

    def emit(self, nc, esem, dsems, st):
        if getattr(self, 'use_sched', True) and os.environ.get("KSCHED", "1") == "1":
            self.schedule()
        ops = self.ops
        pos = {}
        cnt = {e: 0 for e in ENGS}
        for i, o in enumerate(ops):
            pos[i] = cnt[o[0]]
            cnt[o[0]] += 1
        for o in ops:
            if o[3]:
                o[5] = True
        for i, o in enumerate(ops):
            for d in o[2]:
                y = ops[d]
                if y[3]:
                    continue
                if y[0] != o[0] or o[3]:
                    y[5] = True
                elif o[0] != 'pe' and pos[i] - pos[d] <= 2:
                    y[5] = True
        ec = st['ebase']
        sgc = {}
        sgsem = {}
        sgidx = {}
        for o in ops:
            if o[3]:
                sg = o[4]
                if sg not in sgsem:
                    assert len(sgsem) < len(dsems), "too many dma sem groups"
                    sgidx[sg] = len(sgsem)
                    sgsem[sg] = dsems[len(sgsem)]
                    sgc[sg] = st['dbase'][sgidx[sg]]
                sgc[sg] = sgc[sg] + 16
                o[6] = sgc[sg]
            elif o[5]:
                ec[o[0]] += 1
                o[6] = ec[o[0]]
        for sg, i_ in sgidx.items():
            st['dbase'][i_] = sgc[sg]
        prog = {e: [] for e in ENGS}
        seen = {e: {} for e in ENGS}
        for i, o in enumerate(ops):
            waits = {}
            for d in o[2]:
                y = ops[d]
                if y[3]:
                    key = ('d', y[4])
                    sem = sgsem[y[4]]
                else:
                    if y[0] == o[0] and not o[3]:
                        if o[0] == 'pe' or pos[i] - pos[d] > 2:
                            continue
                    key = ('e', y[0])
                    sem = esem[y[0]]
                v = y[6]
                if waits.get(key, (None, 0))[1] < v:
                    waits[key] = (sem, v)
            wl = []
            for key, (sem, v) in waits.items():
                if seen[o[0]].get(key, 0) >= v:
                    continue
                seen[o[0]][key] = v
                wl.append((sem, v))
            prog[o[0]].append((wl, o))

        def run(e, name):
            for wl, o in prog[name]:
                for sem, v in wl:
                    e.wait_ge(sem, v)
                ins = o[1](e)
                if o[3]:
                    ins.then_inc(sgsem[o[4]], 16)
                elif o[5]:
                    ins.then_inc(esem[name], 1)
            if name == 'sp':
                for sg, v in sgc.items():
                    e.wait_ge(sgsem[sg], v)

        with nc.Block() as blk:
            blk.tensor(lambda e: run(e, 'pe'))
            blk.scalar(lambda e: run(e, 'act'))
            blk.vector(lambda e: run(e, 'dve'))
            blk.gpsimd(lambda e: run(e, 'pool'))
            blk.sync(lambda e: run(e, 'sp'))


def _fsz(ap):
    n = 1
    for d in ap.shape[1:]:
        n *= int(d)
    return n


def _cost(eng, ap):
    n = _fsz(ap)
    if eng == 'dve':
        return 70.0 + n / 0.75
    if eng == 'pool':
        return 100.0 + n / 0.485
    return 110.0 + n / 0.96


def mm(S, out, lhsT, rhs, start, stop, r, w):
    c = max(64.0, _fsz(out) / 2.4 + 45.0)
    if lhsT.dtype == F32:
        c *= 4.0
    S.add('pe', lambda e: e.matmul(out, lhsT, rhs, start=start, stop=stop), r, w, cost=c)


def act(S, out, in_, func, r, w, bias=None, scale=None, accum=None):
    kw = {}
    if bias is not None:
        kw['bias'] = bias
    if scale is not None:
        kw['scale'] = scale
    if accum is not None:
        kw['accum_out'] = accum
    S.add('act', lambda e: e.activation(out=out, in_=in_, func=func, **kw), r, w, cost=_cost('act', out))


def tt(S, eng, out, in0, in1, op, r, w):
    S.add(eng, lambda e: e.tensor_tensor(out=out, in0=in0, in1=in1, op=op), r, w, cost=_cost(eng, out))


def ts(S, eng, out, in0, s1, s2, op0, op1, r, w):
    if s2 is None:
        S.add(eng, lambda e: e.tensor_scalar(out=out, in0=in0, scalar1=s1, scalar2=None, op0=op0), r, w, cost=_cost(eng, out))
    else:
        S.add(eng, lambda e: e.tensor_scalar(out=out, in0=in0, scalar1=s1, scalar2=s2, op0=op0, op1=op1), r, w, cost=_cost(eng, out))


def stt(S, out, in0, scalar, in1, op0, op1, r, w):
    S.add('dve', lambda e: e.scalar_tensor_tensor(out=out, in0=in0, scalar=scalar, in1=in1, op0=op0, op1=op1), r, w, cost=_cost('dve', out))


def cp(S, eng, out, in_, r, w):
    S.add(eng, lambda e: e.tensor_copy(out=out, in_=in_), r, w, cost=_cost(eng, out))


def recip(S, out, in_, r, w):
    S.add('dve', lambda e: e.reciprocal(out=out, in_=in_), r, w, cost=_cost('dve', out))


def mset(S, eng, ap, val, r, w):
    S.add(eng, lambda e: e.memset(ap, val), r, w, cost=_cost(eng, ap))


def dma(S, q, out, in_, r, w, sg):
    nb_ = int(out.shape[0]) * _fsz(out) * 4
    S.add(q, lambda e: e.dma_start(out=out, in_=in_), r, w, dma=True, sg=sg,
          cost=(1000.0 if q == 'pool' else 60.0), xfer=2000.0 + nb_ / 150.0)


def pipeline(steps, depth=1):
    pend = []
    for A, B in steps:
        A()
        pend.append(B)
        if len(pend) > depth:
            pend.pop(0)()
    for B in pend:
        B()


def hview(ap):
    return ap.rearrange("(c p) t -> p c t", p=128)


def wview(ap):
    return ap.rearrange("(kc p) f -> p kc f", p=128)


class G:
    pass


_UID = [0]


def _un(name):
    _UID[0] += 1
    return f"{name}_{_UID[0]}"


class _NCW:
    def __init__(self, nc):
        self._nc = nc

    def sbuf_tensor(self, name, shape, dt):
        return self._nc.sbuf_tensor(_un(name), shape, dt)

    def psum_tensor(self, name, shape, dt):
        return self._nc.psum_tensor(_un(name), shape, dt)

    def __getattr__(self, k):
        return getattr(self._nc, k)


def MV(g, l, which, c, s):
    return g.modv[:, l, which * 8 + c, s:s + 1]


def phase_adaln(nc, g, io, sems):
    nc = _NCW(nc)
    S = Sched()
    with ExitStack() as es:
        wb = [es.enter_context(nc.sbuf_tensor(f"adw{i}", [128, 8, 3072], BF16)) for i in range(2)]
        cT = es.enter_context(nc.sbuf_tensor("cT", [128, 8, 2], F32))
        sT = es.enter_context(nc.sbuf_tensor("sT", [128, 8, 2], BF16))
        bT = es.enter_context(nc.sbuf_tensor("bT", [128, 4, 48], F32))
        gm = es.enter_context(nc.sbuf_tensor("gm", [128, 4, 8], F32))
        gf = es.enter_context(nc.sbuf_tensor("gf", [128, 4, 8], F32))
        ps = es.enter_context(nc.psum_tensor("ps_ada", [128, 512], F32))
        mset(S, 'pool', g.ones[:], 1.0, [], ['ones'])
        dma(S, 'sp', g.ident[:], io['ident'], [], ['ident'], 'c0')
        dma(S, 'sp', cT[:], io['cT'], [], ['cT'], 'c1')
        dma(S, 'sp', bT[:], io['ada_bT'], [], ['bT'], 'c2')
        dma(S, 'sp', gm[:], io['gmixT'], [], ['gm'], 'c3')
        dma(S, 'sp', gf[:], io['gffnT'], [], ['gf'], 'c4')
        act(S, sT[:], cT[:], AF.Silu, ['cT'], ['sT'])
        n = 0
        for l in range(4):
            for half in range(2):
                slot = n % 2
                n += 1
                for q in range(2):
                    c0 = half * 3072 + q * 1536
                    dma(S, 'pool', wb[slot][:, :, q * 1536:(q + 1) * 1536], wview(io['ada_w'][l])[:, :, c0:c0 + 1536],
                        [], [('adw', slot, q)], f"adw{slot}_{q}")
                for jj in range(24):
                    j = half * 24 + jj
                    q = (jj * 128) // 1536
                    col = (l * 48 + j) * 2
                    for kc in range(8):
                        mm(S, ps[:, col:col + 2], wb[slot][:, kc, jj * 128:(jj + 1) * 128], sT[:, kc, :],
                           kc == 0, kc == 7, [('adw', slot, q), 'sT'], ['psada'])
        for l in range(4):
            tt(S, 'dve', g.modv[:, l], ps[:, l * 96:(l + 1) * 96].rearrange("p (j s) -> p j s", s=2),
               bT[:, l, :].unsqueeze(2).to_broadcast([128, 48, 2]), ALU.add, ['psada', 'bT'], ['modv'])
            stt(S, g.modv[:, l, 8:16, :], g.modv[:, l, 8:16, :], 1.0,
                gm[:, l, :].unsqueeze(2).to_broadcast([128, 8, 2]), ALU.add, ALU.mult, ['modv', 'gm'], ['modv'])
            stt(S, g.modv[:, l, 32:40, :], g.modv[:, l, 32:40, :], 1.0,
                gf[:, l, :].unsqueeze(2).to_broadcast([128, 8, 2]), ALU.add, ALU.mult, ['modv', 'gf'], ['modv'])
        S.emit(nc, *sems)


def prenorm(S, g, src_cols, hb, hk, ab, ak, tmp, l, which, s, n, sg, a32=None, a32k=None):
    sq = tmp['sq'][:, :, :n]
    xn = tmp['xn'][:, :, :n]
    rs = tmp['rs'][:, :n]
    ps = tmp['ps'][:, :n]
    psk = tmp['psk']
    sqk = tmp.get('sqk', 'sq')
    hks = hk if isinstance(hk, list) else [hk]
    dma(S, 'sp', hb, src_cols, [], hks, sg)
    tt(S, 'pool', sq, hb, hb, ALU.mult, hks, [sqk])
    for c in range(8):
        mm(S, ps, g.ones[:], sq[:, c, :], c == 0, c == 7, [sqk, 'ones'], [psk])
    act(S, rs, ps, AF.Sqrt, [psk], ['rs'], bias=g.epsb[:, 0:1], scale=1.0 / 1024)
    recip(S, rs, rs, ['rs'], ['rs'])
    tt(S, 'dve', xn, hb, rs.unsqueeze(1).to_broadcast([128, 8, n]), ALU.mult, hks + ['rs'], ['xn'])
    for c in range(8):
        if a32 is None:
            act(S, ab[:, c, :], xn[:, c, :], AF.Identity, ['xn'], [ak],
                scale=MV(g, l, which * 3 + 1, c, s), bias=MV(g, l, which * 3, c, s))
        else:
            act(S, a32[:, c, :], xn[:, c, :], AF.Identity, ['xn'], [a32k],
                scale=MV(g, l, which * 3 + 1, c, s), bias=MV(g, l, which * 3, c, s))
            cp(S, 'pool', ab[:, c, :], a32[:, c, :], [a32k], [ak])


def phase_conv(nc, g, io, sems, src, dst, l=0):
    nc = _NCW(nc)
    S = Sched()
    W = 256
    with ExitStack() as es:
        win = es.enter_context(nc.sbuf_tensor("scwin", [128, 8, 3072], BF16))
        wout = es.enter_context(nc.sbuf_tensor("scwout", [128, 8, 1024], BF16))
        cw = es.enter_context(nc.sbuf_tensor("sccw", [128, 8, 3], F32))
        hb = [es.enter_context(nc.sbuf_tensor(f"sch{i}", [128, 8, W + 2], F32)) for i in range(2)]
        ab = [es.enter_context(nc.sbuf_tensor(f"sca{i}", [128, 8, W + 2], BF16)) for i in range(2)]
        sq = es.enter_context(nc.sbuf_tensor("scsq", [128, 8, W + 2], BF16))
        xn = es.enter_context(nc.sbuf_tensor("scxn", [128, 8, W + 2], F32))
        rs = es.enter_context(nc.sbuf_tensor("scrs", [128, W + 2], F32))
        xv = [es.enter_context(nc.sbuf_tensor(f"scxv{i}", [128, W + 2], F32)) for i in range(2)]
        yb = [es.enter_context(nc.sbuf_tensor(f"scy{i}", [128, W + 2], F32)) for i in range(2)]
        cv = [es.enter_context(nc.sbuf_tensor(f"sccv{i}", [128, W], F32)) for i in range(2)]
        ub = [es.enter_context(nc.sbuf_tensor(f"scu{i}", [128, 8, W], BF16)) for i in range(2)]
        pb = [es.enter_context(nc.psum_tensor(f"scp{i}", [128, 512], F32)) for i in range(8)]
        for q in range(2):
            dma(S, 'pool', win[:, :, q * 1536:(q + 1) * 1536], wview(io['sc_in_w'])[:, :, q * 1536:(q + 1) * 1536],
                [], [('win', q)], f"win{q}")
        dma(S, 'pool', wout[:], wview(io['sc_out_w']), [], ['wout'], "wout")
        dma(S, 'sp', cw[:], io['sc_convT'], [], ['cw'], "cw")
        tiles = [(i * W, 0, 0, SEQ) for i in range(SEQ // W)] + [(SEQ, 1, SEQ, T)]
        for ti, (t0, s, slo, shi) in enumerate(tiles):
            sl = ti % 2
            lo = max(t0 - 1, slo)
            hi = min(t0 + W + 1, shi)
            off = lo - (t0 - 1)
            n = hi - lo
            hk = ('h', sl)
            ak = ('a', sl)
            tmp = dict(sq=sq, xn=xn, rs=rs, ps=pb[7], psk=('ps', 7))
            prenorm(S, g, hview(src)[:, :, lo:hi], hb[sl][:, :, off:off + n], hk, ab[sl][:, :, off:off + n], ak,
                    tmp, l, 0, s, n, f"hld{sl}")
            uk = ('u', sl)
            for c in range(8):
                ps3 = [pb[(c % 2) * 3 + i] for i in range(3)]
                pk = [('ps', (c % 2) * 3 + i) for i in range(3)]
                for i in range(3):
                    col = i * 1024 + c * 128
                    q = col // 1536
                    for kc in range(8):
                        mm(S, ps3[i][:, off:off + n], win[:, kc, col:col + 128], ab[sl][:, kc, off:off + n],
                           kc == 0, kc == 7, [('win', q), ak], [pk[i]])
                ys = c % 2
                yk = ('y', ys)
                act(S, xv[ys][:, off:off + n], ps3[2][:, off:off + n], AF.Copy, [pk[2]], [('xv', ys)])
                tt(S, 'dve', yb[ys][:, off:off + n], ps3[1][:, off:off + n], xv[ys][:, off:off + n], ALU.mult,
                   [pk[1], ('xv', ys)], [yk])
                if off == 1:
                    mset(S, 'pool', yb[ys][:, 0:1], 0.0, [], [yk])
                if off + n < W + 2:
                    mset(S, 'pool', yb[ys][:, W + 1:W + 2], 0.0, [], [yk])
                ck = ('cv', ys)
                act(S, cv[ys][:], yb[ys][:, 0:W], AF.Identity, [yk], [ck], scale=cw[:, c, 0:1])
                stt(S, cv[ys][:], yb[ys][:, 1:W + 1], cw[:, c, 1:2], cv[ys][:], ALU.mult, ALU.add, [yk, ck, 'cw'], [ck])
                stt(S, cv[ys][:], yb[ys][:, 2:W + 2], cw[:, c, 2:3], cv[ys][:], ALU.mult, ALU.add, [yk, ck, 'cw'], [ck])
                tt(S, 'dve', ub[sl][:, c, :], ps3[0][:, 1:W + 1], cv[ys][:], ALU.mult, [pk[0], ck], [uk])
            for m in range(8):
                po = pb[6 + (m % 2)]
                pok = ('ps', 6 + (m % 2))
                for c in range(8):
                    mm(S, po[:, :W], wout[:, c, m * 128:(m + 1) * 128], ub[sl][:, c, :], c == 0, c == 7,
                       ['wout', uk], [pok])
                stt(S, hb[sl][:, m, 1:W + 1], po[:, :W], MV(g, l, 2, m, s), hb[sl][:, m, 1:W + 1], ALU.mult, ALU.add,
                    [pok, hk], [hk])
            dma(S, 'sp', hview(dst)[:, :, t0:t0 + W], hb[sl][:, :, 1:W + 1], [hk], [], f"hst{sl}")
        S.emit(nc, *sems)


def phase_ffn(nc, g, io, sems, src, dst, l, moe, with_ctx):
    nc = _NCW(nc)
    S = Sched()
    S.use_sched = False
    f = l // 2
    E = NE if moe else 1
    NG = int(os.environ.get('KNG', FF // 256))
    MAXW = 1280 if with_ctx else 1536
    NS = 4
    with ExitStack() as es:
        hS = es.enter_context(nc.sbuf_tensor("fh", [128, 8, MAXW], F32))
        aS = es.enter_context(nc.sbuf_tensor("fa", [128, 8, MAXW], BF16))
        w1r = [es.enter_context(nc.sbuf_tensor(f"fw1_{i}", [128, 8, 256], BF16)) for i in range(NS)]
        w3r = [es.enter_context(nc.sbuf_tensor(f"fw3_{i}", [128, 8, 256], BF16)) for i in range(NS)]
        w2r = [es.enter_context(nc.sbuf_tensor(f"fw2_{i}", [128, 2, 1024], BF16)) for i in range(NS)]
        sq = es.enter_context(nc.sbuf_tensor("fsq", [128, 8, 512], BF16))
        xn = es.enter_context(nc.sbuf_tensor("fxn", [128, 8, 512], F32))
        rs = es.enter_context(nc.sbuf_tensor("frs", [128, 512], F32))
        s1 = [es.enter_context(nc.sbuf_tensor(f"fs1_{i}", [128, 512], BF16)) for i in range(2)]
        h1 = [es.enter_context(nc.sbuf_tensor(f"fh1_{i}", [128, 2, 512], BF16)) for i in range(2)]
        pb = [es.enter_context(nc.psum_tensor(f"fp{i}", [128, 512], F32)) for i in range(8)]
        if moe:
            gB = es.enter_context(nc.sbuf_tensor("fgB", [128, NE, MAXW], BF16))
            p3g = [es.enter_context(nc.sbuf_tensor(f"fp3g_{i}", [128, 512], BF16)) for i in range(2)]
            yt = [es.enter_context(nc.sbuf_tensor(f"fyt_{i}", [128, 512], F32)) for i in range(2)]
            wr = es.enter_context(nc.sbuf_tensor("fwr", [128, 8, NE], F32))
            brt = es.enter_context(nc.sbuf_tensor("fbr", [128, NE], F32))
            lg = es.enter_context(nc.sbuf_tensor("flg", [128, 4, NE], F32))
            mx = es.enter_context(nc.sbuf_tensor("fmx", [128, 4, 8], F32))
            nt1 = es.enter_context(nc.sbuf_tensor("fnt1", [128, 4], F32))
            msk = es.enter_context(nc.sbuf_tensor("fmsk", [128, 4, NE], F32))
            ex = es.enter_context(nc.sbuf_tensor("fex", [128, 4, NE], F32))
            den = es.enter_context(nc.sbuf_tensor("fden", [128, 4], F32))
            gt = es.enter_context(nc.sbuf_tensor("fgt", [128, 4, NE], F32))
            gbc = es.enter_context(nc.sbuf_tensor("fgbc", [128, 4, NE, 128], F32))
            dma(S, 'sp', wr[:], io['moe_rwT'][f], [], ['wr'], "wr")
            dma(S, 'sp', brt[:], io['moe_rb'][f].partition_broadcast(128), [], ['br'], "br")
            W1 = io['moe_w1'][f]
            W3 = io['moe_w3'][f]
            W2 = io['moe_w2'][f]
        else:
            W1 = [io['ffn_w1'][f]]
            W3 = [io['ffn_w3'][f]]
            W2 = [io['ffn_w2'][f]]
        lat = [(i * 512, 512, 0) for i in range(8)]
        if with_ctx:
            supers = [[lat[2 * i], lat[2 * i + 1]] for i in range(4)]
            supers[3].append((SEQ, CTXL, 1))
        else:
            supers = [[lat[0], lat[1], lat[2]], [lat[3], lat[4], lat[5]], [lat[6], lat[7]]]
        supers = supers[:int(os.environ.get('KSUP', 4))]
        nld = 0
        it = 0
        PF = 2
        gseq = [(si, e_, gi) for si in range(len(supers)) for e_ in range(E) for gi in range(NG)]
        gpos = {k: i for i, k in enumerate(gseq)}
        issued = [0]

        def issue_upto(n):
            while issued[0] < min(n, len(gseq)):
                _, ee, gg = gseq[issued[0]]
                sl_ = issued[0] % NS
                jj0 = gg * 256
                dma(S, 'pool', w1r[sl_][:], wview(W1[ee])[:, :, jj0:jj0 + 256], [], [('w1', sl_)], f"w1_{sl_}")
                dma(S, 'pool', w3r[sl_][:], wview(W3[ee])[:, :, jj0:jj0 + 256], [], [('w3', sl_)], f"w3_{sl_}")
                dma(S, 'pool', w2r[sl_][:], W2[ee][jj0:jj0 + 256, :].rearrange("(j p) m -> p j m", p=128), [],
                    [('w2', sl_)], f"w2_{sl_}")
                issued[0] += 1

        issue_upto(PF)
        for si_, st in enumerate(supers):
            offs = []
            o_ = 0
            for (t0, W, s) in st:
                offs.append(o_)
                o_ += W
            for (t0, W, s), off in zip(st, offs):
                hk = [('h', off, m_) for m_ in range(8)]
                ak = ('a', off)
                tmp = dict(sq=sq, xn=xn, rs=rs, ps=pb[7], psk=('ps', 7))
                if not moe:
                    prenorm(S, g, hview(src)[:, :, t0:t0 + W], hS[:, :, off:off + W], hk, aS[:, :, off:off + W], ak,
                            tmp, l, 1, s, W, f"hld{off}")
                else:
                    prenorm(S, g, hview(src)[:, :, t0:t0 + W], hS[:, :, off:off + W], hk, aS[:, :, off:off + W], ak,
                            tmp, l, 1, s, W, f"hld{off}", a32=xn[:, :, :W], a32k='xn')
                    nb = W // 128
                    for b in range(nb):
                        for kc in range(8):
                            mm(S, pb[6][:, b * 8:(b + 1) * 8], xn[:, kc, b * 128:(b + 1) * 128], wr[:, kc, :],
                               kc == 0, kc == 7, ['xn', 'wr'], [('ps', 6)])
                    tt(S, 'dve', lg[:, :nb, :], pb[6][:, :nb * 8].rearrange("p (b e) -> p b e", e=8),
                       brt[:].unsqueeze(1).to_broadcast([128, nb, NE]), ALU.add, [('ps', 6), 'br'], ['lg'])
                    for b in range(nb):
                        S.add('dve', (lambda o_, i_: (lambda e: e.max(out=o_, in_=i_)))(mx[:, b, :], lg[:, b, :]),
                              ['lg'], ['mx'])
                    ts(S, 'dve', nt1[:, :nb], mx[:, :nb, 0], -1.0, None, ALU.mult, None, ['mx'], ['nt1'])
                    for b in range(nb):
                        ts(S, 'dve', msk[:, b, :], lg[:, b, :], mx[:, b, 1:2], None, ALU.is_ge, None, ['lg', 'mx'], ['msk'])
                        act(S, ex[:, b, :], lg[:, b, :], AF.Exp, ['lg', 'nt1'], ['ex'], bias=nt1[:, b:b + 1], scale=1.0)
                    tt(S, 'dve', ex[:, :nb, :], ex[:, :nb, :], msk[:, :nb, :], ALU.mult, ['ex', 'msk'], ['ex'])
                    S.add('dve', (lambda o_, i_: (lambda e: e.tensor_reduce(out=o_, in_=i_, axis=AX.X, op=ALU.add)))(
                        den[:, :nb], ex[:, :nb, :]), ['ex'], ['den'])
                    recip(S, den[:, :nb], den[:, :nb], ['den'], ['den'])
                    tt(S, 'dve', gt[:, :nb, :], ex[:, :nb, :], den[:, :nb].unsqueeze(2).to_broadcast([128, nb, NE]),
                       ALU.mult, ['ex', 'den'], ['gt'])
                    cp(S, 'pool', gbc[:, :nb], gt[:, :nb, :].unsqueeze(3).to_broadcast([128, nb, NE, 128]), ['gt'], ['gbc'])
                    for e_ in range(NE):
                        pg = pb[4 + (e_ % 2)]
                        pgk = ('ps', 4 + (e_ % 2))
                        for b in range(nb):
                            mm(S, pg[:, b * 128:(b + 1) * 128], gbc[:, b, e_, :], g.ident[:], True, True,
                               ['gbc', 'ident'], [pgk])
                        act(S, gB[:, e_, off:off + W], pg[:, :W], AF.Copy, [pgk], [('gB', off)])
            steps = []
            for e_ in range(E):
                for gi in range(NG):
                    gidx = gpos[(si_, e_, gi)]
                    slot = gidx % NS
                    j0 = gi * 256
                    first = True
                    for (t0, W, s), off in zip(st, offs):
                        hs = it % 2
                        it += 1

                        def A(e_=e_, slot=slot, j0=j0, first=first, W=W, s=s, off=off, hs=hs, gidx=gidx):
                            if first:
                                issue_upto(gidx + PF + 1)
                            ak = ('a', off)
                            h1k = ('h1', hs)
                            for jj in range(2):
                                ss = jj
                                p1 = pb[jj * 2]
                                p3 = pb[jj * 2 + 1]
                                p1k = ('ps', jj * 2)
                                p3k = ('ps', jj * 2 + 1)
                                for kc in range(8):
                                    mm(S, p1[:, :W], w1r[slot][:, kc, jj * 128:(jj + 1) * 128], aS[:, kc, off:off + W],
                                       kc == 0, kc == 7, [('w1', slot), ak], [p1k])
                                for kc in range(8):
                                    mm(S, p3[:, :W], w3r[slot][:, kc, jj * 128:(jj + 1) * 128], aS[:, kc, off:off + W],
                                       kc == 0, kc == 7, [('w3', slot), ak], [p3k])
                                act(S, s1[ss][:, :W], p1[:, :W], AF.Silu, [p1k], [('s1', ss)])
                                if moe:
                                    tt(S, 'dve', p3g[ss][:, :W], p3[:, :W], gB[:, e_, off:off + W], ALU.mult,
                                       [p3k, ('gB', off)], [('p3g', ss)])
                                    tt(S, 'pool', h1[hs][:, jj, :W], s1[ss][:, :W], p3g[ss][:, :W], ALU.mult,
                                       [('s1', ss), ('p3g', ss)], [h1k])
                                else:
                                    tt(S, 'dve', h1[hs][:, jj, :W], p3[:, :W], s1[ss][:, :W], ALU.mult,
                                       [p3k, ('s1', ss)], [h1k])

                        def B(slot=slot, W=W, s=s, off=off, hs=hs):
                            h1k = ('h1', hs)
                            for m in range(8):
                                po = pb[4 + (m % 4)]
                                pok = ('ps', 4 + (m % 4))
                                for jj in range(2):
                                    mm(S, po[:, :W], w2r[slot][:, jj, m * 128:(m + 1) * 128], h1[hs][:, jj, :W],
                                       jj == 0, jj == 1, [('w2', slot), h1k], [pok])
                                hk = ('h', off, m)
                                if m % 2 == 0 or not moe:
                                    stt(S, hS[:, m, off:off + W], po[:, :W], MV(g, l, 5, m, s), hS[:, m, off:off + W],
                                        ALU.mult, ALU.add, [pok, hk], [hk])
                                else:
                                    ys = (m // 2) % 2
                                    act(S, yt[ys][:, :W], po[:, :W], AF.Identity, [pok], [('yt', ys)], scale=MV(g, l, 5, m, s))
                                    tt(S, 'pool', hS[:, m, off:off + W], hS[:, m, off:off + W], yt[ys][:, :W], ALU.add,
                                       [('yt', ys), hk], [hk])

                        steps.append((A, B))
                        first = False
            pipeline(steps, 1)
            for (t0, W, s), off in zip(st, offs):
                dma(S, 'sp', hview(dst)[:, :, t0:t0 + W], hS[:, :, off:off + W], [('h', off, m_) for m_ in range(8)], [], f"hst{off}")
        S.emit(nc, *sems)


def phase_gmlp(nc, g, io, sems, src, dst, l=2):
    nc = _NCW(nc)
    S = Sched()
    S.use_sched = False
    with ExitStack() as es:
        win = es.enter_context(nc.sbuf_tensor("cmwin", [128, 8, 4096], BF16))
        wout = es.enter_context(nc.sbuf_tensor("cmwout", [128, 16, 1024], BF16))
        wsT = es.enter_context(nc.sbuf_tensor("cmws", [128, 8, 128], BF16))
        bU = es.enter_context(nc.sbuf_tensor("cmbu", [128, 16], F32))
        bV = es.enter_context(nc.sbuf_tensor("cmbv", [128, 2048], F32))
        vg = es.enter_context(nc.sbuf_tensor("cmvg", [128, 16], F32))
        bsT = es.enter_context(nc.sbuf_tensor("cmbs", [128, 8, 128], F32))
        hb = [es.enter_context(nc.sbuf_tensor(f"cmh{i}", [128, 8, 512], F32)) for i in range(1)]
        ab = [es.enter_context(nc.sbuf_tensor(f"cma{i}", [128, 8, 512], BF16)) for i in range(1)]
        xn = es.enter_context(nc.sbuf_tensor("cmxn", [128, 8, 512], F32))
        rs = es.enter_context(nc.sbuf_tensor("cmrs", [128, 512], F32))
        vt = [es.enter_context(nc.sbuf_tensor(f"cmvt{i}", [128, 512], F32)) for i in range(2)]
        gv = es.enter_context(nc.sbuf_tensor("cmgv", [128, 2048], F32))
        junk = es.enter_context(nc.sbuf_tensor("cmjunk", [128, 512], BF16))
        ss = es.enter_context(nc.sbuf_tensor("cmss", [128, 4], F32))
        rv = es.enter_context(nc.sbuf_tensor("cmrv", [128, 1], F32))
        vn = [es.enter_context(nc.sbuf_tensor(f"cmvn{i}", [128, 2048], BF16)) for i in range(4)]
        ub = [es.enter_context(nc.sbuf_tensor(f"cmu{i}", [128, 512], F32)) for i in range(2)]
        m1 = [es.enter_context(nc.sbuf_tensor(f"cmm{i}", [128, 512], F32)) for i in range(2)]
        pr = es.enter_context(nc.sbuf_tensor("cmpr", [128, 16, 512], BF16))
        pb = [es.enter_context(nc.psum_tensor(f"cmp{i}", [128, 512], F32)) for i in range(8)]
        for q in range(4):
            dma(S, 'pool', win[:, :, q * 1024:(q + 1) * 1024], wview(io['cm_in_w'])[:, :, q * 1024:(q + 1) * 1024],
                [], [('win', q)], f"win{q}")
        dma(S, 'pool', wout[:], io['cm_out_w'].rearrange("(c p) m -> p c m", p=128), [], ['wout'], "wout")
        dma(S, 'pool', wsT[:], io['cm_wsT'], [], ['wsT'], "wsT")
        dma(S, 'sp', bU[:], io['cm_buT'], [], ['bU'], "bU")
        dma(S, 'sp', bV[:], io['cm_bv'].partition_broadcast(128), [], ['bV'], "bV")
        dma(S, 'sp', vg[:], io['cm_vgT'], [], ['vg'], "vg")
        dma(S, 'sp', bsT[:], io['cm_bsf'].partition_broadcast(128).rearrange("p o (g q) -> p (o g) q", g=8), [], ['bsT'], "bsT")
        tiles = [(i * 512, 512, 0) for i in range(8)] + [(SEQ, CTXL, 1)]
        nv = 0
        nu = 0
        for ti, (t0, W, s) in enumerate(tiles):
            sl = 0
            hk = ('h', sl)
            ak = ('a', sl)
            tmp = dict(sq=pr[:, 0:8, :], sqk='pr', xn=xn, rs=rs, ps=pb[7], psk=('ps', 7))
            prenorm(S, g, hview(src)[:, :, t0:t0 + W], hb[sl][:, :, :W], hk, ab[sl][:, :, :W], ak, tmp, l, 0, s, W,
                    f"hld{sl}")
            nb = W // 128
            for b in range(nb):
                for q4 in range(4):
                    pv = pb[nv % 2]
                    pvk = ('ps', nv % 2)
                    vts = nv % 2
                    nv += 1
                    col = 2048 + q4 * 512
                    for kc in range(8):
                        mm(S, pv[:], ab[sl][:, kc, b * 128:(b + 1) * 128], win[:, kc, col:col + 512], kc == 0, kc == 7,
                           [ak, ('win', col // 1024)], [pvk])
                    tt(S, 'dve', vt[vts][:], pv[:], bV[:, q4 * 512:(q4 + 1) * 512], ALU.add, [pvk, 'bV'], [('vt', vts)])
                    act(S, gv[:, q4 * 512:(q4 + 1) * 512], vt[vts][:], AF.Gelu_apprx_tanh, [('vt', vts)], ['gv'])
                    act(S, junk[:], gv[:, q4 * 512:(q4 + 1) * 512], AF.Square, ['gv'], ['junk', 'ss'],
                        accum=ss[:, q4:q4 + 1])
                S.add('dve', (lambda o_, i_: (lambda e: e.tensor_reduce(out=o_, in_=i_, axis=AX.X, op=ALU.add)))(
                    rv[:], ss[:]), ['ss'], ['rv'])
                act(S, rv[:], rv[:], AF.Sqrt, ['rv'], ['rv'], bias=g.epsb[:, 0:1], scale=1.0 / 2048)
                recip(S, rv[:], rv[:], ['rv'], ['rv'])
                ts(S, 'dve', vn[b][:], gv[:], rv[:, 0:1], None, ALU.mult, None, ['gv', 'rv'], [('vn', b)])
            for cu in range(16):
                pu = pb[2 + (nu % 2)]
                puk = ('ps', 2 + (nu % 2))
                psv = pb[4 + (nu % 2)]
                psk = ('ps', 4 + (nu % 2))
                us = nu % 2
                nu += 1
                gq = cu // 2
                for kc in range(8):
                    mm(S, pu[:, :W], win[:, kc, cu * 128:(cu + 1) * 128], ab[sl][:, kc, :W], kc == 0, kc == 7,
                       [ak, ('win', (cu * 128) // 1024)], [puk])
                act(S, ub[us][:, :W], pu[:, :W], AF.Gelu_apprx_tanh, [puk, 'bU'], [('ub', us)], bias=bU[:, cu:cu + 1])
                for b in range(nb):
                    mm(S, psv[:, b * 128:(b + 1) * 128], vn[b][:, cu * 128:(cu + 1) * 128], wsT[:, gq, :], True, True,
                       [('vn', b), 'wsT'], [psk])
                stt(S, m1[us][:, :W].rearrange("p (b q) -> p b q", q=128),
                    psv[:, :W].rearrange("p (b q) -> p b q", q=128), vg[:, cu:cu + 1],
                    bsT[:, gq, :].unsqueeze(1).to_broadcast([128, nb, 128]), ALU.mult, ALU.add,
                    [psk, 'vg', 'bsT'], [('m1', us)])
                tt(S, 'pool', pr[:, cu, :W], ub[us][:, :W], m1[us][:, :W], ALU.mult, [('ub', us), ('m1', us)], ['pr'])
            for m in range(8):
                po = pb[6 + (m % 2)]
                pok = ('ps', 6 + (m % 2))
                for cu in range(16):
                    mm(S, po[:, :W], wout[:, cu, m * 128:(m + 1) * 128], pr[:, cu, :W], cu == 0, cu == 15,
                       ['wout', 'pr'], [pok])
                stt(S, hb[sl][:, m, :W], po[:, :W], MV(g, l, 2, m, s), hb[sl][:, m, :W], ALU.mult, ALU.add,
                    [pok, hk], [hk])
            dma(S, 'sp', hview(dst)[:, :, t0:t0 + W], hb[sl][:, :, :W], [hk], [], f"hst{sl}")
        S.emit(nc, *sems)


SWA_PERM = [0, 4, 1, 5, 2, 6, 3, 7, 8, 12, 9, 13, 10, 14, 11, 15]


def phase_qkv(nc, g, io, sems, src, l, kind, scr):
    nc = _NCW(nc)
    S = Sched()
    if kind == 'diff':
        wq, NQC, NKC, VF = io['da_qkv_w'], 8, 8, 1024
        gname = 'da_qkg'
    else:
        wq, NQC, NKC, VF = io['sw_qkv_wp'], 8, 2, 256
        gname = 'sw_qkg'
    NC_ = NQC + NKC
    WCOLS = NC_ * 128 + VF
    with ExitStack() as es:
        win = es.enter_context(nc.sbuf_tensor("qw", [128, 8, WCOLS], BF16))
        gq = es.enter_context(nc.sbuf_tensor("qg", [128, 2], F32))
        RT = es.enter_context(nc.sbuf_tensor("qRT", [128, 128], BF16))
        bones = es.enter_context(nc.sbuf_tensor("qbo", [128, 128], BF16))
        cs = [es.enter_context(nc.sbuf_tensor(f"qcs{i}", [128, 2, 512], F32)) for i in range(2)]
        hb = [es.enter_context(nc.sbuf_tensor(f"qh{i}", [128, 8, 512], F32)) for i in range(2)]
        ab = [es.enter_context(nc.sbuf_tensor(f"qa{i}", [128, 8, 512], BF16)) for i in range(2)]
        sq = es.enter_context(nc.sbuf_tensor("qsq", [128, 8, 512], BF16))
        xn = es.enter_context(nc.sbuf_tensor("qxn", [128, 8, 512], F32))
        rs = es.enter_context(nc.sbuf_tensor("qrs", [128, 512], F32))
        xg = [es.enter_context(nc.sbuf_tensor(f"qxg{i}", [128, 512], BF16)) for i in range(2)]
        xs = [es.enter_context(nc.sbuf_tensor(f"qxs{i}", [128, 512], BF16)) for i in range(2)]
        rd = [es.enter_context(nc.sbuf_tensor(f"qrd{i}", [128, 512], F32)) for i in range(2)]
        t1 = [es.enter_context(nc.sbuf_tensor(f"qt1{i}", [128, 512], F32)) for i in range(2)]
        t2 = [es.enter_context(nc.sbuf_tensor(f"qt2{i}", [128, 512], F32)) for i in range(2)]
        ob = [es.enter_context(nc.sbuf_tensor(f"qo{i}", [128, 512], BF16)) for i in range(3)]
        vb = [es.enter_context(nc.sbuf_tensor(f"qv{i}", [128, 512], BF16)) for i in range(2)]
        pb = [es.enter_context(nc.psum_tensor(f"qp{i}", [128, 512], F32)) for i in range(8)]
        nq = (WCOLS + 1023) // 1024
        for q in range(nq):
            c1 = min(WCOLS, (q + 1) * 1024)
            dma(S, 'pool', win[:, :, q * 1024:c1], wview(wq)[:, :, q * 1024:c1], [], [('win', q)], f"win{q}")
        dma(S, 'sp', gq[:], io[gname], [], ['gq'], "gq")
        dma(S, 'pool', RT[:], io['ropeRT'], [], ['RT'], "RT")
        dma(S, 'pool', bones[:], io['blkones'], [], ['bones'], "bones")
        if kind == 'diff':
            ts(S, 'dve', gq[:, 0:1], gq[:, 0:1], 0.125, None, ALU.mult, None, ['gq'], ['gq'])
        else:
            ts(S, 'dve', gq[:, 0:1], gq[:, 0:1], 0.125, None, ALU.mult, None, ['gq'], ['gq'])
        tiles = [(i * 512, 512, 0) for i in range(8)] + [(SEQ, CTXL, 1)]
        n_ = 0
        no = 0
        nvv = 0
        for ti, (t0, W, s) in enumerate(tiles):
            sl = ti % 2
            hk = ('h', sl)
            ak = ('a', sl)
            tmp = dict(sq=sq, xn=xn, rs=rs, ps=pb[7], psk=('ps', 7))
            prenorm(S, g, hview(src)[:, :, t0:t0 + W], hb[sl][:, :, :W], hk, ab[sl][:, :, :W], ak, tmp, l, 0, s, W,
                    f"hld{sl}")
            dma(S, 'sp', cs[sl][:, :, :W], io['ropecs'][:, :, t0:t0 + W], [], [('cs', sl)], f"cs{sl}")
            chunks = list(range(NC_))
            if kind == 'swa' and s == 1:
                chunks = list(range(NQC, NC_))
            steps = []
            for ch in chunks:
                k2 = n_ % 2
                n_ += 1
                o3 = no % 3
                no += 1

                def A(ch=ch, k2=k2, W=W, sl=sl):
                    isq = ch < NQC
                    pp = pb[k2 * 3]
                    ppk = ('ps', k2 * 3)
                    col = ch * 128
                    for kc in range(8):
                        mm(S, pp[:, :W], win[:, kc, col:col + 128], ab[sl][:, kc, :W], kc == 0, kc == 7,
                           [('a', sl), ('win', col // 1024)], [ppk])
                    gcol = gq[:, 0:1] if isq else gq[:, 1:2]
                    act(S, xg[k2][:, :W], pp[:, :W], AF.Identity, [ppk, 'gq'], [('xg', k2)], scale=gcol)
                    act(S, xs[k2][:, :W], pp[:, :W], AF.Square, [ppk], [('xs', k2)])

                def B(ch=ch, k2=k2, o3=o3, W=W, sl=sl, t0=t0):
                    isq = ch < NQC
                    pm = pb[k2 * 3 + 1]
                    pr_ = pb[k2 * 3 + 2]
                    pmk, prk = ('ps', k2 * 3 + 1), ('ps', k2 * 3 + 2)
                    mm(S, pm[:, :W], bones[:], xs[k2][:, :W], True, True, [('xs', k2), 'bones'], [pmk])
                    mm(S, pr_[:, :W], RT[:], xg[k2][:, :W], True, True, [('xg', k2), 'RT'], [prk])
                    act(S, rd[k2][:, :W], pm[:, :W], AF.Sqrt, [pmk], [('rd', k2)], bias=g.epsb[:, 0:1], scale=1.0 / 64)
                    recip(S, rd[k2][:, :W], rd[k2][:, :W], [('rd', k2)], [('rd', k2)])
                    tt(S, 'pool', t1[k2][:, :W], xg[k2][:, :W], cs[sl][:, 0, :W], ALU.mult, [('xg', k2), ('cs', sl)], [('t1', k2)])
                    tt(S, 'dve', t2[k2][:, :W], pr_[:, :W], cs[sl][:, 1, :W], ALU.mult, [prk, ('cs', sl)], [('t2', k2)])
                    tt(S, 'pool', t1[k2][:, :W], t1[k2][:, :W], t2[k2][:, :W], ALU.add, [('t1', k2), ('t2', k2)], [('t1', k2)])
                    tt(S, 'dve', ob[o3][:, :W], t1[k2][:, :W], rd[k2][:, :W], ALU.mult, [('t1', k2), ('rd', k2)], [('ob', o3)])
                    dst_ = scr['QT'] if isq else scr['KT']
                    cc = ch if isq else ch - NQC
                    dma(S, 'sp', dst_[cc * 128:(cc + 1) * 128, t0:t0 + W], ob[o3][:, :W], [('ob', o3)], [], f"qst{o3}")

                steps.append((A, B))
            pipeline(steps, 1)
            for b in range(W // 128):
                for vq in range((VF + 511) // 512):
                    vw = min(512, VF - vq * 512)
                    v2 = nvv % 2
                    nvv += 1
                    pv = pb[6]
                    col = NC_ * 128 + vq * 512
                    for kc in range(8):
                        mm(S, pv[:, :vw], ab[sl][:, kc, b * 128:(b + 1) * 128], win[:, kc, col:col + vw], kc == 0, kc == 7,
                           [ak, ('win', col // 1024), ('win', (col + vw - 1) // 1024)], [('ps', 6)])
                    act(S, vb[v2][:, :vw], pv[:, :vw], AF.Copy, [('ps', 6)], [('vb', v2)])
                    r0 = t0 + b * 128
                    dma(S, 'sp', scr['V'][r0:r0 + 128, vq * 512:vq * 512 + vw], vb[v2][:, :vw], [('vb', v2)], [], f"vst{v2}")
        S.emit(nc, *sems)


def phase_att(nc, g, io, sems, src, dst, l, kind, scr):
    nc = _NCW(nc)
    S = Sched()
    NB = T // 128
    if kind == 'diff':
        NKC, VF = 8, 1024
    else:
        NKC, VF = 2, 256
    with ExitStack() as es:
        KT = es.enter_context(nc.sbuf_tensor("aKT", [128, NKC, T], BF16))
        V = es.enter_context(nc.sbuf_tensor("aV", [128, NB, VF], BF16))
        if kind == 'diff':
            wout = es.enter_context(nc.sbuf_tensor("awo", [128, 8, 1024], BF16))
            lp = es.enter_context(nc.sbuf_tensor("alp", [128, 4, 64], F32))
            pr2 = es.enter_context(nc.sbuf_tensor("apr2", [128, 2, 64], F32))
            s2 = es.enter_context(nc.sbuf_tensor("as2", [128, 2], F32))
            nlam = es.enter_context(nc.sbuf_tensor("anl", [128, 1], F32))
            sg = es.enter_context(nc.sbuf_tensor("asg", [128, 1], F32))
            OT = es.enter_context(nc.sbuf_tensor("aOT", [128, 8, 512], BF16))
            La = [[es.enter_context(nc.sbuf_tensor(f"aLa{r}{q}", [128, 512], F32)) for q in range(2)] for r in range(2)]
            onesf = es.enter_context(nc.sbuf_tensor("aonesf", [128, 128], F32))
        else:
            wout = es.enter_context(nc.sbuf_tensor("awo", [64, 16, 1024], BF16))
            snk = es.enter_context(nc.sbuf_tensor("asnk", [128, 16], F32))
            msk = es.enter_context(nc.sbuf_tensor("amsk", [128, 6, 512], BF16))
            identb = es.enter_context(nc.sbuf_tensor("aidb", [128, 128], BF16))
            OT = es.enter_context(nc.sbuf_tensor("aOT", [64, 16, 512], BF16))
        NSL = 1 if kind == 'diff' else 2
        if kind == 'diff':
            QZ = es.enter_context(nc.sbuf_tensor("aQZ", [128, 8, 1024], BF16))
            qz4 = QZ[:].rearrange("p c (r w) -> p c r w", r=2)
            hv = QZ[:].bitcast(F32)
            QT = hb = None
        else:
            QT = [es.enter_context(nc.sbuf_tensor(f"aQ{i}", [128, 8, 1024], BF16)) for i in range(NSL)]
            qzs = [q_[:].rearrange("p c (r w) -> p c r w", r=2) for q_ in QT]
            hb = [es.enter_context(nc.sbuf_tensor(f"ah{i}", [128, 8, 512], F32)) for i in range(NSL)]
        P = [es.enter_context(nc.sbuf_tensor(f"aP{i}", [128, 512], BF16)) for i in range(4)]
        PM = [es.enter_context(nc.sbuf_tensor(f"aPM{i}", [128, 512], BF16)) for i in range(2)] if kind != 'diff' else None
        r0b = es.enter_context(nc.sbuf_tensor("ar0", [128, 512], F32))
        r1b = es.enter_context(nc.sbuf_tensor("ar1", [128, 512], F32))
        o0 = es.enter_context(nc.sbuf_tensor("ao0", [128, 512], F32))
        o1 = r1b
        osq = es.enter_context(nc.sbuf_tensor("aosq", [128, 512], BF16))
        pb = [es.enter_context(nc.psum_tensor(f"ap{i}", [128, 512], F32)) for i in range(8)]
        for c in range(NKC):
            dma(S, 'sp', KT[:, c, :], scr['KT'][c * 128:(c + 1) * 128, :], [], [('KT', c)], f"kt{c % 4}")
        for q in range(4):
            b0, b1 = q * 9, min(NB, (q + 1) * 9)
            dma(S, 'sp', V[:, b0:b1, :], scr['V'][b0 * 128:b1 * 128, :VF].rearrange("(b p) f -> p b f", p=128),
                [], [('V', q)], f"v{q}")
        if kind == 'diff':
            dma(S, 'pool', wout[:], wview(io['da_out_w']), [], ['wout'], "wout")
            mset(S, 'pool', onesf[:], 1.0, [], ['onesf'])
            dma(S, 'sp', lp[:], io['da_lam'].partition_broadcast(128).rearrange("p o (a d) -> p (o a) d", a=4), [], ['lp'], "lp")
            dma(S, 'sp', sg[:], io['da_subg'], [], ['sg'], "sg")
            lam_init = 0.8 - 0.6 * math.exp(-0.3 * l)
            for i in range(2):
                tt(S, 'dve', pr2[:, i, :], lp[:, 2 * i, :], lp[:, 2 * i + 1, :], ALU.mult, ['lp'], ['pr2'])
            S.add('dve', (lambda o_, i_: (lambda e: e.tensor_reduce(out=o_, in_=i_, axis=AX.X, op=ALU.add)))(
                s2[:], pr2[:]), ['pr2'], ['s2'])
            act(S, s2[:], s2[:], AF.Exp, ['s2'], ['s2'])
            tt(S, 'dve', nlam[:], s2[:, 1:2], s2[:, 0:1], ALU.subtract, ['s2'], ['nlam'])
            ts(S, 'dve', nlam[:], nlam[:], -lam_init, None, ALU.add, None, ['nlam'], ['nlam'])
            ts(S, 'dve', sg[:], sg[:], 1.0 - lam_init, None, ALU.mult, None, ['sg'], ['sg'])
            tiles = [(i * 512, 512, 0) for i in range(8)] + [(SEQ, CTXL, 1)]
        else:
            dma(S, 'pool', wout[:], io['sw_out_wp'].rearrange("(h p) m -> p h m", p=64), [], ['wout'], "wout")
            dma(S, 'sp', snk[:], io['sw_sinkp'].partition_broadcast(128), [], ['snk'], "snk")
            dma(S, 'pool', msk[:], io['swa_mask'], [], ['msk'], "msk")
            dma(S, 'pool', identb[:], io['ident'], [], ['identb'], "identb")
            act(S, snk[:], snk[:], AF.Exp, ['snk'], ['snk'])
            for i_ in range(NSL):
                mset(S, 'pool', QT[i_][:], 0.0, [], [('Q', i_, 0), ('Q', i_, 1)])
            tiles = [(i * 512, 512, 0) for i in range(8)]
        np_ = 0
        for ti, (t0, W, s) in enumerate(tiles):
            sl = ti % NSL
            hk = ('h', sl)
            qk = ('Q', sl)
            if kind == 'diff':
                qsrc = scr['QT'].rearrange("(c p) t -> p c t", p=128)
                for r in range(2):
                    z0 = (1 - r) * 64
                    mset(S, 'pool', qz4[z0:z0 + 64, :, r, :W], 0.0, [], [('QZ', r)])
                    dma(S, 'sp', qz4[r * 64:(r + 1) * 64, :, r, :W], qsrc[r * 64:(r + 1) * 64, :, t0:t0 + W], [],
                        [('QZ', r)], f"qld{r}")
            else:
                dma(S, 'sp', hb[sl][:, :, :W], hview(src)[:, :, t0:t0 + W], [], [hk], f"hld{sl}")
                qsrc = scr['QT'].rearrange("(c p) t -> p c t", p=128)
                for r in range(2):
                    dma(S, 'sp', qzs[sl][r * 64:(r + 1) * 64, :, r, :W], qsrc[r * 64:(r + 1) * 64, :, t0:t0 + W], [],
                        [('Q', sl, r)], f"qld{sl}_{r}")
            if kind == 'diff':
                kbs = [(kb, None) for kb in (range(NB) if s == 0 else range(32, 34))]
                steps = []
                for vh in range(8):
                    for r in range(2):
                        for ki, (kb, _) in enumerate(kbs):
                            i3, i4 = np_ % 3, np_ % 4
                            np_ += 1
                            last = (ki == len(kbs) - 1)

                            def A(vh=vh, r=r, kb=kb, i3=i3, i4=i4, W=W, sl=sl):
                                mm(S, pb[i3][:, :W], KT[:, vh, kb * 128:(kb + 1) * 128],
                                   qz4[:, vh, r, :W], True, True, [('KT', vh), ('QZ', r)], [('ps', i3)])
                                act(S, P[i4][:, :W], pb[i3][:, :W], AF.Exp, [('ps', i3)], [('P', i4)])

                            def B(vh=vh, r=r, kb=kb, ki=ki, last=last, i4=i4, W=W):
                                po = pb[3 + r]
                                pok = ('ps', 3 + r)
                                mm(S, po[:, :W], V[:, kb, vh * 128:(vh + 1) * 128], P[i4][:, :W], ki == 0, last,
                                   [('V', kb // 9), ('P', i4)], [pok])
                                q_ = ki % 2
                                eng = 'dve' if q_ == 0 else 'pool'
                                lak = ('La', r, q_)
                                if ki < 2:
                                    cp(S, eng, La[r][q_][:, :W], P[i4][:, :W], [('P', i4)], [lak])
                                else:
                                    tt(S, eng, La[r][q_][:, :W], La[r][q_][:, :W], P[i4][:, :W], ALU.add, [lak, ('P', i4)], [lak])
                                if last and r == 1:
                                    for rr, rb, rk in ((0, r0b, 'r0'), (1, r1b, 'r1')):
                                        for q2 in range(2):
                                            mm(S, pb[5][:, :W], onesf[:], La[rr][q2][:, :W], q2 == 0, q2 == 1,
                                               [('La', rr, q2), 'onesf'], [('ps', 5)])
                                        recip(S, rb[:, :W], pb[5][:, :W], [('ps', 5)], [rk])
                                    tt(S, 'dve', o0[:, :W], pb[3][:, :W], r0b[:, :W], ALU.mult, [('ps', 3), 'r0'], ['o0'])
                                    tt(S, 'dve', r1b[:, :W], pb[4][:, :W], r1b[:, :W], ALU.mult, [('ps', 4), 'r1'], ['r1'])
                                    stt(S, o0[:, :W], r1b[:, :W], nlam[:, 0:1], o0[:, :W], ALU.mult, ALU.add,
                                        ['o0', 'r1', 'nlam'], ['o0'])
                                    tt(S, 'pool', osq[:, :W], o0[:, :W], o0[:, :W], ALU.mult, ['o0'], ['osq'])
                                    mm(S, pb[6][:, :W], g.ones[:], osq[:, :W], True, True, ['osq'], [('ps', 6)])
                                    act(S, r0b[:, :W], pb[6][:, :W], AF.Sqrt, [('ps', 6)], ['r0'], bias=g.epsb[:, 0:1],
                                        scale=1.0 / 128)
                                    recip(S, r0b[:, :W], r0b[:, :W], ['r0'], ['r0'])
                                    stt(S, OT[:, vh, :W], o0[:, :W], sg[:, 0:1], r0b[:, :W], ALU.mult, ALU.mult,
                                        ['o0', 'sg', 'r0'], ['OT'])

                            steps.append((A, B))
                pipeline(steps, 2)
                hks = [('QZ', 0), ('QZ', 1)]
                dma(S, 'sp', hv[:, :, :W], hview(src)[:, :, t0:t0 + W], [], hks, "hld0")
                for m in range(8):
                    po = pb[6 + (m % 2)]
                    pok = ('ps', 6 + (m % 2))
                    for vh in range(8):
                        mm(S, po[:, :W], wout[:, vh, m * 128:(m + 1) * 128], OT[:, vh, :W], vh == 0, vh == 7,
                           ['wout', 'OT'], [pok])
                    stt(S, hv[:, m, :W], po[:, :W], MV(g, l, 2, m, s), hv[:, m, :W], ALU.mult, ALU.add,
                        [pok] + hks, hks)
                dma(S, 'sp', hview(dst)[:, :, t0:t0 + W], hv[:, :, :W], hks, [], "hst0")
            else:
                j0 = t0 // 128
                kbs = [(32, None), (33, None)] + [(kb, kb - j0 + 1) for kb in range(j0 - 1, j0 + 5) if 0 <= kb < 32]
                steps = []
                SB = [0, 1, 6]
                for n in range(16):
                    for ki, (kb, mi) in enumerate(kbs):
                        sb_, i4 = SB[np_ % 3], np_ % 4
                        np_ += 1
                        last = (ki == len(kbs) - 1)

                        def A(n=n, kb=kb, mi=mi, sb_=sb_, i4=i4, W=W, sl=sl):
                            c, r = n // 2, n % 2
                            gk = SWA_PERM[n] // 4
                            mm(S, pb[sb_][:, :W], KT[:, gk // 2, kb * 128:(kb + 1) * 128],
                               qzs[sl][:, c, r, :W], True, mi is None, [('KT', gk // 2), ('Q', sl, r)], [('ps', sb_)])
                            if mi is not None:
                                mm(S, pb[sb_][:, :W], identb[:], msk[:, mi, :W], False, True, ['identb', 'msk'], [('ps', sb_)])
                            act(S, P[i4][:, :W], pb[sb_][:, :W], AF.Exp, [('ps', sb_)], [('P', i4)])

                        def B(n=n, kb=kb, ki=ki, last=last, i4=i4, W=W):
                            gk = SWA_PERM[n] // 4
                            po = pb[2 + 2 * (n % 2)]
                            pl = pb[3 + 2 * (n % 2)]
                            pok, plk = ('ps', 2 + 2 * (n % 2)), ('ps', 3 + 2 * (n % 2))
                            pu, puk = P[i4], ('P', i4)
                            mm(S, po[:64, :W], V[:, kb, gk * 64:(gk + 1) * 64], pu[:, :W], ki == 0, last,
                               [('V', kb // 9), puk], [pok])
                            mm(S, pl[:64, :W], g.ones[:, :64], pu[:, :W], ki == 0, last, [puk], [plk])
                            if last:
                                ts(S, 'dve', r0b[:64, :W], pl[:64, :W], snk[:64, n:n + 1], None, ALU.add, None,
                                   [plk, 'snk'], ['r0'])
                                recip(S, r0b[:64, :W], r0b[:64, :W], ['r0'], ['r0'])
                                tt(S, 'dve', OT[:, n, :W], po[:64, :W], r0b[:64, :W], ALU.mult, [pok, 'r0'], ['OT'])

                        steps.append((A, B))
                pipeline(steps, 2)
                for m in range(8):
                    po = pb[6 + (m % 2)]
                    pok = ('ps', 6 + (m % 2))
                    for n in range(16):
                        mm(S, po[:, :W], wout[:, n, m * 128:(m + 1) * 128], OT[:, n, :W], n == 0, n == 15,
                           ['wout', 'OT'], [pok])
                    stt(S, hb[sl][:, m, :W], po[:, :W], MV(g, l, 2, m, s), hb[sl][:, m, :W], ALU.mult, ALU.add,
                        [pok, hk], [hk])
            if kind != 'diff':
                dma(S, 'sp', hview(dst)[:, :, t0:t0 + W], hb[sl][:, :, :W], [hk], [], f"hst{sl}")
        if kind == 'swa':
            pass
        S.emit(nc, *sems)


IN_SPECS = [
    ("hT0", [D, T]), ("cT", [128, 8, 2]), ("ada_w", [4, D, 6 * D]), ("ada_bT", [128, 4, 48]),
    ("gmixT", [128, 4, 8]), ("gffnT", [128, 4, 8]), ("ident", [128, 128]),
    ("sc_in_w", [D, 3 * D]), ("sc_convT", [128, 8, 3]), ("sc_out_w", [D, D]),
    ("ffn_w1", [2, D, FF]), ("ffn_w3", [2, D, FF]), ("ffn_w2", [2, FF, D]),
    ("moe_rwT", [2, 128, 8, NE]), ("moe_rb", [2, 1, NE]),
    ("moe_w1", [2, NE, D, FF]), ("moe_w3", [2, NE, D, FF]), ("moe_w2", [2, NE, FF, D]),
    ("cm_in_w", [D, 4096]), ("cm_out_w", [2048, D]), ("cm_wsT", [128, 8, 128]), ("cm_buT", [128, 16]),
    ("cm_bv", [1, 2048]), ("cm_vgT", [128, 16]), ("cm_bsf", [1, 1024]),
    ("da_qkv_w", [D, 3072]), ("da_out_w", [D, D]), ("da_qkg", [128, 2]), ("da_lam", [1, 256]), ("da_subg", [128, 1]),
    ("sw_qkv_wp", [D, 1536]), ("sw_out_wp", [D, D]), ("sw_qkg", [128, 2]), ("sw_sinkp", [1, 16]),
    ("ropecs", [128, 2, T]), ("ropeRT", [128, 128]), ("blkones", [128, 128]), ("swa_mask", [128, 6, 512]),
]


def build(nphase):
    nc = bass.Bass("TRN2", target_bir_lowering=False)
    io = {}
    for name, shape in IN_SPECS:
        io[name] = nc.dram_tensor(name, shape, F32, kind="ExternalInput").ap()
    out = nc.dram_tensor("out", [D, T], F32, kind="ExternalOutput").ap()
    hA = nc.dram_tensor("hA", [D, T], F32, kind="Internal").ap()
    hB = nc.dram_tensor("hB", [D, T], F32, kind="Internal").ap()
    g = G()
    with ExitStack() as es:
        g.modv = es.enter_context(nc.sbuf_tensor("modv", [128, 4, 48, 2], F32))
        g.ones = es.enter_context(nc.sbuf_tensor("ones", [128, 128], BF16))
        g.ident = es.enter_context(nc.sbuf_tensor("ident_sb", [128, 128], F32))
        g.epsb = es.enter_context(nc.sbuf_tensor("epsb", [128, 1], F32))
        NSET = 3
        semsets = []
        for k in range(NSET):
            esem = {e: es.enter_context(nc.semaphore(f"es{k}_{e}")) for e in ENGS}
            dsems = [es.enter_context(nc.semaphore(f"ds{k}_{i}")) for i in range(NDSEM)]
            semsets.append((esem, dsems, dict(ebase={e: 0 for e in ENGS}, dbase=[0] * NDSEM)))
        with nc.Block() as blk:
            blk.vector(lambda e: e.memset(g.epsb[:], EPS))

        scr = dict(
            QT=nc.dram_tensor("scrQT", [D, T], BF16, kind="Internal").ap(),
            KT=nc.dram_tensor("scrKT", [D, T], BF16, kind="Internal").ap(),
            V=nc.dram_tensor("scrV", [T, D], BF16, kind="Internal").ap(),
        )
        P_ = [
            ('conv', lambda sm, a, b: phase_conv(nc, g, io, sm, a, b), True),
            ('ffn0', lambda sm, a, b: phase_ffn(nc, g, io, sm, a, b, 0, False, True), True),
            ('qkv1', lambda sm, a, b: phase_qkv(nc, g, io, sm, a, 1, 'diff', scr), False),
            ('att1', lambda sm, a, b: phase_att(nc, g, io, sm, a, b, 1, 'diff', scr), True),
            ('moe1', lambda sm, a, b: phase_ffn(nc, g, io, sm, a, b, 1, True, True), True),
            ('gmlp', lambda sm, a, b: phase_gmlp(nc, g, io, sm, a, b), True),
            ('ffn2', lambda sm, a, b: phase_ffn(nc, g, io, sm, a, b, 2, False, True), True),
            ('qkv3', lambda sm, a, b: phase_qkv(nc, g, io, sm, a, 3, 'swa', scr), False),
            ('att3', lambda sm, a, b: phase_att(nc, g, io, sm, a, b, 3, 'swa', scr), True),
            ('moe3', lambda sm, a, b: phase_ffn(nc, g, io, sm, a, b, 3, True, False), True),
        ]
        sel = os.environ.get("KPH")
        if sel is not None:
            idx = [int(x) for x in sel.split(",") if x != ""]
        else:
            idx = list(range(min(nphase, len(P_))))
        pi = 0
        phase_adaln(nc, g, io, semsets[0])
        cur = io['hT0']
        bufs = [hB, hA]
        nb_ = 0
        for ix in idx:
            pi += 1
            sm = semsets[pi % NSET]
            dstb = bufs[nb_ % 2]
            P_[ix][1](sm, cur, dstb)
            if P_[ix][2]:
                cur = dstb
                nb_ += 1
        last = cur if nb_ > 0 else None
        with nc.semaphore("fin") as fin, nc.Block() as blk:
            def _fin(e):
                if last is not None:
                    e.dma_start(out=out, in_=last).then_inc(fin, 16)
                    e.wait_ge(fin, 16)
            blk.sync(_fin)
    return nc


_NC_CACHE = {}
_CONST = {}


def _consts():
    if _CONST:
        return _CONST
    t = np.arange(SEQ)
    row = (t // 64).astype(np.float32)
    colp = (t % 64).astype(np.float32)
    inv = (np.float32(10000.0) ** (-np.arange(16, dtype=np.float32) / np.float32(16))).astype(np.float32)
    ang = np.zeros((64, SEQ), np.float32)
    for d in range(64):
        pos = row if d < 32 else colp
        ang[d] = pos * inv[d % 16]
    cos = np.ones((64, T), np.float32)
    sin = np.zeros((64, T), np.float32)
    cos[:, :SEQ] = np.cos(ang)
    sin[:, :SEQ] = np.sin(ang)
    cs = np.stack([np.concatenate([cos, cos], 0), np.concatenate([sin, sin], 0)], axis=1)
    R = np.zeros((64, 64), np.float32)
    for part in range(2):
        b = part * 32
        for i in range(16):
            R[b + i, b + i + 16] = -1.0
            R[b + i + 16, b + i] = 1.0
    RT = np.zeros((128, 128), np.float32)
    RT[:64, :64] = R.T
    RT[64:, 64:] = R.T
    bo = np.zeros((128, 128), np.float32)
    bo[:64, :64] = 1.0
    bo[64:, 64:] = 1.0
    kk = np.arange(128)[:, None, None]
    mi = np.arange(6)[None, :, None]
    qq = np.arange(512)[None, None, :]
    dlt = (mi - 1) * 128 + kk - qq
    mask = np.where(np.abs(dlt) <= 128, 0.0, -30000.0).astype(np.float32)
    _CONST.update(ropecs=np.ascontiguousarray(cs), ropeRT=RT, blkones=bo, swa_mask=np.ascontiguousarray(mask))
    return _CONST


def host_inputs(inputs, b):
    x = inputs['x'][b]
    ctx = inputs['ctx'][b]
    m = {}
    m['hT0'] = np.ascontiguousarray(np.concatenate([x.T, ctx.T], axis=1))
    cc = np.stack([inputs['c'][b], inputs['c_ctx']], axis=1)
    m['cT'] = np.ascontiguousarray(cc.reshape(8, 128, 2).transpose(1, 0, 2))
    m['ada_w'] = inputs['ada_w']
    m['ada_bT'] = np.ascontiguousarray(inputs['ada_b'].reshape(4, 48, 128).transpose(2, 0, 1))
    m['gmixT'] = np.ascontiguousarray(inputs['norm_mix_g'].reshape(4, 8, 128).transpose(2, 0, 1))
    m['gffnT'] = np.ascontiguousarray(inputs['norm_ffn_g'].reshape(4, 8, 128).transpose(2, 0, 1))
    m['ident'] = np.eye(128, dtype=np.float32)
    m['sc_in_w'] = inputs['sc_in_w'][0]
    m['sc_convT'] = np.ascontiguousarray(inputs['sc_conv_w'][0].reshape(3, 8, 128).transpose(2, 1, 0))
    m['sc_out_w'] = inputs['sc_out_w'][0]
    m['ffn_w1'] = inputs['ffn_w1']
    m['ffn_w3'] = inputs['ffn_w3']
    m['ffn_w2'] = inputs['ffn_w2']
    m['moe_rwT'] = np.ascontiguousarray(inputs['moe_router_w'].reshape(2, 8, 128, NE).transpose(0, 2, 1, 3))
    m['moe_rb'] = np.ascontiguousarray(inputs['moe_router_b'].reshape(2, 1, NE))
    m['moe_w1'] = inputs['moe_w1']
    m['cm_in_w'] = inputs['cm_in_w'][0]
    m['da_qkv_w'] = inputs['da_qkv_w'][0]
    m['da_out_w'] = inputs['da_out_w'][0]
    m['da_qkg'] = np.stack([np.tile(inputs['da_q_norm_g'][0], 2), np.tile(inputs['da_k_norm_g'][0], 2)], axis=1)
    m['da_lam'] = inputs['da_lambda'][0].reshape(1, 256)
    m['da_subg'] = inputs['da_sub_norm_g'][0].reshape(128, 1)
    wqkv = inputs['sw_qkv_w'][0]
    perm = np.array(SWA_PERM)
    qcols = (perm[:, None] * 64 + np.arange(64)[None, :]).reshape(-1)
    m['sw_qkv_wp'] = np.concatenate([wqkv[:, qcols], wqkv[:, 1024:]], axis=1)
    m['sw_out_wp'] = inputs['sw_out_w'][0][qcols, :]
    m['sw_qkg'] = np.stack([np.tile(inputs['sw_q_norm_g'][0], 2), np.tile(inputs['sw_k_norm_g'][0], 2)], axis=1)
    m['sw_sinkp'] = inputs['sw_sink'][0][perm].reshape(1, 16)
    m.update(_consts())
    m['cm_out_w'] = inputs['cm_out_w'][0]
    m['cm_wsT'] = np.ascontiguousarray(inputs['cm_ws'][0].transpose(2, 0, 1))
    m['cm_buT'] = np.ascontiguousarray(inputs['cm_in_b'][0][:2048].reshape(16, 128).T)
    m['cm_bv'] = np.ascontiguousarray(inputs['cm_in_b'][0][2048:].reshape(1, 2048))
    m['cm_vgT'] = np.ascontiguousarray(inputs['cm_v_norm_g'][0].reshape(16, 128).T)
    m['cm_bsf'] = np.ascontiguousarray(inputs['cm_bs'][0].reshape(1, 1024))
    m['moe_w3'] = inputs['moe_w3']
    m['moe_w2'] = inputs['moe_w2']
    return {k: np.ascontiguousarray(v, dtype=np.float32) for k, v in m.items()}


def kernel(**inputs):
    nphase = int(os.environ.get("KSTOP", "99"))
    ncores = int(os.environ.get("KCORES", "8"))
    inputs = {k: np.asarray(v) for k, v in inputs.items()}
    if nphase not in _NC_CACHE:
        _NC_CACHE[nphase] = build(nphase)
    nc = _NC_CACHE[nphase]
    in_maps = [host_inputs(inputs, b) for b in range(ncores)]
    res = run_bass_kernel_spmd(nc, in_maps, core_ids=list(range(ncores)))
    outs = [r["out"] for r in res.results]
    if os.environ.get("KRAW"):
        return outs
    full = np.stack([o[:, :SEQ].T for o in outs], axis=0)
    return np.ascontiguousarray(full.astype(np.float32))
```

```python
import os, math
from contextlib import ExitStack
import numpy as np
import concourse.bass as bass
import concourse.mybir as mybir
from concourse.bass_utils import run_bass_kernel_spmd

F32 = mybir.dt.float32
BF16 = mybir.dt.bfloat16
ALU = mybir.AluOpType
AF = mybir.ActivationFunctionType
AX = mybir.AxisListType

D = 1024
SEQ = 4096
CTXL = 256
T = SEQ + CTXL
FF = 2816
NE = 8
EPS = 1e-6
ENGS = ('pe', 'act', 'dve', 'pool', 'sp')
NDSEM = 28


class Sched:
    def __init__(self):
        self.ops = []
        self.lw = {}
        self.rd = {}

    def add(self, eng, fn, r=(), w=(), dma=False, sg=None, cost=300.0, xfer=0.0):
        deps = set()
        for k in r:
            x = self.lw.get(k)
            if x is not None:
                deps.add(x)
        for k in w:
            x = self.lw.get(k)
            if x is not None:
                deps.add(x)
            deps.update(self.rd.get(k, ()))
        i = len(self.ops)
        self.ops.append([eng, fn, deps, dma, sg, False, 0, float(cost), float(xfer)])
        for k in r:
            self.rd.setdefault(k, []).append(i)
        for k in w:
            self.lw[k] = i
            self.rd[k] = []
        return i

    def schedule(self, window=48, lat=150.0):
        ops = self.ops
        n = len(ops)
        left = [len(o[2]) for o in ops]
        users = [[] for _ in range(n)]
        for i, o in enumerate(ops):
            for d in o[2]:
                users[d].append(i)
        rdy = [0.0] * n
        queues = {e: [] for e in ENGS}
        for i, o in enumerate(ops):
            queues[o[0]].append(i)
        qpos = {e: 0 for e in ENGS}
        win = {e: [] for e in ENGS}
        etime = {e: 0.0 for e in ENGS}
        for e in ENGS:
            q = queues[e]
            while len(win[e]) < window and qpos[e] < len(q):
                win[e].append(q[qpos[e]])
                qpos[e] += 1
        order = []
        best = {e: None for e in ENGS}
        dirty = set(ENGS)
        done = 0
        while done < n:
            for e in dirty:
                b = None
                et = etime[e]
                for i in win[e]:
                    if left[i] == 0:
                        t = rdy[i] if rdy[i] > et else et
                        if b is None or t < b[0]:
                            b = (t, i)
                            if t <= et:
                                break
                best[e] = b
            dirty = set()
            pick = None
            for e in ENGS:
                b = best[e]
                if b is not None and (pick is None or b[0] < pick[0] or (b[0] == pick[0] and b[1] < pick[1])):
                    pick = (b[0], b[1], e)
            assert pick is not None, "scheduler stuck"
            t, i, e = pick
            o = ops[i]
            etime[e] = t + o[7]
            fin = etime[e] + o[8]
            order.append(i)
            done += 1
            win[e].remove(i)
            q = queues[e]
            if qpos[e] < len(q):
                win[e].append(q[qpos[e]])
                qpos[e] += 1
            dirty.add(e)
            for u in users[i]:
                ue = ops[u][0]
                r_ = fin if ue == e and not o[3] else fin + lat
                if r_ > rdy[u]:
                    rdy[u] = r_
                left[u] -= 1
                if left[u] == 0:
                    dirty.add(ue)
        newidx = {old_: new_ for new_, old_ in enumerate(order)}
        nops = []
        for old_ in order:
            o = ops[old_]
            o[2] = {newidx[d] for d in o[2]}
            nops.append(o)
        self.ops = nops

    def emit(self, nc, esem, dsems, st):
        if getattr(self, 'use_sched', True) and os.environ.get("KSCHED", "1") == "1":
            self.schedule()
        ops = self.ops
        pos = {}
        cnt = {e: 0 for e in ENGS}
        for i, o in enumerate(ops):
            pos[i] = cnt[o[0]]
            cnt[o[0]] += 1
        for o in ops:
            if o[3]:
                o[5] = True
        for i, o in enumerate(ops):
            for d in o[2]:
                y = ops[d]
                if y[3]:
                    continue
                if y[0] != o[0] or o[3]:
                    y[5] = True
                elif o[0] != 'pe' and pos[i] - pos[d] <= 2:
                    y[5] = True
        ec = st['ebase']
        sgc = {}
        sgsem = {}
        sgidx = {}
        for o in ops:
            if o[3]:
                sg = o[4]
                if sg not in sgsem:
                    assert len(sgsem) < len(dsems), "too many dma sem groups"
                    sgidx[sg] = len(sgsem)
                    sgsem[sg] = dsems[len(sgsem)]
                    sgc[sg] = st['dbase'][sgidx[sg]]
                sgc[sg] = sgc[sg] + 16
                o[6] = sgc[sg]
            elif o[5]:
                ec[o[0]] += 1
                o[6] = ec[o[0]]
        for sg, i_ in sgidx.items():
            st['dbase'][i_] = sgc[sg]
        prog = {e: [] for e in ENGS}
        seen = {e: {} for e in ENGS}
        for i, o in enumerate(ops):
            waits = {}
            for d in o[2]:
                y = ops[d]
                if y[3]:
                    key = ('d', y[4])
                    sem = sgsem[y[4]]
                else:
                    if y[0] == o[0] and not o[3]:
                        if o[0] == 'pe' or pos[i] - pos[d] > 2:
                            continue
                    key = ('e', y[0])
                    sem = esem[y[0]]
                v = y[6]
                if waits.get(key, (None, 0))[1] < v:
                    waits[key] = (sem, v)
            wl = []
            for key, (sem, v) in waits.items():
                if seen[o[0]].get(key, 0) >= v:
                    continue
                seen[o[0]][key] = v
                wl.append((sem, v))
            prog[o[0]].append((wl, o))

        def run(e, name):
            for wl, o in prog[name]:
                for sem, v in wl:
                    e.wait_ge(sem, v)
                ins = o[1](e)
                if o[3]:
                    ins.then_inc(sgsem[o[4]], 16)
                elif o[5]:
                    ins.then_inc(esem[name], 1)
            if name == 'sp':
                for sg, v in sgc.items():
                    e.wait_ge(sgsem[sg], v)

        with nc.Block() as blk:
            blk.tensor(lambda e: run(e, 'pe'))
            blk.scalar(lambda e: run(e, 'act'))
            blk.vector(lambda e: run(e, 'dve'))
            blk.gpsimd(lambda e: run(e, 'pool'))
            blk.sync(lambda e: run(e, 'sp'))


def _fsz(ap):
    n = 1
    for d in ap.shape[1:]:
        n *= int(d)
    return n


def _cost(eng, ap):
    n = _fsz(ap)
    if eng == 'dve':
        return 70.0 + n / 0.75
    if eng == 'pool':
        return 100.0 + n / 0.485
    return 110.0 + n / 0.96


def mm(S, out, lhsT, rhs, start, stop, r, w):
    c = max(64.0, _fsz(out) / 2.4 + 45.0)
    if lhsT.dtype == F32:
        c *= 4.0
    S.add('pe', lambda e: e.matmul(out, lhsT, rhs, start=start, stop=stop), r, w, cost=c)


def act(S, out, in_, func, r, w, bias=None, scale=None, accum=None):
    kw = {}
    if bias is not None:
        kw['bias'] = bias
    if scale is not None:
        kw['scale'] = scale
    if accum is not None:
        kw['accum_out'] = accum
    S.add('act', lambda e: e.activation(out=out, in_=in_, func=func, **kw), r, w, cost=_cost('act', out))


def tt(S, eng, out, in0, in1, op, r, w):
    S.add(eng, lambda e: e.tensor_tensor(out=out, in0=in0, in1=in1, op=op), r, w, cost=_cost(eng, out))


def ts(S, eng, out, in0, s1, s2, op0, op1, r, w):
    if s2 is None:
        S.add(eng, lambda e: e.tensor_scalar(out=out, in0=in0, scalar1=s1, scalar2=None, op0=op0), r, w, cost=_cost(eng, out))
    else:
        S.add(eng, lambda e: e.tensor_scalar(out=out, in0=in0, scalar1=s1, scalar2=s2, op0=op0, op1=op1), r, w, cost=_cost(eng, out))


def stt(S, out, in0, scalar, in1, op0, op1, r, w):
    S.add('dve', lambda e: e.scalar_tensor_tensor(out=out, in0=in0, scalar=scalar, in1=in1, op0=op0, op1=op1), r, w, cost=_cost('dve', out))


def cp(S, eng, out, in_, r, w):
    S.add(eng, lambda e: e.tensor_copy(out=out, in_=in_), r, w, cost=_cost(eng, out))


def recip(S, out, in_, r, w):
    S.add('dve', lambda e: e.reciprocal(out=out, in_=in_), r, w, cost=_cost('dve', out))


def mset(S, eng, ap, val, r, w):
    S.add(eng, lambda e: e.memset(ap, val), r, w, cost=_cost(eng, ap))


def dma(S, q, out, in_, r, w, sg):
    nb_ = int(out.shape[0]) * _fsz(out) * 4
    S.add(q, lambda e: e.dma_start(out=out, in_=in_), r, w, dma=True, sg=sg,
          cost=(1000.0 if q == 'pool' else 60.0), xfer=2000.0 + nb_ / 150.0)


def pipeline(steps, depth=1):
    pend = []
    for A, B in steps:
        A()
        pend.append(B)
        if len(pend) > depth:
            pend.pop(0)()
    for B in pend:
        B()


def hview(ap):
    return ap.rearrange("(c p) t -> p c t", p=128)


def wview(ap):
    return ap.rearrange("(kc p) f -> p kc f", p=128)


class G:
    pass


_UID = [0]


def _un(name):
    _UID[0] += 1
    return f"{name}_{_UID[0]}"


class _NCW:
    def __init__(self, nc):
        self._nc = nc

    def sbuf_tensor(self, name, shape, dt):
        return self._nc.sbuf_tensor(_un(name), shape, dt)

    def psum_tensor(self, name, shape, dt):
        return self._nc.psum_tensor(_un(name), shape, dt)

    def __getattr__(self, k):
        return getattr(self._nc, k)


def MV(g, l, which, c, s):
    return g.modv[:, l, which * 8 + c, s:s + 1]


def phase_adaln(nc, g, io, sems):
    nc = _NCW(nc)
    S = Sched()
    with ExitStack() as es:
        wb = [es.enter_context(nc.sbuf_tensor(f"adw{i}", [128, 8, 3072], BF16)) for i in range(2)]
        cT = es.enter_context(nc.sbuf_tensor("cT", [128, 8, 2], F32))
        sT = es.enter_context(nc.sbuf_tensor("sT", [128, 8, 2], BF16))
        bT = es.enter_context(nc.sbuf_tensor("bT", [128, 4, 48], F32))
        gm = es.enter_context(nc.sbuf_tensor("gm", [128, 4, 8], F32))
        gf = es.enter_context(nc.sbuf_tensor("gf", [128, 4, 8], F32))
        ps = es.enter_context(nc.psum_tensor("ps_ada", [128, 512], F32))
        mset(S, 'pool', g.ones[:], 1.0, [], ['ones'])
        dma(S, 'sp', g.ident[:], io['ident'], [], ['ident'], 'c0')
        dma(S, 'sp', cT[:], io['cT'], [], ['cT'], 'c1')
        dma(S, 'sp', bT[:], io['ada_bT'], [], ['bT'], 'c2')
        dma(S, 'sp', gm[:], io['gmixT'], [], ['gm'], 'c3')
        dma(S, 'sp', gf[:], io['gffnT'], [], ['gf'], 'c4')
        act(S, sT[:], cT[:], AF.Silu, ['cT'], ['sT'])
        n = 0
        for l in range(4):
            for half in range(2):
                slot = n % 2
                n += 1
                for q in range(2):
                    c0 = half * 3072 + q * 1536
                    dma(S, 'pool', wb[slot][:, :, q * 1536:(q + 1) * 1536], wview(io['ada_w'][l])[:, :, c0:c0 + 1536],
                        [], [('adw', slot, q)], f"adw{slot}_{q}")
                for jj in range(24):
                    j = half * 24 + jj
                    q = (jj * 128) // 1536
                    col = (l * 48 + j) * 2
                    for kc in range(8):
                        mm(S, ps[:, col:col + 2], wb[slot][:, kc, jj * 128:(jj + 1) * 128], sT[:, kc, :],
                           kc == 0, kc == 7, [('adw', slot, q), 'sT'], ['psada'])
        for l in range(4):
            tt(S, 'dve', g.modv[:, l], ps[:, l * 96:(l + 1) * 96].rearrange("p (j s) -> p j s", s=2),
               bT[:, l, :].unsqueeze(2).to_broadcast([128, 48, 2]), ALU.add, ['psada', 'bT'], ['modv'])
            stt(S, g.modv[:, l, 8:16, :], g.modv[:, l, 8:16, :], 1.0,
                gm[:, l, :].unsqueeze(2).to_broadcast([128, 8, 2]), ALU.add, ALU.mult, ['modv', 'gm'], ['modv'])
            stt(S, g.modv[:, l, 32:40, :], g.modv[:, l, 32:40, :], 1.0,
                gf[:, l, :].unsqueeze(2).to_broadcast([128, 8, 2]), ALU.add, ALU.mult, ['modv', 'gf'], ['modv'])
        S.emit(nc, *sems)


def prenorm(S, g, src_cols, hb, hk, ab, ak, tmp, l, which, s, n, sg, a32=None, a32k=None):
    sq = tmp['sq'][:, :, :n]
    xn = tmp['xn'][:, :, :n]
    rs = tmp['rs'][:, :n]
    ps = tmp['ps'][:, :n]
    psk = tmp['psk']
    sqk = tmp.get('sqk', 'sq')
    hks = hk if isinstance(hk, list) else [hk]
    dma(S, 'sp', hb, src_cols, [], hks, sg)
    tt(S, 'pool', sq, hb, hb, ALU.mult, hks, [sqk])
    for c in range(8):
        mm(S, ps, g.ones[:], sq[:, c, :], c == 0, c == 7, [sqk, 'ones'], [psk])
    act(S, rs, ps, AF.Sqrt, [psk], ['rs'], bias=g.epsb[:, 0:1], scale=1.0 / 1024)
    recip(S, rs, rs, ['rs'], ['rs'])
    tt(S, 'dve', xn, hb, rs.unsqueeze(1).to_broadcast([128, 8, n]), ALU.mult, hks + ['rs'], ['xn'])
    for c in range(8):
        if a32 is None:
            act(S, ab[:, c, :], xn[:, c, :], AF.Identity, ['xn'], [ak],
                scale=MV(g, l, which * 3 + 1, c, s), bias=MV(g, l, which * 3, c, s))
        else:
            act(S, a32[:, c, :], xn[:, c, :], AF.Identity, ['xn'], [a32k],
                scale=MV(g, l, which * 3 + 1, c, s), bias=MV(g, l, which * 3, c, s))
            cp(S, 'pool', ab[:, c, :], a32[:, c, :], [a32k], [ak])


def phase_conv(nc, g, io, sems, src, dst, l=0):
    nc = _NCW(nc)
    S = Sched()
    W = 256
    with ExitStack() as es:
        win = es.enter_context(nc.sbuf_tensor("scwin", [128, 8, 3072], BF16))
        wout = es.enter_context(nc.sbuf_tensor("scwout", [128, 8, 1024], BF16))
        cw = es.enter_context(nc.sbuf_tensor("sccw", [128, 8, 3], F32))
        hb = [es.enter_context(nc.sbuf_tensor(f"sch{i}", [128, 8, W + 2], F32)) for i in range(2)]
        ab = [es.enter_context(nc.sbuf_tensor(f"sca{i}", [128, 8, W + 2], BF16)) for i in range(2)]
        sq = es.enter_context(nc.sbuf_tensor("scsq", [128, 8, W + 2], BF16))
        xn = es.enter_context(nc.sbuf_tensor("scxn", [128, 8, W + 2], F32))
        rs = es.enter_context(nc.sbuf_tensor("scrs", [128, W + 2], F32))
        xv = [es.enter_context(nc.sbuf_tensor(f"scxv{i}", [128, W + 2], F32)) for i in range(2)]
        yb = [es.enter_context(nc.sbuf_tensor(f"scy{i}", [128, W + 2], F32)) for i in range(2)]
        cv = [es.enter_context(nc.sbuf_tensor(f"sccv{i}", [128, W], F32)) for i in range(2)]
        ub = [es.enter_context(nc.sbuf_tensor(f"scu{i}", [128, 8, W], BF16)) for i in range(2)]
        pb = [es.enter_context(nc.psum_tensor(f"scp{i}", [128, 512], F32)) for i in range(8)]
        for q in range(2):
            dma(S, 'pool', win[:, :, q * 1536:(q + 1) * 1536], wview(io['sc_in_w'])[:, :, q * 1536:(q + 1) * 1536],
                [], [('win', q)], f"win{q}")
        dma(S, 'pool', wout[:], wview(io['sc_out_w']), [], ['wout'], "wout")
        dma(S, 'sp', cw[:], io['sc_convT'], [], ['cw'], "cw")
        tiles = [(i * W, 0, 0, SEQ) for i in range(SEQ // W)] + [(SEQ, 1, SEQ, T)]
        for ti, (t0, s, slo, shi) in enumerate(tiles):
            sl = ti % 2
            lo = max(t0 - 1, slo)
            hi = min(t0 + W + 1, shi)
            off = lo - (t0 - 1)
            n = hi - lo
            hk = ('h', sl)
            ak = ('a', sl)
            tmp = dict(sq=sq, xn=xn, rs=rs, ps=pb[7], psk=('ps', 7))
            prenorm(S, g, hview(src)[:, :, lo:hi], hb[sl][:, :, off:off + n], hk, ab[sl][:, :, off:off + n], ak,
                    tmp, l, 0, s, n, f"hld{sl}")
            uk = ('u', sl)
            for c in range(8):
                ps3 = [pb[(c % 2) * 3 + i] for i in range(3)]
                pk = [('ps', (c % 2) * 3 + i) for i in range(3)]
                for i in range(3):
                    col = i * 1024 + c * 128
                    q = col // 1536
                    for kc in range(8):
                        mm(S, ps3[i][:, off:off + n], win[:, kc, col:col + 128], ab[sl][:, kc, off:off + n],
                           kc == 0, kc == 7, [('win', q), ak], [pk[i]])
                ys = c % 2
                yk = ('y', ys)
                act(S, xv[ys][:, off:off + n], ps3[2][:, off:off + n], AF.Copy, [pk[2]], [('xv', ys)])
                tt(S, 'dve', yb[ys][:, off:off + n], ps3[1][:, off:off + n], xv[ys][:, off:off + n], ALU.mult,
                   [pk[1], ('xv', ys)], [yk])
                if off == 1:
                    mset(S, 'pool', yb[ys][:, 0:1], 0.0, [], [yk])
                if off + n < W + 2:
                    mset(S, 'pool', yb[ys][:, W + 1:W + 2], 0.0, [], [yk])
                ck = ('cv', ys)
                act(S, cv[ys][:], yb[ys][:, 0:W], AF.Identity, [yk], [ck], scale=cw[:, c, 0:1])
                stt(S, cv[ys][:], yb[ys][:, 1:W + 1], cw[:, c, 1:2], cv[ys][:], ALU.mult, ALU.add, [yk, ck, 'cw'], [ck])
                stt(S, cv[ys][:], yb[ys][:, 2:W + 2], cw[:, c, 2:3], cv[ys][:], ALU.mult, ALU.add, [yk, ck, 'cw'], [ck])
                tt(S, 'dve', ub[sl][:, c, :], ps3[0][:, 1:W + 1], cv[ys][:], ALU.mult, [pk[0], ck], [uk])
            for m in range(8):
                po = pb[6 + (m % 2)]
                pok = ('ps', 6 + (m % 2))
                for c in range(8):
                    mm(S, po[:, :W], wout[:, c, m * 128:(m + 1) * 128], ub[sl][:, c, :], c == 0, c == 7,
                       ['wout', uk], [pok])
                stt(S, hb[sl][:, m, 1:W + 1], po[:, :W], MV(g, l, 2, m, s), hb[sl][:, m, 1:W + 1], ALU.mult, ALU.add,
                    [pok, hk], [hk])
            dma(S, 'sp', hview(dst)[:, :, t0:t0 + W], hb[sl][:, :, 1:W + 1], [hk], [], f"hst{sl}")
        S.emit(nc, *sems)


def phase_ffn(nc, g, io, sems, src, dst, l, moe, with_ctx):
    nc = _NCW(nc)
    S = Sched()
    S.use_sched = False
    f = l // 2
    E = NE if moe else 1
    NG = int(os.environ.get('KNG', FF // 256))
    MAXW = 1280 if with_ctx else 1536
    NS = 4
    with ExitStack() as es:
        hS = es.enter_context(nc.sbuf_tensor("fh", [128, 8, MAXW], F32))
        aS = es.enter_context(nc.sbuf_tensor("fa", [128, 8, MAXW], BF16))
        w1r = [es.enter_context(nc.sbuf_tensor(f"fw1_{i}", [128, 8, 256], BF16)) for i in range(NS)]
        w3r = [es.enter_context(nc.sbuf_tensor(f"fw3_{i}", [128, 8, 256], BF16)) for i in range(NS)]
        w2r = [es.enter_context(nc.sbuf_tensor(f"fw2_{i}", [128, 2, 1024], BF16)) for i in range(NS)]
        sq = es.enter_context(nc.sbuf_tensor("fsq", [128, 8, 512], BF16))
        xn = es.enter_context(nc.sbuf_tensor("fxn", [128, 8, 512], F32))
        rs = es.enter_context(nc.sbuf_tensor("frs", [128, 512], F32))
        s1 = [es.enter_context(nc.sbuf_tensor(f"fs1_{i}", [128, 512], BF16)) for i in range(2)]
        h1 = [es.enter_context(nc.sbuf_tensor(f"fh1_{i}", [128, 2, 512], BF16)) for i in range(2)]
        pb = [es.enter_context(nc.psum_tensor(f"fp{i}", [128, 512], F32)) for i in range(8)]
        if moe:
            gB = es.enter_context(nc.sbuf_tensor("fgB", [128, NE, MAXW], BF16))
            p3g = [es.enter_context(nc.sbuf_tensor(f"fp3g_{i}", [128, 512], BF16)) for i in range(2)]
            yt = [es.enter_context(nc.sbuf_tensor(f"fyt_{i}", [128, 512], F32)) for i in range(2)]
            wr = es.enter_context(nc.sbuf_tensor("fwr", [128, 8, NE], F32))
            brt = es.enter_context(nc.sbuf_tensor("fbr", [128, NE], F32))
            lg = es.enter_context(nc.sbuf_tensor("flg", [128, 4, NE], F32))
            mx = es.enter_context(nc.sbuf_tensor("fmx", [128, 4, 8], F32))
            nt1 = es.enter_context(nc.sbuf_tensor("fnt1", [128, 4], F32))
            msk = es.enter_context(nc.sbuf_tensor("fmsk", [128, 4, NE], F32))
            ex = es.enter_context(nc.sbuf_tensor("fex", [128, 4, NE], F32))
            den = es.enter_context(nc.sbuf_tensor("fden", [128, 4], F32))
            gt = es.enter_context(nc.sbuf_tensor("fgt", [128, 4, NE], F32))
            gbc = es.enter_context(nc.sbuf_tensor("fgbc", [128, 4, NE, 128], F32))
            dma(S, 'sp', wr[:], io['moe_rwT'][f], [], ['wr'], "wr")
            dma(S, 'sp', brt[:], io['moe_rb'][f].partition_broadcast(128), [], ['br'], "br")
            W1 = io['moe_w1'][f]
            W3 = io['moe_w3'][f]
            W2 = io['moe_w2'][f]
        else:
            W1 = [io['ffn_w1'][f]]
            W3 = [io['ffn_w3'][f]]
            W2 = [io['ffn_w2'][f]]
        lat = [(i * 512, 512, 0) for i in range(8)]
        if with_ctx:
            supers = [[lat[2 * i], lat[2 * i + 1]] for i in range(4)]
            supers[3].append((SEQ, CTXL, 1))
        else:
            supers = [[lat[0], lat[1], lat[2]], [lat[3], lat[4], lat[5]], [lat[6], lat[7]]]
        supers = supers[:int(os.environ.get('KSUP', 4))]
        nld = 0
        it = 0
        PF = 2
        gseq = [(si, e_, gi) for si in range(len(supers)) for e_ in range(E) for gi in range(NG)]
        gpos = {k: i for i, k in enumerate(gseq)}
        issued = [0]

        def issue_upto(n):
            while issued[0] < min(n, len(gseq)):
                _, ee, gg = gseq[issued[0]]
                sl_ = issued[0] % NS
                jj0 = gg * 256
                dma(S, 'pool', w1r[sl_][:], wview(W1[ee])[:, :, jj0:jj0 + 256], [], [('w1', sl_)], f"w1_{sl_}")
                dma(S, 'pool', w3r[sl_][:], wview(W3[ee])[:, :, jj0:jj0 + 256], [], [('w3', sl_)], f"w3_{sl_}")
                dma(S, 'pool', w2r[sl_][:], W2[ee][jj0:jj0 + 256, :].rearrange("(j p) m -> p j m", p=128), [],
                    [('w2', sl_)], f"w2_{sl_}")
                issued[0] += 1

        issue_upto(PF)
        for si_, st in enumerate(supers):
            offs = []
            o_ = 0
            for (t0, W, s) in st:
                offs.append(o_)
                o_ += W
            for (t0, W, s), off in zip(st, offs):
                hk = [('h', off, m_) for m_ in range(8)]
                ak = ('a', off)
                tmp = dict(sq=sq, xn=xn, rs=rs, ps=pb[7], psk=('ps', 7))
                if not moe:
                    prenorm(S, g, hview(src)[:, :, t0:t0 + W], hS[:, :, off:off + W], hk, aS[:, :, off:off + W], ak,
                            tmp, l, 1, s, W, f"hld{off}")
                else:
                    prenorm(S, g, hview(src)[:, :, t0:t0 + W], hS[:, :, off:off + W], hk, aS[:, :, off:off + W], ak,
                            tmp, l, 1, s, W, f"hld{off}", a32=xn[:, :, :W], a32k='xn')
                    nb = W // 128
                    for b in range(nb):
                        for kc in range(8):
                            mm(S, pb[6][:, b * 8:(b + 1) * 8], xn[:, kc, b * 128:(b + 1) * 128], wr[:, kc, :],
                               kc == 0, kc == 7, ['xn', 'wr'], [('ps', 6)])
                    tt(S, 'dve', lg[:, :nb, :], pb[6][:, :nb * 8].rearrange("p (b e) -> p b e", e=8),
                       brt[:].unsqueeze(1).to_broadcast([128, nb, NE]), ALU.add, [('ps', 6), 'br'], ['lg'])
                    for b in range(nb):
                        S.add('dve', (lambda o_, i_: (lambda e: e.max(out=o_, in_=i_)))(mx[:, b, :], lg[:, b, :]),
                              ['lg'], ['mx'])
                    ts(S, 'dve', nt1[:, :nb], mx[:, :nb, 0], -1.0, None, ALU.mult, None, ['mx'], ['nt1'])
                    for b in range(nb):
                        ts(S, 'dve', msk[:, b, :], lg[:, b, :], mx[:, b, 1:2], None, ALU.is_ge, None, ['lg', 'mx'], ['msk'])
                        act(S, ex[:, b, :], lg[:, b, :], AF.Exp, ['lg', 'nt1'], ['ex'], bias=nt1[:, b:b + 1], scale=1.0)
                    tt(S, 'dve', ex[:, :nb, :], ex[:, :nb, :], msk[:, :nb, :], ALU.mult, ['ex', 'msk'], ['ex'])
                    S.add('dve', (lambda o_, i_: (lambda e: e.tensor_reduce(out=o_, in_=i_, axis=AX.X, op=ALU.add)))(
                        den[:, :nb], ex[:, :nb, :]), ['ex'], ['den'])
                    recip(S, den[:, :nb], den[:, :nb], ['den'], ['den'])
                    tt(S, 'dve', gt[:, :nb, :], ex[:, :nb, :], den[:, :nb].unsqueeze(2).to_broadcast([128, nb, NE]),
                       ALU.mult, ['ex', 'den'], ['gt'])
                    cp(S, 'pool', gbc[:, :nb], gt[:, :nb, :].unsqueeze(3).to_broadcast([128, nb, NE, 128]), ['gt'], ['gbc'])
                    for e_ in range(NE):
                        pg = pb[4 + (e_ % 2)]
                        pgk = ('ps', 4 + (e_ % 2))
                        for b in range(nb):
                            mm(S, pg[:, b * 128:(b + 1) * 128], gbc[:, b, e_, :], g.ident[:], True, True,
                               ['gbc', 'ident'], [pgk])
                        act(S, gB[:, e_, off:off + W], pg[:, :W], AF.Copy, [pgk], [('gB', off)])
            steps = []
            for e_ in range(E):
                for gi in range(NG):
                    gidx = gpos[(si_, e_, gi)]
                    slot = gidx % NS
                    j0 = gi * 256
                    first = True
                    for (t0, W, s), off in zip(st, offs):
                        hs = it % 2
                        it += 1

                        def A(e_=e_, slot=slot, j0=j0, first=first, W=W, s=s, off=off, hs=hs, gidx=gidx):
                            if first:
                                issue_upto(gidx + PF + 1)
                            ak = ('a', off)
                            h1k = ('h1', hs)
                            for jj in range(2):
                                ss = jj
                                p1 = pb[jj * 2]
                                p3 = pb[jj * 2 + 1]
                                p1k = ('ps', jj * 2)
                                p3k = ('ps', jj * 2 + 1)
                                for kc in range(8):
                                    mm(S, p1[:, :W], w1r[slot][:, kc, jj * 128:(jj + 1) * 128], aS[:, kc, off:off + W],
                                       kc == 0, kc == 7, [('w1', slot), ak], [p1k])
                                for kc in range(8):
                                    mm(S, p3[:, :W], w3r[slot][:, kc, jj * 128:(jj + 1) * 128], aS[:, kc, off:off + W],
                                       kc == 0, kc == 7, [('w3', slot), ak], [p3k])
                                act(S, s1[ss][:, :W], p1[:, :W], AF.Silu, [p1k], [('s1', ss)])
                                if moe:
                                    tt(S, 'dve', p3g[ss][:, :W], p3[:, :W], gB[:, e_, off:off + W], ALU.mult,
                                       [p3k, ('gB', off)], [('p3g', ss)])
                                    tt(S, 'pool', h1[hs][:, jj, :W], s1[ss][:, :W], p3g[ss][:, :W], ALU.mult,
                                       [('s1', ss), ('p3g', ss)], [h1k])
                                else:
                                    tt(S, 'dve', h1[hs][:, jj, :W], p3[:, :W], s1[ss][:, :W], ALU.mult,
                                       [p3k, ('s1', ss)], [h1k])

                        def B(slot=slot, W=W, s=s, off=off, hs=hs):
                            h1k = ('h1', hs)
                            for m in range(8):
                                po = pb[4 + (m % 4)]
                                pok = ('ps', 4 + (m % 4))
                                for jj in range(2):
                                    mm(S, po[:, :W], w2r[slot][:, jj, m * 128:(m + 1) * 128], h1[hs][:, jj, :W],
                                       jj == 0, jj == 1, [('w2', slot), h1k], [pok])
                                hk = ('h', off, m)
                                if m % 2 == 0 or not moe:
                                    stt(S, hS[:, m, off:off + W], po[:, :W], MV(g, l, 5, m, s), hS[:, m, off:off + W],
                                        ALU.mult, ALU.add, [pok, hk], [hk])
                                else:
                                    ys = (m // 2) % 2
                                    act(S, yt[ys][:, :W], po[:, :W], AF.Identity, [pok], [('yt', ys)], scale=MV(g, l, 5, m, s))
                                    tt(S, 'pool', hS[:, m, off:off + W], hS[:, m, off:off + W], yt[ys][:, :W], ALU.add,
                                       [('yt', ys), hk], [hk])

                        steps.append((A, B))
                        first = False
            pipeline(steps, 1)
            for (t0, W, s), off in zip(st, offs):
                dma(S, 'sp', hview(dst)[:, :, t0:t0 + W], hS[:, :, off:off + W], [('h', off, m_) for m_ in range(8)], [], f"hst{off}")
        S.emit(nc, *sems)


def phase_gmlp(nc, g, io, sems, src, dst, l=2):
    nc = _NCW(nc)
    S = Sched()
    S.use_sched = False
    with ExitStack() as es:
        win = es.enter_context(nc.sbuf_tensor("cmwin", [128, 8, 4096], BF16))
        wout = es.enter_context(nc.sbuf_tensor("cmwout", [128, 16, 1024], BF16))
        wsT = es.enter_context(nc.sbuf_tensor("cmws", [128, 8, 128], BF16))
        bU = es.enter_context(nc.sbuf_tensor("cmbu", [128, 16], F32))
        bV = es.enter_context(nc.sbuf_tensor("cmbv", [128, 2048], F32))
        vg = es.enter_context(nc.sbuf_tensor("cmvg", [128, 16], F32))
        bsT = es.enter_context(nc.sbuf_tensor("cmbs", [128, 8, 128], F32))
        hb = [es.enter_context(nc.sbuf_tensor(f"cmh{i}", [128, 8, 512], F32)) for i in range(1)]
        ab = [es.enter_context(nc.sbuf_tensor(f"cma{i}", [128, 8, 512], BF16)) for i in range(1)]
        xn = es.enter_context(nc.sbuf_tensor("cmxn", [128, 8, 512], F32))
        rs = es.enter_context(nc.sbuf_tensor("cmrs", [128, 512], F32))
        vt = [es.enter_context(nc.sbuf_tensor(f"cmvt{i}", [128, 512], F32)) for i in range(2)]
        gv = es.enter_context(nc.sbuf_tensor("cmgv", [128, 2048], F32))
        junk = es.enter_context(nc.sbuf_tensor("cmjunk", [128, 512], BF16))
        ss = es.enter_context(nc.sbuf_tensor("cmss", [128, 4], F32))
        rv = es.enter_context(nc.sbuf_tensor("cmrv", [128, 1], F32))
        vn = [es.enter_context(nc.sbuf_tensor(f"cmvn{i}", [128, 2048], BF16)) for i in range(4)]
        ub = [es.enter_context(nc.sbuf_tensor(f"cmu{i}", [128, 512], F32)) for i in range(2)]
        m1 = [es.enter_context(nc.sbuf_tensor(f"cmm{i}", [128, 512], F32)) for i in range(2)]
        pr = es.enter_context(nc.sbuf_tensor("cmpr", [128, 16, 512], BF16))
        pb = [es.enter_context(nc.psum_tensor(f"cmp{i}", [128, 512], F32)) for i in range(8)]
        for q in range(4):
            dma(S, 'pool', win[:, :, q * 1024:(q + 1) * 1024], wview(io['cm_in_w'])[:, :, q * 1024:(q + 1) * 1024],
                [], [('win', q)], f"win{q}")
        dma(S, 'pool', wout[:], io['cm_out_w'].rearrange("(c p) m -> p c m", p=128), [], ['wout'], "wout")
        dma(S, 'pool', wsT[:], io['cm_wsT'], [], ['wsT'], "wsT")
        dma(S, 'sp', bU[:], io['cm_buT'], [], ['bU'], "bU")
        dma(S, 'sp', bV[:], io['cm_bv'].partition_broadcast(128), [], ['bV'], "bV")
        dma(S, 'sp', vg[:], io['cm_vgT'], [], ['vg'], "vg")
        dma(S, 'sp', bsT[:], io['cm_bsf'].partition_broadcast(128).rearrange("p o (g q) -> p (o g) q", g=8), [], ['bsT'], "bsT")
        tiles = [(i * 512, 512, 0) for i in range(8)] + [(SEQ, CTXL, 1)]
        nv = 0
        nu = 0
        for ti, (t0, W, s) in enumerate(tiles):
            sl = 0
            hk = ('h', sl)
            ak = ('a', sl)
            tmp = dict(sq=pr[:, 0:8, :], sqk='pr', xn=xn, rs=rs, ps=pb[7], psk=('ps', 7))
            prenorm(S, g, hview(src)[:, :, t0:t0 + W], hb[sl][:, :, :W], hk, ab[sl][:, :, :W], ak, tmp, l, 0, s, W,
                    f"hld{sl}")
            nb = W // 128
            for b in range(nb):
                for q4 in range(4):
                    pv = pb[nv % 2]
                    pvk = ('ps', nv % 2)
                    vts = nv % 2
                    nv += 1
                    col = 2048 + q4 * 512
                    for kc in range(8):
                        mm(S, pv[:], ab[sl][:, kc, b * 128:(b + 1) * 128], win[:, kc, col:col + 512], kc == 0, kc == 7,
                           [ak, ('win', col // 1024)], [pvk])
                    tt(S, 'dve', vt[vts][:], pv[:], bV[:, q4 * 512:(q4 + 1) * 512], ALU.add, [pvk, 'bV'], [('vt', vts)])
                    act(S, gv[:, q4 * 512:(q4 + 1) * 512], vt[vts][:], AF.Gelu_apprx_tanh, [('vt', vts)], ['gv'])
                    act(S, junk[:], gv[:, q4 * 512:(q4 + 1) * 512], AF.Square, ['gv'], ['junk', 'ss'],
                        accum=ss[:, q4:q4 + 1])
                S.add('dve', (lambda o_, i_: (lambda e: e.tensor_reduce(out=o_, in_=i_, axis=AX.X, op=ALU.add)))(
                    rv[:], ss[:]), ['ss'], ['rv'])
                act(S, rv[:], rv[:], AF.Sqrt, ['rv'], ['rv'], bias=g.epsb[:, 0:1], scale=1.0 / 2048)
                recip(S, rv[:], rv[:], ['rv'], ['rv'])
                ts(S, 'dve', vn[b][:], gv[:], rv[:, 0:1], None, ALU.mult, None, ['gv', 'rv'], [('vn', b)])
            for cu in range(16):
                pu = pb[2 + (nu % 2)]
                puk = ('ps', 2 + (nu % 2))
                psv = pb[4 + (nu % 2)]
                psk = ('ps', 4 + (nu % 2))
                us = nu % 2
                nu += 1
                gq = cu // 2
                for kc in range(8):
                    mm(S, pu[:, :W], win[:, kc, cu * 128:(cu + 1) * 128], ab[sl][:, kc, :W], kc == 0, kc == 7,
                       [ak, ('win', (cu * 128) // 1024)], [puk])
                act(S, ub[us][:, :W], pu[:, :W], AF.Gelu_apprx_tanh, [puk, 'bU'], [('ub', us)], bias=bU[:, cu:cu + 1])
                for b in range(nb):
                    mm(S, psv[:, b * 128:(b + 1) * 128], vn[b][:, cu * 128:(cu + 1) * 128], wsT[:, gq, :], True, True,
                       [('vn', b), 'wsT'], [psk])
                stt(S, m1[us][:, :W].rearrange("p (b q) -> p b q", q=128),
                    psv[:, :W].rearrange("p (b q) -> p b q", q=128), vg[:, cu:cu + 1],
                    bsT[:, gq, :].unsqueeze(1).to_broadcast([128, nb, 128]), ALU.mult, ALU.add,
                    [psk, 'vg', 'bsT'], [('m1', us)])
                tt(S, 'pool', pr[:, cu, :W], ub[us][:, :W], m1[us][:, :W], ALU.mult, [('ub', us), ('m1', us)], ['pr'])
            for m in range(8):
                po = pb[6 + (m % 2)]
                pok = ('ps', 6 + (m % 2))
                for cu in range(16):
                    mm(S, po[:, :W], wout[:, cu, m * 128:(m + 1) * 128], pr[:, cu, :W], cu == 0, cu == 15,
                       ['wout', 'pr'], [pok])
                stt(S, hb[sl][:, m, :W], po[:, :W], MV(g, l, 2, m, s), hb[sl][:, m, :W], ALU.mult, ALU.add,
                    [pok, hk], [hk])
            dma(S, 'sp', hview(dst)[:, :, t0:t0 + W], hb[sl][:, :, :W], [hk], [], f"hst{sl}")
        S.emit(nc, *sems)


SWA_PERM = [0, 4, 1, 5, 2, 6, 3, 7, 8, 12, 9, 13, 10, 14, 11, 15]


def phase_qkv(nc, g, io, sems, src, l, kind, scr):
    nc = _NCW(nc)
    S = Sched()
    if kind == 'diff':
        wq, NQC, NKC, VF = io['da_qkv_w'], 8, 8, 1024
        gname = 'da_qkg'
    else:
        wq, NQC, NKC, VF = io['sw_qkv_wp'], 8, 2, 256
        gname = 'sw_qkg'
    NC_ = NQC + NKC
    WCOLS = NC_ * 128 + VF
    with ExitStack() as es:
        win = es.enter_context(nc.sbuf_tensor("qw", [128, 8, WCOLS], BF16))
        gq = es.enter_context(nc.sbuf_tensor("qg", [128, 2], F32))
        RT = es.enter_context(nc.sbuf_tensor("qRT", [128, 128], BF16))
        bones = es.enter_context(nc.sbuf_tensor("qbo", [128, 128], BF16))
        cs = [es.enter_context(nc.sbuf_tensor(f"qcs{i}", [128, 2, 512], F32)) for i in range(2)]
        hb = [es.enter_context(nc.sbuf_tensor(f"qh{i}", [128, 8, 512], F32)) for i in range(2)]
        ab = [es.enter_context(nc.sbuf_tensor(f"qa{i}", [128, 8, 512], BF16)) for i in range(2)]
        sq = es.enter_context(nc.sbuf_tensor("qsq", [128, 8, 512], BF16))
        xn = es.enter_context(nc.sbuf_tensor("qxn", [128, 8, 512], F32))
        rs = es.enter_context(nc.sbuf_tensor("qrs", [128, 512], F32))
        xg = [es.enter_context(nc.sbuf_tensor(f"qxg{i}", [128, 512], BF16)) for i in range(2)]
        xs = [es.enter_context(nc.sbuf_tensor(f"qxs{i}", [128, 512], BF16)) for i in range(2)]
        rd = [es.enter_context(nc.sbuf_tensor(f"qrd{i}", [128, 512], F32)) for i in range(2)]
        t1 = [es.enter_context(nc.sbuf_tensor(f"qt1{i}", [128, 512], F32)) for i in range(2)]
        t2 = [es.enter_context(nc.sbuf_tensor(f"qt2{i}", [128, 512], F32)) for i in range(2)]
        ob = [es.enter_context(nc.sbuf_tensor(f"qo{i}", [128, 512], BF16)) for i in range(3)]
        vb = [es.enter_context(nc.sbuf_tensor(f"qv{i}", [128, 512], BF16)) for i in range(2)]
        pb = [es.enter_context(nc.psum_tensor(f"qp{i}", [128, 512], F32)) for i in range(8)]
        nq = (WCOLS + 1023) // 1024
        for q in range(nq):
            c1 = min(WCOLS, (q + 1) * 1024)
            dma(S, 'pool', win[:, :, q * 1024:c1], wview(wq)[:, :, q * 1024:c1], [], [('win', q)], f"win{q}")
        dma(S, 'sp', gq[:], io[gname], [], ['gq'], "gq")
        dma(S, 'pool', RT[:], io['ropeRT'], [], ['RT'], "RT")
        dma(S, 'pool', bones[:], io['blkones'], [], ['bones'], "bones")
        if kind == 'diff':
            ts(S, 'dve', gq[:, 0:1], gq[:, 0:1], 0.125, None, ALU.mult, None, ['gq'], ['gq'])
        else:
            ts(S, 'dve', gq[:, 0:1], gq[:, 0:1], 0.125, None, ALU.mult, None, ['gq'], ['gq'])
        tiles = [(i * 512, 512, 0) for i in range(8)] + [(SEQ, CTXL, 1)]
        n_ = 0
        no = 0
        nvv = 0
        for ti, (t0, W, s) in enumerate(tiles):
            sl = ti % 2
            hk = ('h', sl)
            ak = ('a', sl)
            tmp = dict(sq=sq, xn=xn, rs=rs, ps=pb[7], psk=('ps', 7))
            prenorm(S, g, hview(src)[:, :, t0:t0 + W], hb[sl][:, :, :W], hk, ab[sl][:, :, :W], ak, tmp, l, 0, s, W,
                    f"hld{sl}")
            dma(S, 'sp', cs[sl][:, :, :W], io['ropecs'][:, :, t0:t0 + W], [], [('cs', sl)], f"cs{sl}")
            chunks = list(range(NC_))
            if kind == 'swa' and s == 1:
                chunks = list(range(NQC, NC_))
            steps = []
            for ch in chunks:
                k2 = n_ % 2
                n_ += 1
                o3 = no % 3
                no += 1

                def A(ch=ch, k2=k2, W=W, sl=sl):
                    isq = ch < NQC
                    pp = pb[k2 * 3]
                    ppk = ('ps', k2 * 3)
                    col = ch * 128
                    for kc in range(8):
                        mm(S, pp[:, :W], win[:, kc, col:col + 128], ab[sl][:, kc, :W], kc == 0, kc == 7,
                           [('a', sl), ('win', col // 1024)], [ppk])
                    gcol = gq[:, 0:1] if isq else gq[:, 1:2]
                    act(S, xg[k2][:, :W], pp[:, :W], AF.Identity, [ppk, 'gq'], [('xg', k2)], scale=gcol)
                    act(S, xs[k2][:, :W], pp[:, :W], AF.Square, [ppk], [('xs', k2)])

                def B(ch=ch, k2=k2, o3=o3, W=W, sl=sl, t0=t0):
                    isq = ch < NQC
                    pm = pb[k2 * 3 + 1]
                    pr_ = pb[k2 * 3 + 2]
                    pmk, prk = ('ps', k2 * 3 + 1), ('ps', k2 * 3 + 2)
                    mm(S, pm[:, :W], bones[:], xs[k2][:, :W], True, True, [('xs', k2), 'bones'], [pmk])
                    mm(S, pr_[:, :W], RT[:], xg[k2][:, :W], True, True, [('xg', k2), 'RT'], [prk])
                    act(S, rd[k2][:, :W], pm[:, :W], AF.Sqrt, [pmk], [('rd', k2)], bias=g.epsb[:, 0:1], scale=1.0 / 64)
                    recip(S, rd[k2][:, :W], rd[k2][:, :W], [('rd', k2)], [('rd', k2)])
                    tt(S, 'pool', t1[k2][:, :W], xg[k2][:, :W], cs[sl][:, 0, :W], ALU.mult, [('xg', k2), ('cs', sl)], [('t1', k2)])
                    tt(S, 'dve', t2[k2][:, :W], pr_[:, :W], cs[sl][:, 1, :W], ALU.mult, [prk, ('cs', sl)], [('t2', k2)])
                    tt(S, 'pool', t1[k2][:, :W], t1[k2][:, :W], t2[k2][:, :W], ALU.add, [('t1', k2), ('t2', k2)], [('t1', k2)])
                    tt(S, 'dve', ob[o3][:, :W], t1[k2][:, :W], rd[k2][:, :W], ALU.mult, [('t1', k2), ('rd', k2)], [('ob', o3)])
                    dst_ = scr['QT'] if isq else scr['KT']
                    cc = ch if isq else ch - NQC
                    dma(S, 'sp', dst_[cc * 128:(cc + 1) * 128, t0:t0 + W], ob[o3][:, :W], [('ob', o3)], [], f"qst{o3}")

                steps.append((A, B))
            pipeline(steps, 1)
            for b in range(W // 128):
                for vq in range((VF + 511) // 512):
                    vw = min(512, VF - vq * 512)
                    v2 = nvv % 2
                    nvv += 1
                    pv = pb[6]
                    col = NC_ * 128 + vq * 512
                    for kc in range(8):
                        mm(S, pv[:, :vw], ab[sl][:, kc, b * 128:(b + 1) * 128], win[:, kc, col:col + vw], kc == 0, kc == 7,
                           [ak, ('win', col // 1024), ('win', (col + vw - 1) // 1024)], [('ps', 6)])
                    act(S, vb[v2][:, :vw], pv[:, :vw], AF.Copy, [('ps', 6)], [('vb', v2)])
                    r0 = t0 + b * 128
                    dma(S, 'sp', scr['V'][r0:r0 + 128, vq * 512:vq * 512 + vw], vb[v2][:, :vw], [('vb', v2)], [], f"vst{v2}")
        S.emit(nc, *sems)


def phase_att(nc, g, io, sems, src, dst, l, kind, scr):
    nc = _NCW(nc)
    S = Sched()
    NB = T // 128
    if kind == 'diff':
        NKC, VF = 8, 1024
    else:
        NKC, VF = 2, 256
    with ExitStack() as es:
        KT = es.enter_context(nc.sbuf_tensor("aKT", [128, NKC, T], BF16))
        V = es.enter_context(nc.sbuf_tensor("aV", [128, NB, VF], BF16))
        if kind == 'diff':
            wout = es.enter_context(nc.sbuf_tensor("awo", [128, 8, 1024], BF16))
            lp = es.enter_context(nc.sbuf_tensor("alp", [128, 4, 64], F32))
            pr2 = es.enter_context(nc.sbuf_tensor("apr2", [128, 2, 64], F32))
            s2 = es.enter_context(nc.sbuf_tensor("as2", [128, 2], F32))
            nlam = es.enter_context(nc.sbuf_tensor("anl", [128, 1], F32))
            sg = es.enter_context(nc.sbuf_tensor("asg", [128, 1], F32))
            OT = es.enter_context(nc.sbuf_tensor("aOT", [128, 8, 512], BF16))
            La = [[es.enter_context(nc.sbuf_tensor(f"aLa{r}{q}", [128, 512], F32)) for q in range(2)] for r in range(2)]
            onesf = es.enter_context(nc.sbuf_tensor("aonesf", [128, 128], F32))
        else:
            wout = es.enter_context(nc.sbuf_tensor("awo", [64, 16, 1024], BF16))
            snk = es.enter_context(nc.sbuf_tensor("asnk", [128, 16], F32))
            msk = es.enter_context(nc.sbuf_tensor("amsk", [128, 6, 512], BF16))
            identb = es.enter_context(nc.sbuf_tensor("aidb", [128, 128], BF16))
            OT = es.enter_context(nc.sbuf_tensor("aOT", [64, 16, 512], BF16))
        NSL = 1 if kind == 'diff' else 2
        if kind == 'diff':
            QZ = es.enter_context(nc.sbuf_tensor("aQZ", [128, 8, 1024], BF16))
            qz4 = QZ[:].rearrange("p c (r w) -> p c r w", r=2)
            hv = QZ[:].bitcast(F32)
            QT = hb = None
        else:
            QT = [es.enter_context(nc.sbuf_tensor(f"aQ{i}", [128, 8, 1024], BF16)) for i in range(NSL)]
            qzs = [q_[:].rearrange("p c (r w) -> p c r w", r=2) for q_ in QT]
            hb = [es.enter_context(nc.sbuf_tensor(f"ah{i}", [128, 8, 512], F32)) for i in range(NSL)]
        P = [es.enter_context(nc.sbuf_tensor(f"aP{i}", [128, 512], BF16)) for i in range(4)]
        PM = [es.enter_context(nc.sbuf_tensor(f"aPM{i}", [128, 512], BF16)) for i in range(2)] if kind != 'diff' else None
        r0b = es.enter_context(nc.sbuf_tensor("ar0", [128, 512], F32))
        r1b = es.enter_context(nc.sbuf_tensor("ar1", [128, 512], F32))
        o0 = es.enter_context(nc.sbuf_tensor("ao0", [128, 512], F32))
        o1 = r1b
        osq = es.enter_context(nc.sbuf_tensor("aosq", [128, 512], BF16))
        pb = [es.enter_context(nc.psum_tensor(f"ap{i}", [128, 512], F32)) for i in range(8)]
        for c in range(NKC):
            dma(S, 'sp', KT[:, c, :], scr['KT'][c * 128:(c + 1) * 128, :], [], [('KT', c)], f"kt{c % 4}")
        for q in range(4):
            b0, b1 = q * 9, min(NB, (q + 1) * 9)
            dma(S, 'sp', V[:, b0:b1, :], scr['V'][b0 * 128:b1 * 128, :VF].rearrange("(b p) f -> p b f", p=128),
                [], [('V', q)], f"v{q}")
        if kind == 'diff':
            dma(S, 'pool', wout[:], wview(io['da_out_w']), [], ['wout'], "wout")
            mset(S, 'pool', onesf[:], 1.0, [], ['onesf'])
            dma(S, 'sp', lp[:], io['da_lam'].partition_broadcast(128).rearrange("p o (a d) -> p (o a) d", a=4), [], ['lp'], "lp")
            dma(S, 'sp', sg[:], io['da_subg'], [], ['sg'], "sg")
            lam_init = 0.8 - 0.6 * math.exp(-0.3 * l)
            for i in range(2):
                tt(S, 'dve', pr2[:, i, :], lp[:, 2 * i, :], lp[:, 2 * i + 1, :], ALU.mult, ['lp'], ['pr2'])
            S.add('dve', (lambda o_, i_: (lambda e: e.tensor_reduce(out=o_, in_=i_, axis=AX.X, op=ALU.add)))(
                s2[:], pr2[:]), ['pr2'], ['s2'])
            act(S, s2[:], s2[:], AF.Exp, ['s2'], ['s2'])
            tt(S, 'dve', nlam[:], s2[:, 1:2], s2[:, 0:1], ALU.subtract, ['s2'], ['nlam'])
            ts(S, 'dve', nlam[:], nlam[:], -lam_init, None, ALU.add, None, ['nlam'], ['nlam'])
            ts(S, 'dve', sg[:], sg[:], 1.0 - lam_init, None, ALU.mult, None, ['sg'], ['sg'])
            tiles = [(i * 512, 512, 0) for i in range(8)] + [(SEQ, CTXL, 1)]
        else:
            dma(S, 'pool', wout[:], io['sw_out_wp'].rearrange("(h p) m -> p h m", p=64), [], ['wout'], "wout")
            dma(S, 'sp', snk[:], io['sw_sinkp'].partition_broadcast(128), [], ['snk'], "snk")
            dma(S, 'pool', msk[:], io['swa_mask'], [], ['msk'], "msk")
            dma(S, 'pool', identb[:], io['ident'], [], ['identb'], "identb")
            act(S, snk[:], snk[:], AF.Exp, ['snk'], ['snk'])
            for i_ in range(NSL):
                mset(S, 'pool', QT[i_][:], 0.0, [], [('Q', i_, 0), ('Q', i_, 1)])
            tiles = [(i * 512, 512, 0) for i in range(8)]
        np_ = 0
        for ti, (t0, W, s) in enumerate(tiles):
            sl = ti % NSL
            hk = ('h', sl)
            qk = ('Q', sl)
            if kind == 'diff':
                qsrc = scr['QT'].rearrange("(c p) t -> p c t", p=128)
                for r in range(2):
                    z0 = (1 - r) * 64
                    mset(S, 'pool', qz4[z0:z0 + 64, :, r, :W], 0.0, [], [('QZ', r)])
                    dma(S, 'sp', qz4[r * 64:(r + 1) * 64, :, r, :W], qsrc[r * 64:(r + 1) * 64, :, t0:t0 + W], [],
                        [('QZ', r)], f"qld{r}")
            else:
                dma(S, 'sp', hb[sl][:, :, :W], hview(src)[:, :, t0:t0 + W], [], [hk], f"hld{sl}")
                qsrc = scr['QT'].rearrange("(c p) t -> p c t", p=128)
                for r in range(2):
                    dma(S, 'sp', qzs[sl][r * 64:(r + 1) * 64, :, r, :W], qsrc[r * 64:(r + 1) * 64, :, t0:t0 + W], [],
                        [('Q', sl, r)], f"qld{sl}_{r}")
            if kind == 'diff':
                kbs = [(kb, None) for kb in (range(NB) if s == 0 else range(32, 34))]
                steps = []
                for vh in range(8):
                    for r in range(2):
                        for ki, (kb, _) in enumerate(kbs):
                            i3, i4 = np_ % 3, np_ % 4
                            np_ += 1
                            last = (ki == len(kbs) - 1)

                            def A(vh=vh, r=r, kb=kb, i3=i3, i4=i4, W=W, sl=sl):
                                mm(S, pb[i3][:, :W], KT[:, vh, kb * 128:(kb + 1) * 128],
                                   qz4[:, vh, r, :W], True, True, [('KT', vh), ('QZ', r)], [('ps', i3)])
                                act(S, P[i4][:, :W], pb[i3][:, :W], AF.Exp, [('ps', i3)], [('P', i4)])

                            def B(vh=vh, r=r, kb=kb, ki=ki, last=last, i4=i4, W=W):
                                po = pb[3 + r]
                                pok = ('ps', 3 + r)
                                mm(S, po[:, :W], V[:, kb, vh * 128:(vh + 1) * 128], P[i4][:, :W], ki == 0, last,
                                   [('V', kb // 9), ('P', i4)], [pok])
                                q_ = 0
                                eng = 'dve'
                                lak = ('La', r, q_)
                                if ki < 1:
                                    cp(S, eng, La[r][q_][:, :W], P[i4][:, :W], [('P', i4)], [lak])
                                else:
                                    tt(S, eng, La[r][q_][:, :W], La[r][q_][:, :W], P[i4][:, :W], ALU.add, [lak, ('P', i4)], [lak])
                                if last and r == 1:
                                    for rr, rb, rk in ((0, r0b, 'r0'), (1, r1b, 'r1')):
                                        mm(S, pb[5][:, :W], onesf[:], La[rr][0][:, :W], True, True,
                                           [('La', rr, 0), 'onesf'], [('ps', 5)])
                                        recip(S, rb[:, :W], pb[5][:, :W], [('ps', 5)], [rk])
                                    tt(S, 'dve', o0[:, :W], pb[3][:, :W], r0b[:, :W], ALU.mult, [('ps', 3), 'r0'], ['o0'])
                                    tt(S, 'dve', r1b[:, :W], pb[4][:, :W], r1b[:, :W], ALU.mult, [('ps', 4), 'r1'], ['r1'])
                                    stt(S, o0[:, :W], r1b[:, :W], nlam[:, 0:1], o0[:, :W], ALU.mult, ALU.add,
                                        ['o0', 'r1', 'nlam'], ['o0'])
                                    tt(S, 'pool', osq[:, :W], o0[:, :W], o0[:, :W], ALU.mult, ['o0'], ['osq'])
                                    mm(S, pb[6][:, :W], g.ones[:], osq[:, :W], True, True, ['osq'], [('ps', 6)])
                                    act(S, r0b[:, :W], pb[6][:, :W], AF.Sqrt, [('ps', 6)], ['r0'], bias=g.epsb[:, 0:1],
                                        scale=1.0 / 128)
                                    recip(S, r0b[:, :W], r0b[:, :W], ['r0'], ['r0'])
                                    stt(S, OT[:, vh, :W], o0[:, :W], sg[:, 0:1], r0b[:, :W], ALU.mult, ALU.mult,
                                        ['o0', 'sg', 'r0'], ['OT'])

                            steps.append((A, B))
                pipeline(steps, 2)
                hks = [('QZ', 0), ('QZ', 1)]
                dma(S, 'sp', hv[:, :, :W], hview(src)[:, :, t0:t0 + W], [], hks, "hld0")
                for m in range(8):
                    po = pb[6 + (m % 2)]
                    pok = ('ps', 6 + (m % 2))
                    for vh in range(8):
                        mm(S, po[:, :W], wout[:, vh, m * 128:(m + 1) * 128], OT[:, vh, :W], vh == 0, vh == 7,
                           ['wout', 'OT'], [pok])
                    stt(S, hv[:, m, :W], po[:, :W], MV(g, l, 2, m, s), hv[:, m, :W], ALU.mult, ALU.add,
                        [pok] + hks, hks)
                dma(S, 'sp', hview(dst)[:, :, t0:t0 + W], hv[:, :, :W], hks, [], "hst0")
            else:
                j0 = t0 // 128
                kbs = [(32, None), (33, None)] + [(kb, kb - j0 + 1) for kb in range(j0 - 1, j0 + 5) if 0 <= kb < 32]
                steps = []
                SB = [0, 1, 6]
                for n in range(16):
                    for ki, (kb, mi) in enumerate(kbs):
                        sb_, i4 = SB[np_ % 3], np_ % 4
                        np_ += 1
                        last = (ki == len(kbs) - 1)

                        def A(n=n, kb=kb, mi=mi, sb_=sb_, i4=i4, W=W, sl=sl):
                            c, r = n // 2, n % 2
                            gk = SWA_PERM[n] // 4
                            mm(S, pb[sb_][:, :W], KT[:, gk // 2, kb * 128:(kb + 1) * 128],
                               qzs[sl][:, c, r, :W], True, mi is None, [('KT', gk // 2), ('Q', sl, r)], [('ps', sb_)])
                            if mi is not None:
                                mm(S, pb[sb_][:, :W], identb[:], msk[:, mi, :W], False, True, ['identb', 'msk'], [('ps', sb_)])
                            act(S, P[i4][:, :W], pb[sb_][:, :W], AF.Exp, [('ps', sb_)], [('P', i4)])

                        def B(n=n, kb=kb, ki=ki, last=last, i4=i4, W=W):
                            gk = SWA_PERM[n] // 4
                            po = pb[2 + 2 * (n % 2)]
                            pl = pb[3 + 2 * (n % 2)]
                            pok, plk = ('ps', 2 + 2 * (n % 2)), ('ps', 3 + 2 * (n % 2))
                            pu, puk = P[i4], ('P', i4)
                            mm(S, po[:64, :W], V[:, kb, gk * 64:(gk + 1) * 64], pu[:, :W], ki == 0, last,
                               [('V', kb // 9), puk], [pok])
                            mm(S, pl[:64, :W], g.ones[:, :64], pu[:, :W], ki == 0, last, [puk], [plk])
                            if last:
                                ts(S, 'dve', r0b[:64, :W], pl[:64, :W], snk[:64, n:n + 1], None, ALU.add, None,
                                   [plk, 'snk'], ['r0'])
                                recip(S, r0b[:64, :W], r0b[:64, :W], ['r0'], ['r0'])
                                tt(S, 'dve', OT[:, n, :W], po[:64, :W], r0b[:64, :W], ALU.mult, [pok, 'r0'], ['OT'])

                        steps.append((A, B))
                pipeline(steps, 2)
                for m in range(8):
                    po = pb[6 + (m % 2)]
                    pok = ('ps', 6 + (m % 2))
                    for n in range(16):
                        mm(S, po[:, :W], wout[:, n, m * 128:(m + 1) * 128], OT[:, n, :W], n == 0, n == 15,
                           ['wout', 'OT'], [pok])
                    stt(S, hb[sl][:, m, :W], po[:, :W], MV(g, l, 2, m, s), hb[sl][:, m, :W], ALU.mult, ALU.add,
                        [pok, hk], [hk])
            if kind != 'diff':
                dma(S, 'sp', hview(dst)[:, :, t0:t0 + W], hb[sl][:, :, :W], [hk], [], f"hst{sl}")
        if kind == 'swa':
            pass
        S.emit(nc, *sems)


IN_SPECS = [
    ("hT0", [D, T]), ("cT", [128, 8, 2]), ("ada_w", [4, D, 6 * D]), ("ada_bT", [128, 4, 48]),
    ("gmixT", [128, 4, 8]), ("gffnT", [128, 4, 8]), ("ident", [128, 128]),
    ("sc_in_w", [D, 3 * D]), ("sc_convT", [128, 8, 3]), ("sc_out_w", [D, D]),
    ("ffn_w1", [2, D, FF]), ("ffn_w3", [2, D, FF]), ("ffn_w2", [2, FF, D]),
    ("moe_rwT", [2, 128, 8, NE]), ("moe_rb", [2, 1, NE]),
    ("moe_w1", [2, NE, D, FF]), ("moe_w3", [2, NE, D, FF]), ("moe_w2", [2, NE, FF, D]),
    ("cm_in_w", [D, 4096]), ("cm_out_w", [2048, D]), ("cm_wsT", [128, 8, 128]), ("cm_buT", [128, 16]),
    ("cm_bv", [1, 2048]), ("cm_vgT", [128, 16]), ("cm_bsf", [1, 1024]),
    ("da_qkv_w", [D, 3072]), ("da_out_w", [D, D]), ("da_qkg", [128, 2]), ("da_lam", [1, 256]), ("da_subg", [128, 1]),
    ("sw_qkv_wp", [D, 1536]), ("sw_out_wp", [D, D]), ("sw_qkg", [128, 2]), ("sw_sinkp", [1, 16]),
    ("ropecs", [128, 2, T]), ("ropeRT", [128, 128]), ("blkones", [128, 128]), ("swa_mask", [128, 6, 512]),
]


def build(nphase):
    nc = bass.Bass("TRN2", target_bir_lowering=False)
    io = {}
    for name, shape in IN_SPECS:
        io[name] = nc.dram_tensor(name, shape, F32, kind="ExternalInput").ap()
    out = nc.dram_tensor("out", [D, T], F32, kind="ExternalOutput").ap()
    hA = nc.dram_tensor("hA", [D, T], F32, kind="Internal").ap()
    hB = nc.dram_tensor("hB", [D, T], F32, kind="Internal").ap()
    g = G()
    with ExitStack() as es:
        g.modv = es.enter_context(nc.sbuf_tensor("modv", [128, 4, 48, 2], F32))
        g.ones = es.enter_context(nc.sbuf_tensor("ones", [128, 128], BF16))
        g.ident = es.enter_context(nc.sbuf_tensor("ident_sb", [128, 128], F32))
        g.epsb = es.enter_context(nc.sbuf_tensor("epsb", [128, 1], F32))
        NSET = 3
        semsets = []
        for k in range(NSET):
            esem = {e: es.enter_context(nc.semaphore(f"es{k}_{e}")) for e in ENGS}
            dsems = [es.enter_context(nc.semaphore(f"ds{k}_{i}")) for i in range(NDSEM)]
            semsets.append((esem, dsems, dict(ebase={e: 0 for e in ENGS}, dbase=[0] * NDSEM)))
        with nc.Block() as blk:
            blk.vector(lambda e: e.memset(g.epsb[:], EPS))

        scr = dict(
            QT=nc.dram_tensor("scrQT", [D, T], BF16, kind="Internal").ap(),
            KT=nc.dram_tensor("scrKT", [D, T], BF16, kind="Internal").ap(),
            V=nc.dram_tensor("scrV", [T, D], BF16, kind="Internal").ap(),
        )
        P_ = [
            ('conv', lambda sm, a, b: phase_conv(nc, g, io, sm, a, b), True),
            ('ffn0', lambda sm, a, b: phase_ffn(nc, g, io, sm, a, b, 0, False, True), True),
            ('qkv1', lambda sm, a, b: phase_qkv(nc, g, io, sm, a, 1, 'diff', scr), False),
            ('att1', lambda sm, a, b: phase_att(nc, g, io, sm, a, b, 1, 'diff', scr), True),
            ('moe1', lambda sm, a, b: phase_ffn(nc, g, io, sm, a, b, 1, True, True), True),
            ('gmlp', lambda sm, a, b: phase_gmlp(nc, g, io, sm, a, b), True),
            ('ffn2', lambda sm, a, b: phase_ffn(nc, g, io, sm, a, b, 2, False, True), True),
            ('qkv3', lambda sm, a, b: phase_qkv(nc, g, io, sm, a, 3, 'swa', scr), False),
            ('att3', lambda sm, a, b: phase_att(nc, g, io, sm, a, b, 3, 'swa', scr), True),
            ('moe3', lambda sm, a, b: phase_ffn(nc, g, io, sm, a, b, 3, True, False), True),
        ]
        sel = os.environ.get("KPH")
        if sel is not None:
            idx = [int(x) for x in sel.split(",") if x != ""]
        else:
            idx = list(range(min(nphase, len(P_))))
        pi = 0
        phase_adaln(nc, g, io, semsets[0])
        cur = io['hT0']
        bufs = [hB, hA]
        nb_ = 0
        for ix in idx:
            pi += 1
            sm = semsets[pi % NSET]
            dstb = bufs[nb_ % 2]
            P_[ix][1](sm, cur, dstb)
            if P_[ix][2]:
                cur = dstb
                nb_ += 1
        last = cur if nb_ > 0 else None
        with nc.semaphore("fin") as fin, nc.Block() as blk:
            def _fin(e):
                if last is not None:
                    e.dma_start(out=out, in_=last).then_inc(fin, 16)
                    e.wait_ge(fin, 16)
            blk.sync(_fin)
    return nc


_NC_CACHE = {}
_CONST = {}


def _consts():
    if _CONST:
        return _CONST
    t = np.arange(SEQ)
    row = (t // 64).astype(np.float32)
    colp = (t % 64).astype(np.float32)
    inv = (np.float32(10000.0) ** (-np.arange(16, dtype=np.float32) / np.float32(16))).astype(np.float32)
    ang = np.zeros((64, SEQ), np.float32)
    for d in range(64):
        pos = row if d < 32 else colp
        ang[d] = pos * inv[d % 16]
    cos = np.ones((64, T), np.float32)
    sin = np.zeros((64, T), np.float32)
    cos[:, :SEQ] = np.cos(ang)
    sin[:, :SEQ] = np.sin(ang)
    cs = np.stack([np.concatenate([cos, cos], 0), np.concatenate([sin, sin], 0)], axis=1)
    R = np.zeros((64, 64), np.float32)
    for part in range(2):
        b = part * 32
        for i in range(16):
            R[b + i, b + i + 16] = -1.0
            R[b + i + 16, b + i] = 1.0
    RT = np.zeros((128, 128), np.float32)
    RT[:64, :64] = R.T
    RT[64:, 64:] = R.T
    bo = np.zeros((128, 128), np.float32)
    bo[:64, :64] = 1.0
    bo[64:, 64:] = 1.0
    kk = np.arange(128)[:, None, None]
    mi = np.arange(6)[None, :, None]
    qq = np.arange(512)[None, None, :]
    dlt = (mi - 1) * 128 + kk - qq
    mask = np.where(np.abs(dlt) <= 128, 0.0, -30000.0).astype(np.float32)
    _CONST.update(ropecs=np.ascontiguousarray(cs), ropeRT=RT, blkones=bo, swa_mask=np.ascontiguousarray(mask))
    return _CONST


def host_inputs(inputs, b):
    x = inputs['x'][b]
    ctx = inputs['ctx'][b]
    m = {}
    m['hT0'] = np.ascontiguousarray(np.concatenate([x.T, ctx.T], axis=1))
    cc = np.stack([inputs['c'][b], inputs['c_ctx']], axis=1)
    m['cT'] = np.ascontiguousarray(cc.reshape(8, 128, 2).transpose(1, 0, 2))
    m['ada_w'] = inputs['ada_w']
    m['ada_bT'] = np.ascontiguousarray(inputs['ada_b'].reshape(4, 48, 128).transpose(2, 0, 1))
    m['gmixT'] = np.ascontiguousarray(inputs['norm_mix_g'].reshape(4, 8, 128).transpose(2, 0, 1))
    m['gffnT'] = np.ascontiguousarray(inputs['norm_ffn_g'].reshape(4, 8, 128).transpose(2, 0, 1))
    m['ident'] = np.eye(128, dtype=np.float32)
    m['sc_in_w'] = inputs['sc_in_w'][0]
    m['sc_convT'] = np.ascontiguousarray(inputs['sc_conv_w'][0].reshape(3, 8, 128).transpose(2, 1, 0))
    m['sc_out_w'] = inputs['sc_out_w'][0]
    m['ffn_w1'] = inputs['ffn_w1']
    m['ffn_w3'] = inputs['ffn_w3']
    m['ffn_w2'] = inputs['ffn_w2']
    m['moe_rwT'] = np.ascontiguousarray(inputs['moe_router_w'].reshape(2, 8, 128, NE).transpose(0, 2, 1, 3))
    m['moe_rb'] = np.ascontiguousarray(inputs['moe_router_b'].reshape(2, 1, NE))
    m['moe_w1'] = inputs['moe_w1']
    m['cm_in_w'] = inputs['cm_in_w'][0]
    m['da_qkv_w'] = inputs['da_qkv_w'][0]
    m['da_out_w'] = inputs['da_out_w'][0]
    m['da_qkg'] = np.stack([np.tile(inputs['da_q_norm_g'][0], 2), np.tile(inputs['da_k_norm_g'][0], 2)], axis=1)
    m['da_lam'] = inputs['da_lambda'][0].reshape(1, 256)
    m['da_subg'] = inputs['da_sub_norm_g'][0].reshape(128, 1)
    wqkv = inputs['sw_qkv_w'][0]
    perm = np.array(SWA_PERM)
    qcols = (perm[:, None] * 64 + np.arange(64)[None, :]).reshape(-1)
    m['sw_qkv_wp'] = np.concatenate([wqkv[:, qcols], wqkv[:, 1024:]], axis=1)
    m['sw_out_wp'] = inputs['sw_out_w'][0][qcols, :]
    m['sw_qkg'] = np.stack([np.tile(inputs['sw_q_norm_g'][0], 2), np.tile(inputs['sw_k_norm_g'][0], 2)], axis=1)
    m['sw_sinkp'] = inputs['sw_sink'][0][perm].reshape(1, 16)
    m.update(_consts())
    m['cm_out_w'] = inputs['cm_out_w'][0]
    m['cm_wsT'] = np.ascontiguousarray(inputs['cm_ws'][0].transpose(2, 0, 1))
    m['cm_buT'] = np.ascontiguousarray(inputs['cm_in_b'][0][:2048].reshape(16, 128).T)
    m['cm_bv'] = np.ascontiguousarray(inputs['cm_in_b'][0][2048:].reshape(1, 2048))
    m['cm_vgT'] = np.ascontiguousarray(inputs['cm_v_norm_g'][0].reshape(16, 128).T)
    m['cm_bsf'] = np.ascontiguousarray(inputs['cm_bs'][0].reshape(1, 1024))
    m['moe_w3'] = inputs['moe_w3']
    m['moe_w2'] = inputs['moe_w2']
    return {k: np.ascontiguousarray(v, dtype=np.float32) for k, v in m.items()}


def kernel(**inputs):
    nphase = int(os.environ.get("KSTOP", "99"))
    ncores = int(os.environ.get("KCORES", "8"))
    inputs = {k: np.asarray(v) for k, v in inputs.items()}
    if nphase not in _NC_CACHE:
        _NC_CACHE[nphase] = build(nphase)
    nc = _NC_CACHE[nphase]
    in_maps = [host_inputs(inputs, b) for b in range(ncores)]
    res = run_bass_kernel_spmd(nc, in_maps, core_ids=list(range(ncores)))
    outs = [r["out"] for r in res.results]
    if os.environ.get("KRAW"):
        return outs
    full = np.stack([o[:, :SEQ].T for o in outs], axis=0)
    return np.ascontiguousarray(full.astype(np.float32))
```

```python
import os, math
from contextlib import ExitStack
import numpy as np
import concourse.bass as bass
import concourse.mybir as mybir
from concourse.bass_utils import run_bass_kernel_spmd

F32 = mybir.dt.float32
BF16 = mybir.dt.bfloat16
ALU = mybir.AluOpType
AF = mybir.ActivationFunctionType
AX = mybir.AxisListType

D = 1024
SEQ = 4096
CTXL = 256
T = SEQ + CTXL
FF = 2816
NE = 8
EPS = 1e-6
ENGS = ('pe', 'act', 'dve', 'pool', 'sp')
NDSEM = 28


class Sched:
    def __init__(self):
        self.ops = []
        self.lw = {}
        self.rd = {}

    def add(self, eng, fn, r=(), w=(), dma=False, sg=None, cost=300.0, xfer=0.0):
        deps = set()
        for k in r:
            x = self.lw.get(k)
            if x is not None:
                deps.add(x)
        for k in w:
            x = self.lw.get(k)
            if x is not None:
                deps.add(x)
            deps.update(self.rd.get(k, ()))
        i = len(self.ops)
        self.ops.append([eng, fn, deps, dma, sg, False, 0, float(cost), float(xfer)])
        for k in r:
            self.rd.setdefault(k, []).append(i)
        for k in w:
            self.lw[k] = i
            self.rd[k] = []
        return i

    def schedule(self, window=48, lat=150.0):
        ops = self.ops
        n = len(ops)
        left = [len(o[2]) for o in ops]
        users = [[] for _ in range(n)]
        for i, o in enumerate(ops):
            for d in o[2]:
                users[d].append(i)
        rdy = [0.0] * n
        queues = {e: [] for e in ENGS}
        for i, o in enumerate(ops):
            queues[o[0]].append(i)
        qpos = {e: 0 for e in ENGS}
        win = {e: [] for e in ENGS}
        etime = {e: 0.0 for e in ENGS}
        for e in ENGS:
            q = queues[e]
            while len(win[e]) < window and qpos[e] < len(q):
                win[e].append(q[qpos[e]])
                qpos[e] += 1
        order = []
        best = {e: None for e in ENGS}
        dirty = set(ENGS)
        done = 0
        while done < n:
            for e in dirty:
                b = None
                et = etime[e]
                for i in win[e]:
                    if left[i] == 0:
                        t = rdy[i] if rdy[i] > et else et
                        if b is None or t < b[0]:
                            b = (t, i)
                            if t <= et:
                                break
                best[e] = b
            dirty = set()
            pick = None
            for e in ENGS:
                b = best[e]
                if b is not None and (pick is None or b[0] < pick[0] or (b[0] == pick[0] and b[1] < pick[1])):
                    pick = (b[0], b[1], e)
            assert pick is not None, "scheduler stuck"
            t, i, e = pick
            o = ops[i]
            etime[e] = t + o[7]
            fin = etime[e] + o[8]
            order.append(i)
            done += 1
            win[e].remove(i)
            q = queues[e]
            if qpos[e] < len(q):
                win[e].append(q[qpos[e]])
                qpos[e] += 1
            dirty.add(e)
            for u in users[i]:
                ue = ops[u][0]
                r_ = fin if ue == e and not o[3] else fin + lat
                if r_ > rdy[u]:
                    rdy[u] = r_
                left[u] -= 1
                if left[u] == 0:
                    dirty.add(ue)
        newidx = {old_: new_ for new_, old_ in enumerate(order)}
        nops = []
        for old_ in order:
            o = ops[old_]
            o[2] = {newidx[d] for d in o[2]}
            nops.append(o)
        self.ops = nops

    def emit(self, nc, esem, dsems, st):
        if getattr(self, 'use_sched', True) and os.environ.get("KSCHED", "1") == "1":
            self.schedule()
        ops = self.ops
        pos = {}
        cnt = {e: 0 for e in ENGS}
        for i, o in enumerate(ops):
            pos[i] = cnt[o[0]]
            cnt[o[0]] += 1
        for o in ops:
            if o[3]:
                o[5] = True
        for i, o in enumerate(ops):
            for d in o[2]:
                y = ops[d]
                if y[3]:
                    continue
                if y[0] != o[0] or o[3]:
                    y[5] = True
                elif o[0] != 'pe' and pos[i] - pos[d] <= 2:
                    y[5] = True
        ec = st['ebase']
        sgc = {}
        sgsem = {}
        sgidx = {}
        for o in ops:
            if o[3]:
                sg = o[4]
                if sg not in sgsem:
                    assert len(sgsem) < len(dsems), "too many dma sem groups"
                    sgidx[sg] = len(sgsem)
                    sgsem[sg] = dsems[len(sgsem)]
                    sgc[sg] = st['dbase'][sgidx[sg]]
                sgc[sg] = sgc[sg] + 16
                o[6] = sgc[sg]
            elif o[5]:
                ec[o[0]] += 1
                o[6] = ec[o[0]]
        for sg, i_ in sgidx.items():
            st['dbase'][i_] = sgc[sg]
        prog = {e: [] for e in ENGS}
        seen = {e: {} for e in ENGS}
        for i, o in enumerate(ops):
            waits = {}
            for d in o[2]:
                y = ops[d]
                if y[3]:
                    key = ('d', y[4])
                    sem = sgsem[y[4]]
                else:
                    if y[0] == o[0] and not o[3]:
                        if o[0] == 'pe' or pos[i] - pos[d] > 2:
                            continue
                    key = ('e', y[0])
                    sem = esem[y[0]]
                v = y[6]
                if waits.get(key, (None, 0))[1] < v:
                    waits[key] = (sem, v)
            wl = []
            for key, (sem, v) in waits.items():
                if seen[o[0]].get(key, 0) >= v:
                    continue
                seen[o[0]][key] = v
                wl.append((sem, v))
            prog[o[0]].append((wl, o))

        def run(e, name):
            for wl, o in prog[name]:
                for sem, v in wl:
                    e.wait_ge(sem, v)
                ins = o[1](e)
                if o[3]:
                    ins.then_inc(sgsem[o[4]], 16)
                elif o[5]:
                    ins.then_inc(esem[name], 1)
            if name == 'sp':
                for sg, v in sgc.items():
                    e.wait_ge(sgsem[sg], v)

        with nc.Block() as blk:
            blk.tensor(lambda e: run(e, 'pe'))
            blk.scalar(lambda e: run(e, 'act'))
            blk.vector(lambda e: run(e, 'dve'))
            blk.gpsimd(lambda e: run(e, 'pool'))
            blk.sync(lambda e: run(e, 'sp'))


def _fsz(ap):
    n = 1
    for d in ap.shape[1:]:
        n *= int(d)
    return n


def _cost(eng, ap):
    n = _fsz(ap)
    if eng == 'dve':
        return 70.0 + n / 0.75
    if eng == 'pool':
        return 100.0 + n / 0.485
    return 110.0 + n / 0.96


def mm(S, out, lhsT, rhs, start, stop, r, w):
    c = max(64.0, _fsz(out) / 2.4 + 45.0)
    if lhsT.dtype == F32:
        c *= 4.0
    S.add('pe', lambda e: e.matmul(out, lhsT, rhs, start=start, stop=stop), r, w, cost=c)


def act(S, out, in_, func, r, w, bias=None, scale=None, accum=None):
    kw = {}
    if bias is not None:
        kw['bias'] = bias
    if scale is not None:
        kw['scale'] = scale
    if accum is not None:
        kw['accum_out'] = accum
    S.add('act', lambda e: e.activation(out=out, in_=in_, func=func, **kw), r, w, cost=_cost('act', out))


def tt(S, eng, out, in0, in1, op, r, w):
    S.add(eng, lambda e: e.tensor_tensor(out=out, in0=in0, in1=in1, op=op), r, w, cost=_cost(eng, out))


def ts(S, eng, out, in0, s1, s2, op0, op1, r, w):
    if s2 is None:
        S.add(eng, lambda e: e.tensor_scalar(out=out, in0=in0, scalar1=s1, scalar2=None, op0=op0), r, w, cost=_cost(eng, out))
    else:
        S.add(eng, lambda e: e.tensor_scalar(out=out, in0=in0, scalar1=s1, scalar2=s2, op0=op0, op1=op1), r, w, cost=_cost(eng, out))


def stt(S, out, in0, scalar, in1, op0, op1, r, w):
    S.add('dve', lambda e: e.scalar_tensor_tensor(out=out, in0=in0, scalar=scalar, in1=in1, op0=op0, op1=op1), r, w, cost=_cost('dve', out))


def cp(S, eng, out, in_, r, w):
    S.add(eng, lambda e: e.tensor_copy(out=out, in_=in_), r, w, cost=_cost(eng, out))


def recip(S, out, in_, r, w):
    S.add('dve', lambda e: e.reciprocal(out=out, in_=in_), r, w, cost=_cost('dve', out))


def mset(S, eng, ap, val, r, w):
    S.add(eng, lambda e: e.memset(ap, val), r, w, cost=_cost(eng, ap))


def dma(S, q, out, in_, r, w, sg):
    nb_ = int(out.shape[0]) * _fsz(out) * 4
    S.add(q, lambda e: e.dma_start(out=out, in_=in_), r, w, dma=True, sg=sg,
          cost=(1000.0 if q == 'pool' else 60.0), xfer=2000.0 + nb_ / 150.0)


def pipeline(steps, depth=1):
    pend = []
    for A, B in steps:
        A()
        pend.append(B)
        if len(pend) > depth:
            pend.pop(0)()
    for B in pend:
        B()


def hview(ap):
    return ap.rearrange("(c p) t -> p c t", p=128)


def wview(ap):
    return ap.rearrange("(kc p) f -> p kc f", p=128)


class G:
    pass


_UID = [0]


def _un(name):
    _UID[0] += 1
    return f"{name}_{_UID[0]}"


class _NCW:
    def __init__(self, nc):
        self._nc = nc

    def sbuf_tensor(self, name, shape, dt):
        return self._nc.sbuf_tensor(_un(name), shape, dt)

    def psum_tensor(self, name, shape, dt):
        return self._nc.psum_tensor(_un(name), shape, dt)

    def __getattr__(self, k):
        return getattr(self._nc, k)


def MV(g, l, which, c, s):
    return g.modv[:, l, which * 8 + c, s:s + 1]


def phase_adaln(nc, g, io, sems):
    nc = _NCW(nc)
    S = Sched()
    with ExitStack() as es:
        wb = [es.enter_context(nc.sbuf_tensor(f"adw{i}", [128, 8, 3072], BF16)) for i in range(2)]
        cT = es.enter_context(nc.sbuf_tensor("cT", [128, 8, 2], F32))
        sT = es.enter_context(nc.sbuf_tensor("sT", [128, 8, 2], BF16))
        bT = es.enter_context(nc.sbuf_tensor("bT", [128, 4, 48], F32))
        gm = es.enter_context(nc.sbuf_tensor("gm", [128, 4, 8], F32))
        gf = es.enter_context(nc.sbuf_tensor("gf", [128, 4, 8], F32))
        ps = es.enter_context(nc.psum_tensor("ps_ada", [128, 512], F32))
        mset(S, 'pool', g.ones[:], 1.0, [], ['ones'])
        dma(S, 'sp', g.ident[:], io['ident'], [], ['ident'], 'c0')
        dma(S, 'sp', cT[:], io['cT'], [], ['cT'], 'c1')
        dma(S, 'sp', bT[:], io['ada_bT'], [], ['bT'], 'c2')
        dma(S, 'sp', gm[:], io['gmixT'], [], ['gm'], 'c3')
        dma(S, 'sp', gf[:], io['gffnT'], [], ['gf'], 'c4')
        act(S, sT[:], cT[:], AF.Silu, ['cT'], ['sT'])
        n = 0
        for l in range(4):
            for half in range(2):
                slot = n % 2
                n += 1
                for q in range(2):
                    c0 = half * 3072 + q * 1536
                    dma(S, 'pool', wb[slot][:, :, q * 1536:(q + 1) * 1536], wview(io['ada_w'][l])[:, :, c0:c0 + 1536],
                        [], [('adw', slot, q)], f"adw{slot}_{q}")
                for jj in range(24):
                    j = half * 24 + jj
                    q = (jj * 128) // 1536
                    col = (l * 48 + j) * 2
                    for kc in range(8):
                        mm(S, ps[:, col:col + 2], wb[slot][:, kc, jj * 128:(jj + 1) * 128], sT[:, kc, :],
                           kc == 0, kc == 7, [('adw', slot, q), 'sT'], ['psada'])
        for l in range(4):
            tt(S, 'dve', g.modv[:, l], ps[:, l * 96:(l + 1) * 96].rearrange("p (j s) -> p j s", s=2),
               bT[:, l, :].unsqueeze(2).to_broadcast([128, 48, 2]), ALU.add, ['psada', 'bT'], ['modv'])
            stt(S, g.modv[:, l, 8:16, :], g.modv[:, l, 8:16, :], 1.0,
                gm[:, l, :].unsqueeze(2).to_broadcast([128, 8, 2]), ALU.add, ALU.mult, ['modv', 'gm'], ['modv'])
            stt(S, g.modv[:, l, 32:40, :], g.modv[:, l, 32:40, :], 1.0,
                gf[:, l, :].unsqueeze(2).to_broadcast([128, 8, 2]), ALU.add, ALU.mult, ['modv', 'gf'], ['modv'])
        S.emit(nc, *sems)


def prenorm(S, g, src_cols, hb, hk, ab, ak, tmp, l, which, s, n, sg, a32=None, a32k=None):
    sq = tmp['sq'][:, :, :n]
    xn = tmp['xn'][:, :, :n]
    rs = tmp['rs'][:, :n]
    ps = tmp['ps'][:, :n]
    psk = tmp['psk']
    sqk = tmp.get('sqk', 'sq')
    hks = hk if isinstance(hk, list) else [hk]
    dma(S, 'sp', hb, src_cols, [], hks, sg)
    tt(S, 'pool', sq, hb, hb, ALU.mult, hks, [sqk])
    for c in range(8):
        mm(S, ps, g.ones[:], sq[:, c, :], c == 0, c == 7, [sqk, 'ones'], [psk])
    act(S, rs, ps, AF.Sqrt, [psk], ['rs'], bias=g.epsb[:, 0:1], scale=1.0 / 1024)
    recip(S, rs, rs, ['rs'], ['rs'])
    tt(S, 'dve', xn, hb, rs.unsqueeze(1).to_broadcast([128, 8, n]), ALU.mult, hks + ['rs'], ['xn'])
    for c in range(8):
        if a32 is None:
            act(S, ab[:, c, :], xn[:, c, :], AF.Identity, ['xn'], [ak],
                scale=MV(g, l, which * 3 + 1, c, s), bias=MV(g, l, which * 3, c, s))
        else:
            act(S, a32[:, c, :], xn[:, c, :], AF.Identity, ['xn'], [a32k],
                scale=MV(g, l, which * 3 + 1, c, s), bias=MV(g, l, which * 3, c, s))
            cp(S, 'pool', ab[:, c, :], a32[:, c, :], [a32k], [ak])


def phase_conv(nc, g, io, sems, src, dst, l=0):
    nc = _NCW(nc)
    S = Sched()
    W = 256
    with ExitStack() as es:
        win = es.enter_context(nc.sbuf_tensor("scwin", [128, 8, 3072], BF16))
        wout = es.enter_context(nc.sbuf_tensor("scwout", [128, 8, 1024], BF16))
        cw = es.enter_context(nc.sbuf_tensor("sccw", [128, 8, 3], F32))
        hb = [es.enter_context(nc.sbuf_tensor(f"sch{i}", [128, 8, W + 2], F32)) for i in range(2)]
        ab = [es.enter_context(nc.sbuf_tensor(f"sca{i}", [128, 8, W + 2], BF16)) for i in range(2)]
        sq = es.enter_context(nc.sbuf_tensor("scsq", [128, 8, W + 2], BF16))
        xn = es.enter_context(nc.sbuf_tensor("scxn", [128, 8, W + 2], F32))
        rs = es.enter_context(nc.sbuf_tensor("scrs", [128, W + 2], F32))
        xv = [es.enter_context(nc.sbuf_tensor(f"scxv{i}", [128, W + 2], F32)) for i in range(2)]
        yb = [es.enter_context(nc.sbuf_tensor(f"scy{i}", [128, W + 2], F32)) for i in range(2)]
        cv = [es.enter_context(nc.sbuf_tensor(f"sccv{i}", [128, W], F32)) for i in range(2)]
        ub = [es.enter_context(nc.sbuf_tensor(f"scu{i}", [128, 8, W], BF16)) for i in range(2)]
        pb = [es.enter_context(nc.psum_tensor(f"scp{i}", [128, 512], F32)) for i in range(8)]
        for q in range(2):
            dma(S, 'pool', win[:, :, q * 1536:(q + 1) * 1536], wview(io['sc_in_w'])[:, :, q * 1536:(q + 1) * 1536],
                [], [('win', q)], f"win{q}")
        dma(S, 'pool', wout[:], wview(io['sc_out_w']), [], ['wout'], "wout")
        dma(S, 'sp', cw[:], io['sc_convT'], [], ['cw'], "cw")
        tiles = [(i * W, 0, 0, SEQ) for i in range(SEQ // W)] + [(SEQ, 1, SEQ, T)]
        for ti, (t0, s, slo, shi) in enumerate(tiles):
            sl = ti % 2
            lo = max(t0 - 1, slo)
            hi = min(t0 + W + 1, shi)
            off = lo - (t0 - 1)
            n = hi - lo
            hk = ('h', sl)
            ak = ('a', sl)
            tmp = dict(sq=sq, xn=xn, rs=rs, ps=pb[7], psk=('ps', 7))
            prenorm(S, g, hview(src)[:, :, lo:hi], hb[sl][:, :, off:off + n], hk, ab[sl][:, :, off:off + n], ak,
                    tmp, l, 0, s, n, f"hld{sl}")
            uk = ('u', sl)
            for c in range(8):
                ps3 = [pb[(c % 2) * 3 + i] for i in range(3)]
                pk = [('ps', (c % 2) * 3 + i) for i in range(3)]
                for i in range(3):
                    col = i * 1024 + c * 128
                    q = col // 1536
                    for kc in range(8):
                        mm(S, ps3[i][:, off:off + n], win[:, kc, col:col + 128], ab[sl][:, kc, off:off + n],
                           kc == 0, kc == 7, [('win', q), ak], [pk[i]])
                ys = c % 2
                yk = ('y', ys)
                act(S, xv[ys][:, off:off + n], ps3[2][:, off:off + n], AF.Copy, [pk[2]], [('xv', ys)])
                tt(S, 'dve', yb[ys][:, off:off + n], ps3[1][:, off:off + n], xv[ys][:, off:off + n], ALU.mult,
                   [pk[1], ('xv', ys)], [yk])
                if off == 1:
                    mset(S, 'pool', yb[ys][:, 0:1], 0.0, [], [yk])
                if off + n < W + 2:
                    mset(S, 'pool', yb[ys][:, W + 1:W + 2], 0.0, [], [yk])
                ck = ('cv', ys)
                act(S, cv[ys][:], yb[ys][:, 0:W], AF.Identity, [yk], [ck], scale=cw[:, c, 0:1])
                stt(S, cv[ys][:], yb[ys][:, 1:W + 1], cw[:, c, 1:2], cv[ys][:], ALU.mult, ALU.add, [yk, ck, 'cw'], [ck])
                stt(S, cv[ys][:], yb[ys][:, 2:W + 2], cw[:, c, 2:3], cv[ys][:], ALU.mult, ALU.add, [yk, ck, 'cw'], [ck])
                tt(S, 'dve', ub[sl][:, c, :], ps3[0][:, 1:W + 1], cv[ys][:], ALU.mult, [pk[0], ck], [uk])
            for m in range(8):
                po = pb[6 + (m % 2)]
                pok = ('ps', 6 + (m % 2))
                for c in range(8):
                    mm(S, po[:, :W], wout[:, c, m * 128:(m + 1) * 128], ub[sl][:, c, :], c == 0, c == 7,
                       ['wout', uk], [pok])
                stt(S, hb[sl][:, m, 1:W + 1], po[:, :W], MV(g, l, 2, m, s), hb[sl][:, m, 1:W + 1], ALU.mult, ALU.add,
                    [pok, hk], [hk])
            dma(S, 'sp', hview(dst)[:, :, t0:t0 + W], hb[sl][:, :, 1:W + 1], [hk], [], f"hst{sl}")
        S.emit(nc, *sems)


def phase_ffn(nc, g, io, sems, src, dst, l, moe, with_ctx):
    nc = _NCW(nc)
    S = Sched()
    S.use_sched = False
    f = l // 2
    E = NE if moe else 1
    NG = int(os.environ.get('KNG', FF // 256))
    MAXW = 1280 if with_ctx else 1536
    NS = 4
    with ExitStack() as es:
        hS = es.enter_context(nc.sbuf_tensor("fh", [128, 8, MAXW], F32))
        aS = es.enter_context(nc.sbuf_tensor("fa", [128, 8, MAXW], BF16))
        w1r = [es.enter_context(nc.sbuf_tensor(f"fw1_{i}", [128, 8, 256], BF16)) for i in range(NS)]
        w3r = [es.enter_context(nc.sbuf_tensor(f"fw3_{i}", [128, 8, 256], BF16)) for i in range(NS)]
        w2r = [es.enter_context(nc.sbuf_tensor(f"fw2_{i}", [128, 2, 1024], BF16)) for i in range(NS)]
        sq = es.enter_context(nc.sbuf_tensor("fsq", [128, 8, 512], BF16))
        xn = es.enter_context(nc.sbuf_tensor("fxn", [128, 8, 512], F32))
        rs = es.enter_context(nc.sbuf_tensor("frs", [128, 512], F32))
        s1 = [es.enter_context(nc.sbuf_tensor(f"fs1_{i}", [128, 512], BF16)) for i in range(2)]
        h1 = [es.enter_context(nc.sbuf_tensor(f"fh1_{i}", [128, 2, 512], BF16)) for i in range(2)]
        pb = [es.enter_context(nc.psum_tensor(f"fp{i}", [128, 512], F32)) for i in range(8)]
        if moe:
            gB = es.enter_context(nc.sbuf_tensor("fgB", [128, NE, MAXW], BF16))
            p3g = [es.enter_context(nc.sbuf_tensor(f"fp3g_{i}", [128, 512], BF16)) for i in range(2)]
            yt = [es.enter_context(nc.sbuf_tensor(f"fyt_{i}", [128, 512], F32)) for i in range(2)]
            wr = es.enter_context(nc.sbuf_tensor("fwr", [128, 8, NE], F32))
            brt = es.enter_context(nc.sbuf_tensor("fbr", [128, NE], F32))
            lg = es.enter_context(nc.sbuf_tensor("flg", [128, 4, NE], F32))
            mx = es.enter_context(nc.sbuf_tensor("fmx", [128, 4, 8], F32))
            nt1 = es.enter_context(nc.sbuf_tensor("fnt1", [128, 4], F32))
            msk = es.enter_context(nc.sbuf_tensor("fmsk", [128, 4, NE], F32))
            ex = es.enter_context(nc.sbuf_tensor("fex", [128, 4, NE], F32))
            den = es.enter_context(nc.sbuf_tensor("fden", [128, 4], F32))
            gt = es.enter_context(nc.sbuf_tensor("fgt", [128, 4, NE], F32))
            gbc = es.enter_context(nc.sbuf_tensor("fgbc", [128, 4, NE, 128], F32))
            dma(S, 'sp', wr[:], io['moe_rwT'][f], [], ['wr'], "wr")
            dma(S, 'sp', brt[:], io['moe_rb'][f].partition_broadcast(128), [], ['br'], "br")
            W1 = io['moe_w1'][f]
            W3 = io['moe_w3'][f]
            W2 = io['moe_w2'][f]
        else:
            W1 = [io['ffn_w1'][f]]
            W3 = [io['ffn_w3'][f]]
            W2 = [io['ffn_w2'][f]]
        lat = [(i * 512, 512, 0) for i in range(8)]
        if with_ctx:
            supers = [[lat[2 * i], lat[2 * i + 1]] for i in range(4)]
            supers[3].append((SEQ, CTXL, 1))
        else:
            supers = [[lat[0], lat[1], lat[2]], [lat[3], lat[4], lat[5]], [lat[6], lat[7]]]
        supers = supers[:int(os.environ.get('KSUP', 4))]
        nld = 0
        it = 0
        PF = 2
        gseq = [(si, e_, gi) for si in range(len(supers)) for e_ in range(E) for gi in range(NG)]
        gpos = {k: i for i, k in enumerate(gseq)}
        issued = [0]

        def issue_upto(n):
            while issued[0] < min(n, len(gseq)):
                _, ee, gg = gseq[issued[0]]
                sl_ = issued[0] % NS
                jj0 = gg * 256
                dma(S, 'pool', w1r[sl_][:], wview(W1[ee])[:, :, jj0:jj0 + 256], [], [('w1', sl_)], f"w1_{sl_}")
                dma(S, 'pool', w3r[sl_][:], wview(W3[ee])[:, :, jj0:jj0 + 256], [], [('w3', sl_)], f"w3_{sl_}")
                dma(S, 'pool', w2r[sl_][:], W2[ee][jj0:jj0 + 256, :].rearrange("(j p) m -> p j m", p=128), [],
                    [('w2', sl_)], f"w2_{sl_}")
                issued[0] += 1

        issue_upto(PF)
        for si_, st in enumerate(supers):
            offs = []
            o_ = 0
            for (t0, W, s) in st:
                offs.append(o_)
                o_ += W
            for (t0, W, s), off in zip(st, offs):
                hk = [('h', off, m_) for m_ in range(8)]
                ak = ('a', off)
                tmp = dict(sq=sq, xn=xn, rs=rs, ps=pb[7], psk=('ps', 7))
                if not moe:
                    prenorm(S, g, hview(src)[:, :, t0:t0 + W], hS[:, :, off:off + W], hk, aS[:, :, off:off + W], ak,
                            tmp, l, 1, s, W, f"hld{off}")
                else:
                    prenorm(S, g, hview(src)[:, :, t0:t0 + W], hS[:, :, off:off + W], hk, aS[:, :, off:off + W], ak,
                            tmp, l, 1, s, W, f"hld{off}", a32=xn[:, :, :W], a32k='xn')
                    nb = W // 128
                    for b in range(nb):
                        for kc in range(8):
                            mm(S, pb[6][:, b * 8:(b + 1) * 8], xn[:, kc, b * 128:(b + 1) * 128], wr[:, kc, :],
                               kc == 0, kc == 7, ['xn', 'wr'], [('ps', 6)])
                    tt(S, 'dve', lg[:, :nb, :], pb[6][:, :nb * 8].rearrange("p (b e) -> p b e", e=8),
                       brt[:].unsqueeze(1).to_broadcast([128, nb, NE]), ALU.add, [('ps', 6), 'br'], ['lg'])
                    for b in range(nb):
                        S.add('dve', (lambda o_, i_: (lambda e: e.max(out=o_, in_=i_)))(mx[:, b, :], lg[:, b, :]),
                              ['lg'], ['mx'])
                    ts(S, 'dve', nt1[:, :nb], mx[:, :nb, 0], -1.0, None, ALU.mult, None, ['mx'], ['nt1'])
                    for b in range(nb):
                        ts(S, 'dve', msk[:, b, :], lg[:, b, :], mx[:, b, 1:2], None, ALU.is_ge, None, ['lg', 'mx'], ['msk'])
                        act(S, ex[:, b, :], lg[:, b, :], AF.Exp, ['lg', 'nt1'], ['ex'], bias=nt1[:, b:b + 1], scale=1.0)
                    tt(S, 'dve', ex[:, :nb, :], ex[:, :nb, :], msk[:, :nb, :], ALU.mult, ['ex', 'msk'], ['ex'])
                    S.add('dve', (lambda o_, i_: (lambda e: e.tensor_reduce(out=o_, in_=i_, axis=AX.X, op=ALU.add)))(
                        den[:, :nb], ex[:, :nb, :]), ['ex'], ['den'])
                    recip(S, den[:, :nb], den[:, :nb], ['den'], ['den'])
                    tt(S, 'dve', gt[:, :nb, :], ex[:, :nb, :], den[:, :nb].unsqueeze(2).to_broadcast([128, nb, NE]),
                       ALU.mult, ['ex', 'den'], ['gt'])
                    cp(S, 'pool', gbc[:, :nb], gt[:, :nb, :].unsqueeze(3).to_broadcast([128, nb, NE, 128]), ['gt'], ['gbc'])
                    for e_ in range(NE):
                        pg = pb[4 + (e_ % 2)]
                        pgk = ('ps', 4 + (e_ % 2))
                        for b in range(nb):
                            mm(S, pg[:, b * 128:(b + 1) * 128], gbc[:, b, e_, :], g.ident[:], True, True,
                               ['gbc', 'ident'], [pgk])
                        act(S, gB[:, e_, off:off + W], pg[:, :W], AF.Copy, [pgk], [('gB', off)])
            steps = []
            for e_ in range(E):
                for gi in range(NG):
                    gidx = gpos[(si_, e_, gi)]
                    slot = gidx % NS
                    j0 = gi * 256
                    first = True
                    for (t0, W, s), off in zip(st, offs):
                        hs = it % 2
                        it += 1

                        def A(e_=e_, slot=slot, j0=j0, first=first, W=W, s=s, off=off, hs=hs, gidx=gidx):
                            if first:
                                issue_upto(gidx + PF + 1)
                            ak = ('a', off)
                            h1k = ('h1', hs)
                            for jj in range(2):
                                ss = jj
                                p1 = pb[jj * 2]
                                p3 = pb[jj * 2 + 1]
                                p1k = ('ps', jj * 2)
                                p3k = ('ps', jj * 2 + 1)
                                for kc in range(8):
                                    mm(S, p1[:, :W], w1r[slot][:, kc, jj * 128:(jj + 1) * 128], aS[:, kc, off:off + W],
                                       kc == 0, kc == 7, [('w1', slot), ak], [p1k])
                                for kc in range(8):
                                    mm(S, p3[:, :W], w3r[slot][:, kc, jj * 128:(jj + 1) * 128], aS[:, kc, off:off + W],
                                       kc == 0, kc == 7, [('w3', slot), ak], [p3k])
                                act(S, s1[ss][:, :W], p1[:, :W], AF.Silu, [p1k], [('s1', ss)])
                                if moe:
                                    tt(S, 'dve', p3g[ss][:, :W], p3[:, :W], gB[:, e_, off:off + W], ALU.mult,
                                       [p3k, ('gB', off)], [('p3g', ss)])
                                    tt(S, 'pool', h1[hs][:, jj, :W], s1[ss][:, :W], p3g[ss][:, :W], ALU.mult,
                                       [('s1', ss), ('p3g', ss)], [h1k])
                                else:
                                    tt(S, 'dve', h1[hs][:, jj, :W], p3[:, :W], s1[ss][:, :W], ALU.mult,
                                       [p3k, ('s1', ss)], [h1k])

                        def B(slot=slot, W=W, s=s, off=off, hs=hs):
                            h1k = ('h1', hs)
                            for m in range(8):
                                po = pb[4 + (m % 4)]
                                pok = ('ps', 4 + (m % 4))
                                for jj in range(2):
                                    mm(S, po[:, :W], w2r[slot][:, jj, m * 128:(m + 1) * 128], h1[hs][:, jj, :W],
                                       jj == 0, jj == 1, [('w2', slot), h1k], [pok])
                                hk = ('h', off, m)
                                if m % 2 == 0 or not moe:
                                    stt(S, hS[:, m, off:off + W], po[:, :W], MV(g, l, 5, m, s), hS[:, m, off:off + W],
                                        ALU.mult, ALU.add, [pok, hk], [hk])
                                else:
                                    ys = (m // 2) % 2
                                    act(S, yt[ys][:, :W], po[:, :W], AF.Identity, [pok], [('yt', ys)], scale=MV(g, l, 5, m, s))
                                    tt(S, 'pool', hS[:, m, off:off + W], hS[:, m, off:off + W], yt[ys][:, :W], ALU.add,
                                       [('yt', ys), hk], [hk])

                        steps.append((A, B))
                        first = False
            pipeline(steps, 1)
            for (t0, W, s), off in zip(st, offs):
                dma(S, 'sp', hview(dst)[:, :, t0:t0 + W], hS[:, :, off:off + W], [('h', off, m_) for m_ in range(8)], [], f"hst{off}")
        S.emit(nc, *sems)


def phase_gmlp(nc, g, io, sems, src, dst, l=2):
    nc = _NCW(nc)
    S = Sched()
    S.use_sched = False
    with ExitStack() as es:
        win = es.enter_context(nc.sbuf_tensor("cmwin", [128, 8, 4096], BF16))
        wout = es.enter_context(nc.sbuf_tensor("cmwout", [128, 16, 1024], BF16))
        wsT = es.enter_context(nc.sbuf_tensor("cmws", [128, 8, 128], BF16))
        bU = es.enter_context(nc.sbuf_tensor("cmbu", [128, 16], F32))
        bV = es.enter_context(nc.sbuf_tensor("cmbv", [128, 2048], F32))
        vg = es.enter_context(nc.sbuf_tensor("cmvg", [128, 16], F32))
        bsT = es.enter_context(nc.sbuf_tensor("cmbs", [128, 8, 128], F32))
        hb = [es.enter_context(nc.sbuf_tensor(f"cmh{i}", [128, 8, 512], F32)) for i in range(1)]
        ab = [es.enter_context(nc.sbuf_tensor(f"cma{i}", [128, 8, 512], BF16)) for i in range(1)]
        xn = es.enter_context(nc.sbuf_tensor("cmxn", [128, 8, 512], F32))
        rs = es.enter_context(nc.sbuf_tensor("cmrs", [128, 512], F32))
        vt = [es.enter_context(nc.sbuf_tensor(f"cmvt{i}", [128, 512], F32)) for i in range(2)]
        gv = es.enter_context(nc.sbuf_tensor("cmgv", [128, 2048], F32))
        junk = es.enter_context(nc.sbuf_tensor("cmjunk", [128, 512], BF16))
        ss = es.enter_context(nc.sbuf_tensor("cmss", [128, 4], F32))
        rv = es.enter_context(nc.sbuf_tensor("cmrv", [128, 1], F32))
        vn = [es.enter_context(nc.sbuf_tensor(f"cmvn{i}", [128, 2048], BF16)) for i in range(4)]
        ub = [es.enter_context(nc.sbuf_tensor(f"cmu{i}", [128, 512], F32)) for i in range(2)]
        m1 = [es.enter_context(nc.sbuf_tensor(f"cmm{i}", [128, 512], F32)) for i in range(2)]
        pr = es.enter_context(nc.sbuf_tensor("cmpr", [128, 16, 512], BF16))
        pb = [es.enter_context(nc.psum_tensor(f"cmp{i}", [128, 512], F32)) for i in range(8)]
        for q in range(4):
            dma(S, 'pool', win[:, :, q * 1024:(q + 1) * 1024], wview(io['cm_in_w'])[:, :, q * 1024:(q + 1) * 1024],
                [], [('win', q)], f"win{q}")
        dma(S, 'pool', wout[:], io['cm_out_w'].rearrange("(c p) m -> p c m", p=128), [], ['wout'], "wout")
        dma(S, 'pool', wsT[:], io['cm_wsT'], [], ['wsT'], "wsT")
        dma(S, 'sp', bU[:], io['cm_buT'], [], ['bU'], "bU")
        dma(S, 'sp', bV[:], io['cm_bv'].partition_broadcast(128), [], ['bV'], "bV")
        dma(S, 'sp', vg[:], io['cm_vgT'], [], ['vg'], "vg")
        dma(S, 'sp', bsT[:], io['cm_bsf'].partition_broadcast(128).rearrange("p o (g q) -> p (o g) q", g=8), [], ['bsT'], "bsT")
        tiles = [(i * 512, 512, 0) for i in range(8)] + [(SEQ, CTXL, 1)]
        nv = 0
        nu = 0
        for ti, (t0, W, s) in enumerate(tiles):
            sl = 0
            hk = ('h', sl)
            ak = ('a', sl)
            tmp = dict(sq=pr[:, 0:8, :], sqk='pr', xn=xn, rs=rs, ps=pb[7], psk=('ps', 7))
            prenorm(S, g, hview(src)[:, :, t0:t0 + W], hb[sl][:, :, :W], hk, ab[sl][:, :, :W], ak, tmp, l, 0, s, W,
                    f"hld{sl}")
            nb = W // 128
            for b in range(nb):
                for q4 in range(4):
                    pv = pb[nv % 2]
                    pvk = ('ps', nv % 2)
                    vts = nv % 2
                    nv += 1
                    col = 2048 + q4 * 512
                    for kc in range(8):
                        mm(S, pv[:], ab[sl][:, kc, b * 128:(b + 1) * 128], win[:, kc, col:col + 512], kc == 0, kc == 7,
                           [ak, ('win', col // 1024)], [pvk])
                    tt(S, 'dve', vt[vts][:], pv[:], bV[:, q4 * 512:(q4 + 1) * 512], ALU.add, [pvk, 'bV'], [('vt', vts)])
                    act(S, gv[:, q4 * 512:(q4 + 1) * 512], vt[vts][:], AF.Gelu_apprx_tanh, [('vt', vts)], ['gv'])
                    act(S, junk[:], gv[:, q4 * 512:(q4 + 1) * 512], AF.Square, ['gv'], ['junk', 'ss'],
                        accum=ss[:, q4:q4 + 1])
                S.add('dve', (lambda o_, i_: (lambda e: e.tensor_reduce(out=o_, in_=i_, axis=AX.X, op=ALU.add)))(
                    rv[:], ss[:]), ['ss'], ['rv'])
                act(S, rv[:], rv[:], AF.Sqrt, ['rv'], ['rv'], bias=g.epsb[:, 0:1], scale=1.0 / 2048)
                recip(S, rv[:], rv[:], ['rv'], ['rv'])
                ts(S, 'dve', vn[b][:], gv[:], rv[:, 0:1], None, ALU.mult, None, ['gv', 'rv'], [('vn', b)])
            for cu in range(16):
                pu = pb[2 + (nu % 2)]
                puk = ('ps', 2 + (nu % 2))
                psv = pb[4 + (nu % 2)]
                psk = ('ps', 4 + (nu % 2))
                us = nu % 2
                nu += 1
                gq = cu // 2
                for kc in range(8):
                    mm(S, pu[:, :W], win[:, kc, cu * 128:(cu + 1) * 128], ab[sl][:, kc, :W], kc == 0, kc == 7,
                       [ak, ('win', (cu * 128) // 1024)], [puk])
                act(S, ub[us][:, :W], pu[:, :W], AF.Gelu_apprx_tanh, [puk, 'bU'], [('ub', us)], bias=bU[:, cu:cu + 1])
                for b in range(nb):
                    mm(S, psv[:, b * 128:(b + 1) * 128], vn[b][:, cu * 128:(cu + 1) * 128], wsT[:, gq, :], True, True,
                       [('vn', b), 'wsT'], [psk])
                stt(S, m1[us][:, :W].rearrange("p (b q) -> p b q", q=128),
                    psv[:, :W].rearrange("p (b q) -> p b q", q=128), vg[:, cu:cu + 1],
                    bsT[:, gq, :].unsqueeze(1).to_broadcast([128, nb, 128]), ALU.mult, ALU.add,
                    [psk, 'vg', 'bsT'], [('m1', us)])
                tt(S, 'pool', pr[:, cu, :W], ub[us][:, :W], m1[us][:, :W], ALU.mult, [('ub', us), ('m1', us)], ['pr'])
            for m in range(8):
                po = pb[6 + (m % 2)]
                pok = ('ps', 6 + (m % 2))
                for cu in range(16):
                    mm(S, po[:, :W], wout[:, cu, m * 128:(m + 1) * 128], pr[:, cu, :W], cu == 0, cu == 15,
                       ['wout', 'pr'], [pok])
                stt(S, hb[sl][:, m, :W], po[:, :W], MV(g, l, 2, m, s), hb[sl][:, m, :W], ALU.mult, ALU.add,
                    [pok, hk], [hk])
            dma(S, 'sp', hview(dst)[:, :, t0:t0 + W], hb[sl][:, :, :W], [hk], [], f"hst{sl}")
        S.emit(nc, *sems)


SWA_PERM = [0, 4, 1, 5, 2, 6, 3, 7, 8, 12, 9, 13, 10, 14, 11, 15]


def phase_qkv(nc, g, io, sems, src, l, kind, scr):
    nc = _NCW(nc)
    S = Sched()
    if kind == 'diff':
        wq, NQC, NKC, VF = io['da_qkv_w'], 8, 8, 1024
        gname = 'da_qkg'
    else:
        wq, NQC, NKC, VF = io['sw_qkv_wp'], 8, 2, 256
        gname = 'sw_qkg'
    NC_ = NQC + NKC
    WCOLS = NC_ * 128 + VF
    with ExitStack() as es:
        win = es.enter_context(nc.sbuf_tensor("qw", [128, 8, WCOLS], BF16))
        gq = es.enter_context(nc.sbuf_tensor("qg", [128, 2], F32))
        RT = es.enter_context(nc.sbuf_tensor("qRT", [128, 128], BF16))
        bones = es.enter_context(nc.sbuf_tensor("qbo", [128, 128], BF16))
        cs = [es.enter_context(nc.sbuf_tensor(f"qcs{i}", [128, 2, 512], F32)) for i in range(2)]
        hb = [es.enter_context(nc.sbuf_tensor(f"qh{i}", [128, 8, 512], F32)) for i in range(2)]
        ab = [es.enter_context(nc.sbuf_tensor(f"qa{i}", [128, 8, 512], BF16)) for i in range(2)]
        sq = es.enter_context(nc.sbuf_tensor("qsq", [128, 8, 512], BF16))
        xn = es.enter_context(nc.sbuf_tensor("qxn", [128, 8, 512], F32))
        rs = es.enter_context(nc.sbuf_tensor("qrs", [128, 512], F32))
        xg = [es.enter_context(nc.sbuf_tensor(f"qxg{i}", [128, 512], BF16)) for i in range(2)]
        xs = [es.enter_context(nc.sbuf_tensor(f"qxs{i}", [128, 512], BF16)) for i in range(2)]
        rd = [es.enter_context(nc.sbuf_tensor(f"qrd{i}", [128, 512], F32)) for i in range(2)]
        t1 = [es.enter_context(nc.sbuf_tensor(f"qt1{i}", [128, 512], F32)) for i in range(2)]
        t2 = [es.enter_context(nc.sbuf_tensor(f"qt2{i}", [128, 512], F32)) for i in range(2)]
        ob = [es.enter_context(nc.sbuf_tensor(f"qo{i}", [128, 512], BF16)) for i in range(3)]
        vb = [es.enter_context(nc.sbuf_tensor(f"qv{i}", [128, 512], BF16)) for i in range(2)]
        pb = [es.enter_context(nc.psum_tensor(f"qp{i}", [128, 512], F32)) for i in range(8)]
        nq = (WCOLS + 1023) // 1024
        for q in range(nq):
            c1 = min(WCOLS, (q + 1) * 1024)
            dma(S, 'pool', win[:, :, q * 1024:c1], wview(wq)[:, :, q * 1024:c1], [], [('win', q)], f"win{q}")
        dma(S, 'sp', gq[:], io[gname], [], ['gq'], "gq")
        dma(S, 'pool', RT[:], io['ropeRT'], [], ['RT'], "RT")
        dma(S, 'pool', bones[:], io['blkones'], [], ['bones'], "bones")
        if kind == 'diff':
            ts(S, 'dve', gq[:, 0:1], gq[:, 0:1], 0.125, None, ALU.mult, None, ['gq'], ['gq'])
        else:
            ts(S, 'dve', gq[:, 0:1], gq[:, 0:1], 0.125, None, ALU.mult, None, ['gq'], ['gq'])
        tiles = [(i * 512, 512, 0) for i in range(8)] + [(SEQ, CTXL, 1)]
        n_ = 0
        no = 0
        nvv = 0
        for ti, (t0, W, s) in enumerate(tiles):
            sl = ti % 2
            hk = ('h', sl)
            ak = ('a', sl)
            tmp = dict(sq=sq, xn=xn, rs=rs, ps=pb[7], psk=('ps', 7))
            prenorm(S, g, hview(src)[:, :, t0:t0 + W], hb[sl][:, :, :W], hk, ab[sl][:, :, :W], ak, tmp, l, 0, s, W,
                    f"hld{sl}")
            dma(S, 'sp', cs[sl][:, :, :W], io['ropecs'][:, :, t0:t0 + W], [], [('cs', sl)], f"cs{sl}")
            chunks = list(range(NC_))
            if kind == 'swa' and s == 1:
                chunks = list(range(NQC, NC_))
            steps = []
            for ch in chunks:
                k2 = n_ % 2
                n_ += 1
                o3 = no % 3
                no += 1

                def A(ch=ch, k2=k2, W=W, sl=sl):
                    isq = ch < NQC
                    pp = pb[k2 * 3]
                    ppk = ('ps', k2 * 3)
                    col = ch * 128
                    for kc in range(8):
                        mm(S, pp[:, :W], win[:, kc, col:col + 128], ab[sl][:, kc, :W], kc == 0, kc == 7,
                           [('a', sl), ('win', col // 1024)], [ppk])
                    gcol = gq[:, 0:1] if isq else gq[:, 1:2]
                    act(S, xg[k2][:, :W], pp[:, :W], AF.Identity, [ppk, 'gq'], [('xg', k2)], scale=gcol)
                    act(S, xs[k2][:, :W], pp[:, :W], AF.Square, [ppk], [('xs', k2)])

                def B(ch=ch, k2=k2, o3=o3, W=W, sl=sl, t0=t0):
                    isq = ch < NQC
                    pm = pb[k2 * 3 + 1]
                    pr_ = pb[k2 * 3 + 2]
                    pmk, prk = ('ps', k2 * 3 + 1), ('ps', k2 * 3 + 2)
                    mm(S, pm[:, :W], bones[:], xs[k2][:, :W], True, True, [('xs', k2), 'bones'], [pmk])
                    mm(S, pr_[:, :W], RT[:], xg[k2][:, :W], True, True, [('xg', k2), 'RT'], [prk])
                    act(S, rd[k2][:, :W], pm[:, :W], AF.Sqrt, [pmk], [('rd', k2)], bias=g.epsb[:, 0:1], scale=1.0 / 64)
                    recip(S, rd[k2][:, :W], rd[k2][:, :W], [('rd', k2)], [('rd', k2)])
                    tt(S, 'pool', t1[k2][:, :W], xg[k2][:, :W], cs[sl][:, 0, :W], ALU.mult, [('xg', k2), ('cs', sl)], [('t1', k2)])
                    tt(S, 'dve', t2[k2][:, :W], pr_[:, :W], cs[sl][:, 1, :W], ALU.mult, [prk, ('cs', sl)], [('t2', k2)])
                    tt(S, 'pool', t1[k2][:, :W], t1[k2][:, :W], t2[k2][:, :W], ALU.add, [('t1', k2), ('t2', k2)], [('t1', k2)])
                    tt(S, 'dve', ob[o3][:, :W], t1[k2][:, :W], rd[k2][:, :W], ALU.mult, [('t1', k2), ('rd', k2)], [('ob', o3)])
                    dst_ = scr['QT'] if isq else scr['KT']
                    cc = ch if isq else ch - NQC
                    dma(S, 'sp', dst_[cc * 128:(cc + 1) * 128, t0:t0 + W], ob[o3][:, :W], [('ob', o3)], [], f"qst{o3}")

                steps.append((A, B))
            pipeline(steps, 1)
            for b in range(W // 128):
                for vq in range((VF + 511) // 512):
                    vw = min(512, VF - vq * 512)
                    v2 = nvv % 2
                    nvv += 1
                    pv = pb[6]
                    col = NC_ * 128 + vq * 512
                    for kc in range(8):
                        mm(S, pv[:, :vw], ab[sl][:, kc, b * 128:(b + 1) * 128], win[:, kc, col:col + vw], kc == 0, kc == 7,
                           [ak, ('win', col // 1024), ('win', (col + vw - 1) // 1024)], [('ps', 6)])
                    act(S, vb[v2][:, :vw], pv[:, :vw], AF.Copy, [('ps', 6)], [('vb', v2)])
                    r0 = t0 + b * 128
                    dma(S, 'sp', scr['V'][r0:r0 + 128, vq * 512:vq * 512 + vw], vb[v2][:, :vw], [('vb', v2)], [], f"vst{v2}")
        S.emit(nc, *sems)


def phase_att(nc, g, io, sems, src, dst, l, kind, scr):
    nc = _NCW(nc)
    S = Sched()
    NB = T // 128
    if kind == 'diff':
        NKC, VF = 8, 1024
    else:
        NKC, VF = 2, 256
    with ExitStack() as es:
        KT = es.enter_context(nc.sbuf_tensor("aKT", [128, NKC, T], BF16))
        V = es.enter_context(nc.sbuf_tensor("aV", [128, NB, VF], BF16))
        if kind == 'diff':
            wout = es.enter_context(nc.sbuf_tensor("awo", [128, 8, 1024], BF16))
            lp = es.enter_context(nc.sbuf_tensor("alp", [128, 4, 64], F32))
            pr2 = es.enter_context(nc.sbuf_tensor("apr2", [128, 2, 64], F32))
            s2 = es.enter_context(nc.sbuf_tensor("as2", [128, 2], F32))
            nlam = es.enter_context(nc.sbuf_tensor("anl", [128, 1], F32))
            sg = es.enter_context(nc.sbuf_tensor("asg", [128, 1], F32))
            OT = es.enter_context(nc.sbuf_tensor("aOT", [128, 8, 512], BF16))
            La = [[es.enter_context(nc.sbuf_tensor(f"aLa{r}{q}", [128, 512], F32)) for q in range(2)] for r in range(2)]
            onesf = es.enter_context(nc.sbuf_tensor("aonesf", [128, 128], F32))
        else:
            wout = es.enter_context(nc.sbuf_tensor("awo", [64, 16, 1024], BF16))
            snk = es.enter_context(nc.sbuf_tensor("asnk", [128, 16], F32))
            msk = es.enter_context(nc.sbuf_tensor("amsk", [128, 6, 512], BF16))
            identb = es.enter_context(nc.sbuf_tensor("aidb", [128, 128], BF16))
            OT = es.enter_context(nc.sbuf_tensor("aOT", [64, 16, 512], BF16))
        NSL = 1 if kind == 'diff' else 2
        if kind == 'diff':
            QZ = es.enter_context(nc.sbuf_tensor("aQZ", [128, 8, 1024], BF16))
            qz4 = QZ[:].rearrange("p c (r w) -> p c r w", r=2)
            hv = QZ[:].bitcast(F32)
            QT = hb = None
        else:
            QT = [es.enter_context(nc.sbuf_tensor(f"aQ{i}", [128, 8, 1024], BF16)) for i in range(NSL)]
            qzs = [q_[:].rearrange("p c (r w) -> p c r w", r=2) for q_ in QT]
            hb = [es.enter_context(nc.sbuf_tensor(f"ah{i}", [128, 8, 512], F32)) for i in range(NSL)]
        P = [es.enter_context(nc.sbuf_tensor(f"aP{i}", [128, 512], BF16)) for i in range(4)]
        PM = [es.enter_context(nc.sbuf_tensor(f"aPM{i}", [128, 512], BF16)) for i in range(2)] if kind != 'diff' else None
        r0b = es.enter_context(nc.sbuf_tensor("ar0", [128, 512], F32))
        r1b = es.enter_context(nc.sbuf_tensor("ar1", [128, 512], F32))
        o0 = es.enter_context(nc.sbuf_tensor("ao0", [128, 512], F32))
        o1 = r1b
        osq = es.enter_context(nc.sbuf_tensor("aosq", [128, 512], BF16))
        pb = [es.enter_context(nc.psum_tensor(f"ap{i}", [128, 512], F32)) for i in range(8)]
        for c in range(NKC):
            dma(S, 'sp', KT[:, c, :], scr['KT'][c * 128:(c + 1) * 128, :], [], [('KT', c)], f"kt{c % 4}")
        for q in range(4):
            b0, b1 = q * 9, min(NB, (q + 1) * 9)
            dma(S, 'sp', V[:, b0:b1, :], scr['V'][b0 * 128:b1 * 128, :VF].rearrange("(b p) f -> p b f", p=128),
                [], [('V', q)], f"v{q}")
        if kind == 'diff':
            dma(S, 'pool', wout[:], wview(io['da_out_w']), [], ['wout'], "wout")
            mset(S, 'pool', onesf[:], 1.0, [], ['onesf'])
            dma(S, 'sp', lp[:], io['da_lam'].partition_broadcast(128).rearrange("p o (a d) -> p (o a) d", a=4), [], ['lp'], "lp")
            dma(S, 'sp', sg[:], io['da_subg'], [], ['sg'], "sg")
            lam_init = 0.8 - 0.6 * math.exp(-0.3 * l)
            for i in range(2):
                tt(S, 'dve', pr2[:, i, :], lp[:, 2 * i, :], lp[:, 2 * i + 1, :], ALU.mult, ['lp'], ['pr2'])
            S.add('dve', (lambda o_, i_: (lambda e: e.tensor_reduce(out=o_, in_=i_, axis=AX.X, op=ALU.add)))(
                s2[:], pr2[:]), ['pr2'], ['s2'])
            act(S, s2[:], s2[:], AF.Exp, ['s2'], ['s2'])
            tt(S, 'dve', nlam[:], s2[:, 1:2], s2[:, 0:1], ALU.subtract, ['s2'], ['nlam'])
            ts(S, 'dve', nlam[:], nlam[:], -lam_init, None, ALU.add, None, ['nlam'], ['nlam'])
            ts(S, 'dve', sg[:], sg[:], 1.0 - lam_init, None, ALU.mult, None, ['sg'], ['sg'])
            tiles = [(i * 512, 512, 0) for i in range(8)] + [(SEQ, CTXL, 1)]
        else:
            dma(S, 'pool', wout[:], io['sw_out_wp'].rearrange("(h p) m -> p h m", p=64), [], ['wout'], "wout")
            dma(S, 'sp', snk[:], io['sw_sinkp'].partition_broadcast(128), [], ['snk'], "snk")
            dma(S, 'pool', msk[:], io['swa_mask'], [], ['msk'], "msk")
            dma(S, 'pool', identb[:], io['ident'], [], ['identb'], "identb")
            act(S, snk[:], snk[:], AF.Exp, ['snk'], ['snk'])
            for i_ in range(NSL):
                mset(S, 'pool', QT[i_][:], 0.0, [], [('Q', i_, 0), ('Q', i_, 1)])
            tiles = [(i * 512, 512, 0) for i in range(8)]
        np_ = 0
        for ti, (t0, W, s) in enumerate(tiles):
            sl = ti % NSL
            hk = ('h', sl)
            qk = ('Q', sl)
            if kind == 'diff':
                qsrc = scr['QT'].rearrange("(c p) t -> p c t", p=128)
                for r in range(2):
                    z0 = (1 - r) * 64
                    mset(S, 'pool', qz4[z0:z0 + 64, :, r, :W], 0.0, [], [('QZ', r)])
                    dma(S, 'sp', qz4[r * 64:(r + 1) * 64, :, r, :W], qsrc[r * 64:(r + 1) * 64, :, t0:t0 + W], [],
                        [('QZ', r)], f"qld{r}")
            else:
                dma(S, 'sp', hb[sl][:, :, :W], hview(src)[:, :, t0:t0 + W], [], [hk], f"hld{sl}")
                qsrc = scr['QT'].rearrange("(c p) t -> p c t", p=128)
                for r in range(2):
                    dma(S, 'sp', qzs[sl][r * 64:(r + 1) * 64, :, r, :W], qsrc[r * 64:(r + 1) * 64, :, t0:t0 + W], [],
                        [('Q', sl, r)], f"qld{sl}_{r}")
            if kind == 'diff':
                kbs = [(kb, None) for kb in (range(NB) if s == 0 else range(32, 34))]
                steps = []
                for vh in range(8):
                    for r in range(2):
                        for ki, (kb, _) in enumerate(kbs):
                            i3, i4 = np_ % 3, np_ % 4
                            np_ += 1
                            last = (ki == len(kbs) - 1)

                            def A(vh=vh, r=r, kb=kb, i3=i3, i4=i4, W=W, sl=sl):
                                mm(S, pb[i3][:, :W], KT[:, vh, kb * 128:(kb + 1) * 128],
                                   qz4[:, vh, r, :W], True, True, [('KT', vh), ('QZ', r)], [('ps', i3)])
                                act(S, P[i4][:, :W], pb[i3][:, :W], AF.Exp, [('ps', i3)], [('P', i4)])

                            def B(vh=vh, r=r, kb=kb, ki=ki, last=last, i4=i4, W=W):
                                po = pb[3 + r]
                                pok = ('ps', 3 + r)
                                mm(S, po[:, :W], V[:, kb, vh * 128:(vh + 1) * 128], P[i4][:, :W], ki == 0, last,
                                   [('V', kb // 9), ('P', i4)], [pok])
                                q_ = ki % 2
                                eng = 'dve' if q_ == 0 else 'pool'
                                lak = ('La', r, q_)
                                if ki < 2:
                                    cp(S, eng, La[r][q_][:, :W], P[i4][:, :W], [('P', i4)], [lak])
                                else:
                                    tt(S, eng, La[r][q_][:, :W], La[r][q_][:, :W], P[i4][:, :W], ALU.add, [lak, ('P', i4)], [lak])
                                if last and r == 1:
                                    for rr, rb, rk in ((0, r0b, 'r0'), (1, r1b, 'r1')):
                                        for q2 in range(2):
                                            mm(S, pb[5][:, :W], onesf[:], La[rr][q2][:, :W], q2 == 0, q2 == 1,
                                               [('La', rr, q2), 'onesf'], [('ps', 5)])
                                        recip(S, rb[:, :W], pb[5][:, :W], [('ps', 5)], [rk])
                                    tt(S, 'dve', o0[:, :W], pb[3][:, :W], r0b[:, :W], ALU.mult, [('ps', 3), 'r0'], ['o0'])
                                    tt(S, 'dve', r1b[:, :W], pb[4][:, :W], r1b[:, :W], ALU.mult, [('ps', 4), 'r1'], ['r1'])
                                    stt(S, o0[:, :W], r1b[:, :W], nlam[:, 0:1], o0[:, :W], ALU.mult, ALU.add,
                                        ['o0', 'r1', 'nlam'], ['o0'])
                                    tt(S, 'pool', osq[:, :W], o0[:, :W], o0[:, :W], ALU.mult, ['o0'], ['osq'])
                                    mm(S, pb[6][:, :W], g.ones[:], osq[:, :W], True, True, ['osq'], [('ps', 6)])
                                    act(S, r0b[:, :W], pb[6][:, :W], AF.Sqrt, [('ps', 6)], ['r0'], bias=g.epsb[:, 0:1],
                                        scale=1.0 / 128)
                                    recip(S, r0b[:, :W], r0b[:, :W], ['r0'], ['r0'])
                                    stt(S, OT[:, vh, :W], o0[:, :W], sg[:, 0:1], r0b[:, :W], ALU.mult, ALU.mult,
                                        ['o0', 'sg', 'r0'], ['OT'])

                            steps.append((A, B))
                pipeline(steps, 2)
                hks = [('QZ', 0), ('QZ', 1)]
                dma(S, 'sp', hv[:, :, :W], hview(src)[:, :, t0:t0 + W], [], hks, "hld0")
                for m in range(8):
                    po = pb[6 + (m % 2)]
                    pok = ('ps', 6 + (m % 2))
                    for vh in range(8):
                        mm(S, po[:, :W], wout[:, vh, m * 128:(m + 1) * 128], OT[:, vh, :W], vh == 0, vh == 7,
                           ['wout', 'OT'], [pok])
                    stt(S, hv[:, m, :W], po[:, :W], MV(g, l, 2, m, s), hv[:, m, :W], ALU.mult, ALU.add,
                        [pok] + hks, hks)
                dma(S, 'sp', hview(dst)[:, :, t0:t0 + W], hv[:, :, :W], hks, [], "hst0")
            else:
                j0 = t0 // 128
                kbs = [(32, None), (33, None)] + [(kb, kb - j0 + 1) for kb in range(j0 - 1, j0 + 5) if 0 <= kb < 32]
                steps = []
                SB = [0, 1, 6]
                for n in range(16):
                    for ki, (kb, mi) in enumerate(kbs):
                        sb_, i4 = SB[np_ % 3], np_ % 4
                        np_ += 1
                        last = (ki == len(kbs) - 1)

                        if mi is None:
                            c0, c1 = 0, W
                        else:
                            c0, c1 = max(0, (mi - 2) * 128), min(W, (mi + 1) * 128)

                        def A(n=n, kb=kb, mi=mi, sb_=sb_, i4=i4, W=W, sl=sl, c0=c0, c1=c1):
                            c, r = n // 2, n % 2
                            gk = SWA_PERM[n] // 4
                            mm(S, pb[sb_][:, c0:c1], KT[:, gk // 2, kb * 128:(kb + 1) * 128],
                               qzs[sl][:, c, r, c0:c1], True, mi is None, [('KT', gk // 2), ('Q', sl, r)], [('ps', sb_)])
                            if mi is not None:
                                mm(S, pb[sb_][:, c0:c1], identb[:], msk[:, mi, c0:c1], False, True, ['identb', 'msk'],
                                   [('ps', sb_)])
                            act(S, P[i4][:, c0:c1], pb[sb_][:, c0:c1], AF.Exp, [('ps', sb_)], [('P', i4)])

                        def B(n=n, kb=kb, ki=ki, last=last, i4=i4, W=W, c0=c0, c1=c1):
                            gk = SWA_PERM[n] // 4
                            po = pb[2 + 2 * (n % 2)]
                            pl = pb[3 + 2 * (n % 2)]
                            pok, plk = ('ps', 2 + 2 * (n % 2)), ('ps', 3 + 2 * (n % 2))
                            pu, puk = P[i4], ('P', i4)
                            mm(S, po[:64, c0:c1], V[:, kb, gk * 64:(gk + 1) * 64], pu[:, c0:c1], ki == 0, last,
                               [('V', kb // 9), puk], [pok])
                            mm(S, pl[:64, c0:c1], g.ones[:, :64], pu[:, c0:c1], ki == 0, last, [puk], [plk])
                            if last:
                                ts(S, 'dve', r0b[:64, :W], pl[:64, :W], snk[:64, n:n + 1], None, ALU.add, None,
                                   [plk, 'snk'], ['r0'])
                                recip(S, r0b[:64, :W], r0b[:64, :W], ['r0'], ['r0'])
                                tt(S, 'dve', OT[:, n, :W], po[:64, :W], r0b[:64, :W], ALU.mult, [pok, 'r0'], ['OT'])

                        steps.append((A, B))
                pipeline(steps, 2)
                for m in range(8):
                    po = pb[6 + (m % 2)]
                    pok = ('ps', 6 + (m % 2))
                    for n in range(16):
                        mm(S, po[:, :W], wout[:, n, m * 128:(m + 1) * 128], OT[:, n, :W], n == 0, n == 15,
                           ['wout', 'OT'], [pok])
                    stt(S, hb[sl][:, m, :W], po[:, :W], MV(g, l, 2, m, s), hb[sl][:, m, :W], ALU.mult, ALU.add,
                        [pok, hk], [hk])
            if kind != 'diff':
                dma(S, 'sp', hview(dst)[:, :, t0:t0 + W], hb[sl][:, :, :W], [hk], [], f"hst{sl}")
        if kind == 'swa':
            pass
        S.emit(nc, *sems)


IN_SPECS = [
    ("hT0", [D, T]), ("cT", [128, 8, 2]), ("ada_w", [4, D, 6 * D]), ("ada_bT", [128, 4, 48]),
    ("gmixT", [128, 4, 8]), ("gffnT", [128, 4, 8]), ("ident", [128, 128]),
    ("sc_in_w", [D, 3 * D]), ("sc_convT", [128, 8, 3]), ("sc_out_w", [D, D]),
    ("ffn_w1", [2, D, FF]), ("ffn_w3", [2, D, FF]), ("ffn_w2", [2, FF, D]),
    ("moe_rwT", [2, 128, 8, NE]), ("moe_rb", [2, 1, NE]),
    ("moe_w1", [2, NE, D, FF]), ("moe_w3", [2, NE, D, FF]), ("moe_w2", [2, NE, FF, D]),
    ("cm_in_w", [D, 4096]), ("cm_out_w", [2048, D]), ("cm_wsT", [128, 8, 128]), ("cm_buT", [128, 16]),
    ("cm_bv", [1, 2048]), ("cm_vgT", [128, 16]), ("cm_bsf", [1, 1024]),
    ("da_qkv_w", [D, 3072]), ("da_out_w", [D, D]), ("da_qkg", [128, 2]), ("da_lam", [1, 256]), ("da_subg", [128, 1]),
    ("sw_qkv_wp", [D, 1536]), ("sw_out_wp", [D, D]), ("sw_qkg", [128, 2]), ("sw_sinkp", [1, 16]),
    ("ropecs", [128, 2, T]), ("ropeRT", [128, 128]), ("blkones", [128, 128]), ("swa_mask", [128, 6, 512]),
]


def build(nphase):
    nc = bass.Bass("TRN2", target_bir_lowering=False)
    io = {}
    for name, shape in IN_SPECS:
        io[name] = nc.dram_tensor(name, shape, F32, kind="ExternalInput").ap()
    out = nc.dram_tensor("out", [D, T], F32, kind="ExternalOutput").ap()
    hA = nc.dram_tensor("hA", [D, T], F32, kind="Internal").ap()
    hB = nc.dram_tensor("hB", [D, T], F32, kind="Internal").ap()
    g = G()
    with ExitStack() as es:
        g.modv = es.enter_context(nc.sbuf_tensor("modv", [128, 4, 48, 2], F32))
        g.ones = es.enter_context(nc.sbuf_tensor("ones", [128, 128], BF16))
        g.ident = es.enter_context(nc.sbuf_tensor("ident_sb", [128, 128], F32))
        g.epsb = es.enter_context(nc.sbuf_tensor("epsb", [128, 1], F32))
        NSET = 3
        semsets = []
        for k in range(NSET):
            esem = {e: es.enter_context(nc.semaphore(f"es{k}_{e}")) for e in ENGS}
            dsems = [es.enter_context(nc.semaphore(f"ds{k}_{i}")) for i in range(NDSEM)]
            semsets.append((esem, dsems, dict(ebase={e: 0 for e in ENGS}, dbase=[0] * NDSEM)))
        with nc.Block() as blk:
            blk.vector(lambda e: e.memset(g.epsb[:], EPS))

        scr = dict(
            QT=nc.dram_tensor("scrQT", [D, T], BF16, kind="Internal").ap(),
            KT=nc.dram_tensor("scrKT", [D, T], BF16, kind="Internal").ap(),
            V=nc.dram_tensor("scrV", [T, D], BF16, kind="Internal").ap(),
        )
        P_ = [
            ('conv', lambda sm, a, b: phase_conv(nc, g, io, sm, a, b), True),
            ('ffn0', lambda sm, a, b: phase_ffn(nc, g, io, sm, a, b, 0, False, True), True),
            ('qkv1', lambda sm, a, b: phase_qkv(nc, g, io, sm, a, 1, 'diff', scr), False),
            ('att1', lambda sm, a, b: phase_att(nc, g, io, sm, a, b, 1, 'diff', scr), True),
            ('moe1', lambda sm, a, b: phase_ffn(nc, g, io, sm, a, b, 1, True, True), True),
            ('gmlp', lambda sm, a, b: phase_gmlp(nc, g, io, sm, a, b), True),
            ('ffn2', lambda sm, a, b: phase_ffn(nc, g, io, sm, a, b, 2, False, True), True),
            ('qkv3', lambda sm, a, b: phase_qkv(nc, g, io, sm, a, 3, 'swa', scr), False),
            ('att3', lambda sm, a, b: phase_att(nc, g, io, sm, a, b, 3, 'swa', scr), True),
            ('moe3', lambda sm, a, b: phase_ffn(nc, g, io, sm, a, b, 3, True, False), True),
        ]
        sel = os.environ.get("KPH")
        if sel is not None:
            idx = [int(x) for x in sel.split(",") if x != ""]
        else:
            idx = list(range(min(nphase, len(P_))))
        pi = 0
        phase_adaln(nc, g, io, semsets[0])
        cur = io['hT0']
        bufs = [hB, hA]
        nb_ = 0
        for ix in idx:
            pi += 1
            sm = semsets[pi % NSET]
            dstb = bufs[nb_ % 2]
            P_[ix][1](sm, cur, dstb)
            if P_[ix][2]:
                cur = dstb
                nb_ += 1
        last = cur if nb_ > 0 else None
        with nc.semaphore("fin") as fin, nc.Block() as blk:
            def _fin(e):
                if last is not None:
                    e.dma_start(out=out, in_=last).then_inc(fin, 16)
                    e.wait_ge(fin, 16)
            blk.sync(_fin)
    return nc


_NC_CACHE = {}
_CONST = {}


def _consts():
    if _CONST:
        return _CONST
    t = np.arange(SEQ)
    row = (t // 64).astype(np.float32)
    colp = (t % 64).astype(np.float32)
    inv = (np.float32(10000.0) ** (-np.arange(16, dtype=np.float32) / np.float32(16))).astype(np.float32)
    ang = np.zeros((64, SEQ), np.float32)
    for d in range(64):
        pos = row if d < 32 else colp
        ang[d] = pos * inv[d % 16]
    cos = np.ones((64, T), np.float32)
    sin = np.zeros((64, T), np.float32)
    cos[:, :SEQ] = np.cos(ang)
    sin[:, :SEQ] = np.sin(ang)
    cs = np.stack([np.concatenate([cos, cos], 0), np.concatenate([sin, sin], 0)], axis=1)
    R = np.zeros((64, 64), np.float32)
    for part in range(2):
        b = part * 32
        for i in range(16):
            R[b + i, b + i + 16] = -1.0
            R[b + i + 16, b + i] = 1.0
    RT = np.zeros((128, 128), np.float32)
    RT[:64, :64] = R.T
    RT[64:, 64:] = R.T
    bo = np.zeros((128, 128), np.float32)
    bo[:64, :64] = 1.0
    bo[64:, 64:] = 1.0
    kk = np.arange(128)[:, None, None]
    mi = np.arange(6)[None, :, None]
    qq = np.arange(512)[None, None, :]
    dlt = (mi - 1) * 128 + kk - qq
    mask = np.where(np.abs(dlt) <= 128, 0.0, -30000.0).astype(np.float32)
    _CONST.update(ropecs=np.ascontiguousarray(cs), ropeRT=RT, blkones=bo, swa_mask=np.ascontiguousarray(mask))
    return _CONST


def host_inputs(inputs, b):
    x = inputs['x'][b]
    ctx = inputs['ctx'][b]
    m = {}
    m['hT0'] = np.ascontiguousarray(np.concatenate([x.T, ctx.T], axis=1))
    cc = np.stack([inputs['c'][b], inputs['c_ctx']], axis=1)
    m['cT'] = np.ascontiguousarray(cc.reshape(8, 128, 2).transpose(1, 0, 2))
    m['ada_w'] = inputs['ada_w']
    m['ada_bT'] = np.ascontiguousarray(inputs['ada_b'].reshape(4, 48, 128).transpose(2, 0, 1))
    m['gmixT'] = np.ascontiguousarray(inputs['norm_mix_g'].reshape(4, 8, 128).transpose(2, 0, 1))
    m['gffnT'] = np.ascontiguousarray(inputs['norm_ffn_g'].reshape(4, 8, 128).transpose(2, 0, 1))
    m['ident'] = np.eye(128, dtype=np.float32)
    m['sc_in_w'] = inputs['sc_in_w'][0]
    m['sc_convT'] = np.ascontiguousarray(inputs['sc_conv_w'][0].reshape(3, 8, 128).transpose(2, 1, 0))
    m['sc_out_w'] = inputs['sc_out_w'][0]
    m['ffn_w1'] = inputs['ffn_w1']
    m['ffn_w3'] = inputs['ffn_w3']
    m['ffn_w2'] = inputs['ffn_w2']
    m['moe_rwT'] = np.ascontiguousarray(inputs['moe_router_w'].reshape(2, 8, 128, NE).transpose(0, 2, 1, 3))
    m['moe_rb'] = np.ascontiguousarray(inputs['moe_router_b'].reshape(2, 1, NE))
    m['moe_w1'] = inputs['moe_w1']
    m['cm_in_w'] = inputs['cm_in_w'][0]
    m['da_qkv_w'] = inputs['da_qkv_w'][0]
    m['da_out_w'] = inputs['da_out_w'][0]
    m['da_qkg'] = np.stack([np.tile(inputs['da_q_norm_g'][0], 2), np.tile(inputs['da_k_norm_g'][0], 2)], axis=1)
    m['da_lam'] = inputs['da_lambda'][0].reshape(1, 256)
    m['da_subg'] = inputs['da_sub_norm_g'][0].reshape(128, 1)
    wqkv = inputs['sw_qkv_w'][0]
    perm = np.array(SWA_PERM)
    qcols = (perm[:, None] * 64 + np.arange(64)[None, :]).reshape(-1)
    m['sw_qkv_wp'] = np.concatenate([wqkv[:, qcols], wqkv[:, 1024:]], axis=1)
    m['sw_out_wp'] = inputs['sw_out_w'][0][qcols, :]
    m['sw_qkg'] = np.stack([np.tile(inputs['sw_q_norm_g'][0], 2), np.tile(inputs['sw_k_norm_g'][0], 2)], axis=1)
    m['sw_sinkp'] = inputs['sw_sink'][0][perm].reshape(1, 16)
    m.update(_consts())
    m['cm_out_w'] = inputs['cm_out_w'][0]
    m['cm_wsT'] = np.ascontiguousarray(inputs['cm_ws'][0].transpose(2, 0, 1))
    m['cm_buT'] = np.ascontiguousarray(inputs['cm_in_b'][0][:2048].reshape(16, 128).T)
    m['cm_bv'] = np.ascontiguousarray(inputs['cm_in_b'][0][2048:].reshape(1, 2048))
    m['cm_vgT'] = np.ascontiguousarray(inputs['cm_v_norm_g'][0].reshape(16, 128).T)
    m['cm_bsf'] = np.ascontiguousarray(inputs['cm_bs'][0].reshape(1, 1024))
    m['moe_w3'] = inputs['moe_w3']
    m['moe_w2'] = inputs['moe_w2']
    return {k: np.ascontiguousarray(v, dtype=np.float32) for k, v in m.items()}


def kernel(**inputs):
    nphase = int(os.environ.get("KSTOP", "99"))
    ncores = int(os.environ.get("KCORES", "8"))
    inputs = {k: np.asarray(v) for k, v in inputs.items()}
    if nphase not in _NC_CACHE:
        _NC_CACHE[nphase] = build(nphase)
    nc = _NC_CACHE[nphase]
    in_maps = [host_inputs(inputs, b) for b in range(ncores)]
    res = run_bass_kernel_spmd(nc, in_maps, core_ids=list(range(ncores)))
    outs = [r["out"] for r in res.results]
    if os.environ.get("KRAW"):
        return outs
    full = np.stack([o[:, :SEQ].T for o in outs], axis=0)
    return np.ascontiguousarray(full.astype(np.float32))
```
